# Optimizing a Trainium2 kernel written in Bass

```python
import jax, jax.numpy as jnp
from jax import lax
import numpy as np

D_MODEL = 1024
BATCH = 32
SEQ = 2048
DEPTH = 1

N_HEADS = 16
HEAD_DIM = 64
ATTN_WIDTH = N_HEADS * HEAD_DIM
KV_LATENT = 128
IDX_HEADS = 8
IDX_DIM = 64
TOPK_MAX = 256
Q_BLOCK = 128
RNN_WIDTH = 1024
RNN_BLOCKS = 16
RNN_BLOCK_DIM = RNN_WIDTH // RNN_BLOCKS
CONV_WIDTH = 4
LRU_C = 8.0
NORM_EPS = 1e-6
SPLITS = (ATTN_WIDTH, KV_LATENT, IDX_HEADS * IDX_DIM, IDX_DIM, IDX_HEADS, ATTN_WIDTH, RNN_WIDTH, RNN_WIDTH, 2 * D_MODEL)
IN_WIDTH = sum(SPLITS)

kernel_name = 'hybrid_dsa_rglru_gated_block'


def rms_norm(x, g):
    xf = x.astype(jnp.float32)
    y = xf * lax.rsqrt(jnp.mean(xf * xf, axis=-1, keepdims=True) + NORM_EPS)
    return (y * g.astype(jnp.float32)).astype(x.dtype)


def layer_norm(x, g, b):
    xf = x.astype(jnp.float32)
    mu = jnp.mean(xf, axis=-1, keepdims=True)
    xc = xf - mu
    y = xc * lax.rsqrt(jnp.mean(xc * xc, axis=-1, keepdims=True) + NORM_EPS)
    return (y * g.astype(jnp.float32) + b.astype(jnp.float32)).astype(x.dtype)


def alibi_slopes(n):
    return jnp.exp2(-8.0 * jnp.arange(1, n + 1, dtype=jnp.float32) / n)


def split_cols(z):
    offsets = np.cumsum(np.array(SPLITS))[:-1].tolist()
    return jnp.split(z, offsets, axis=-1)


def sparse_mla_attention(q, c_kv, q_idx, k_idx, w_idx, w_uk, w_uv):
    B, S = q.shape[0], q.shape[1]
    topk = min(TOPK_MAX, S // 4)
    tb = min(Q_BLOCK, S)
    nblk = S // tb
    f32 = jnp.float32
    q_lat = jnp.einsum('bshd,hcd->bshc', q, w_uk) * (HEAD_DIM ** -0.5)
    slopes = alibi_slopes(N_HEADS)
    key_pos = jnp.arange(S, dtype=jnp.int32)
    k_idx_f = k_idx.astype(f32)

    def to_blocks(a):
        return a.reshape((B, nblk, tb) + a.shape[2:]).swapaxes(0, 1)

    def block_fn(args):
        ql, qi, wi, t0 = args
        t = t0 + jnp.arange(tb, dtype=jnp.int32)
        s_idx = jax.nn.relu(jnp.einsum('btjd,bsd->btjs', qi.astype(f32), k_idx_f))
        s_idx = jnp.einsum('btjs,btj->bts', s_idx, wi.astype(f32))
        causal = key_pos[None, :] <= t[:, None]
        s_idx = jnp.where(causal[None], s_idx, -jnp.inf)
        _, idx = lax.top_k(s_idx, topk)
        c_sel = jax.vmap(lambda c, i: c[i])(c_kv, idx).astype(f32)
        logits = jnp.einsum('bthc,btkc->bthk', ql.astype(f32), c_sel)
        dist = (t[None, :, None] - idx).astype(f32)
        logits = logits - slopes[None, None, :, None] * dist[:, :, None, :]
        valid = idx <= t[None, :, None]
        logits = jnp.where(valid[:, :, None, :], logits, -jnp.inf)
        p = jax.nn.softmax(logits, axis=-1)
        o = jnp.einsum('bthk,btkc->bthc', p, c_sel)
        return o.astype(ql.dtype)

    t0s = jnp.arange(nblk, dtype=jnp.int32) * tb
    o_lat = lax.map(block_fn, (to_blocks(q_lat), to_blocks(q_idx), to_blocks(w_idx), t0s))
    o_lat = o_lat.swapaxes(0, 1).reshape(B, S, N_HEADS, KV_LATENT)
    return jnp.einsum('bshc,hcd->bshd', o_lat, w_uv)


def causal_depthwise_conv(x, w, b):
    S = x.shape[1]
    xp = jnp.pad(x, ((0, 0), (CONV_WIDTH - 1, 0), (0, 0)))
    out = b
    for k in range(CONV_WIDTH):
        out = out + xp[:, k:k + S] * w[k]
    return out


def rg_lru(x, w_a, b_a, w_x, b_x, lam):
    B, S, C = x.shape
    f32 = jnp.float32
    xb = x.reshape(B, S, RNN_BLOCKS, RNN_BLOCK_DIM)
    r = jax.nn.sigmoid(jnp.einsum('bsgi,gij->bsgj', xb, w_a).reshape(B, S, C) + b_a).astype(f32)
    i = jax.nn.sigmoid(jnp.einsum('bsgi,gij->bsgj', xb, w_x).reshape(B, S, C) + b_x).astype(f32)
    log_a = -LRU_C * r * jax.nn.softplus(-lam.astype(f32))
    a = jnp.exp(log_a)
    b_in = jnp.sqrt(-jnp.expm1(2.0 * log_a)) * (i * x.astype(f32))

    def combine(left, right):
        a1, b1 = left
        a2, b2 = right
        return a1 * a2, a2 * b1 + b2

    _, h = lax.associative_scan(combine, (a, b_in), axis=1)
    return h.astype(x.dtype)


def hybrid_layer(x, norm_g, w_in, b_merge, kv_norm_g, w_uk, w_uv, idx_ln_g, idx_ln_b,
                 w_attn_proj, conv_w, conv_b, w_rg_a, b_rg_a, w_rg_x, b_rg_x, lru_lambda,
                 w_rnn_proj, w_out):
    B, S, _ = x.shape
    xn = rms_norm(x, norm_g)
    z = xn @ w_in
    q, c_kv, q_idx, k_idx, w_idx, g_attn, x_rnn, g_rnn, merge = split_cols(z)
    q = q.reshape(B, S, N_HEADS, HEAD_DIM)
    c_kv = rms_norm(c_kv, kv_norm_g)
    q_idx = q_idx.reshape(B, S, IDX_HEADS, IDX_DIM) * (IDX_DIM ** -0.5)
    k_idx = layer_norm(k_idx, idx_ln_g, idx_ln_b)
    w_idx = w_idx * (IDX_HEADS ** -0.5)
    o = sparse_mla_attention(q, c_kv, q_idx, k_idx, w_idx, w_uk, w_uv).reshape(B, S, ATTN_WIDTH)
    y_attn = (o * jax.nn.silu(g_attn)) @ w_attn_proj
    h = rg_lru(causal_depthwise_conv(x_rnn, conv_w, conv_b), w_rg_a, b_rg_a, w_rg_x, b_rg_x, lru_lambda)
    y_rnn = (h * jax.nn.silu(g_rnn)) @ w_rnn_proj
    gates = jax.nn.sigmoid(merge + b_merge)
    g_a, g_r = gates[..., :D_MODEL], gates[..., D_MODEL:]
    mixed = g_a * y_attn + g_r * y_rnn
    return x + mixed @ w_out


def setup_inputs(seed: int = 0) -> dict:
    key = jax.random.key(seed)
    ks = jax.random.split(key, 24)
    f32 = jnp.float32

    def nrm(k, shape, scale):
        return jax.random.normal(k, shape, f32) * scale

    u = jax.random.uniform(ks[20], (DEPTH, RNN_WIDTH), f32, minval=0.9, maxval=0.999)
    return {
        'x': jax.random.normal(ks[0], (BATCH, SEQ, D_MODEL), f32),
        'norm_gain': 1.0 + nrm(ks[1], (DEPTH, D_MODEL), 0.01),
        'w_in': nrm(ks[2], (DEPTH, D_MODEL, IN_WIDTH), D_MODEL ** -0.5),
        'b_merge': nrm(ks[3], (DEPTH, 2 * D_MODEL), 0.01),
        'kv_norm_gain': 1.0 + nrm(ks[4], (DEPTH, KV_LATENT), 0.01),
        'w_uk': nrm(ks[5], (DEPTH, N_HEADS, KV_LATENT, HEAD_DIM), KV_LATENT ** -0.5),
        'w_uv': nrm(ks[6], (DEPTH, N_HEADS, KV_LATENT, HEAD_DIM), KV_LATENT ** -0.5),
        'idx_ln_gain': 1.0 + nrm(ks[7], (DEPTH, IDX_DIM), 0.01),
        'idx_ln_bias': nrm(ks[8], (DEPTH, IDX_DIM), 0.01),
        'w_attn_proj': nrm(ks[9], (DEPTH, ATTN_WIDTH, D_MODEL), ATTN_WIDTH ** -0.5),
        'conv_w': nrm(ks[10], (DEPTH, CONV_WIDTH, RNN_WIDTH), CONV_WIDTH ** -0.5),
        'conv_b': nrm(ks[11], (DEPTH, RNN_WIDTH), 0.01),
        'w_rg_a': nrm(ks[12], (DEPTH, RNN_BLOCKS, RNN_BLOCK_DIM, RNN_BLOCK_DIM), RNN_BLOCK_DIM ** -0.5),
        'b_rg_a': nrm(ks[13], (DEPTH, RNN_WIDTH), 0.01),
        'w_rg_x': nrm(ks[14], (DEPTH, RNN_BLOCKS, RNN_BLOCK_DIM, RNN_BLOCK_DIM), RNN_BLOCK_DIM ** -0.5),
        'b_rg_x': nrm(ks[15], (DEPTH, RNN_WIDTH), 0.01),
        'lru_lambda': jnp.log(u) - jnp.log1p(-u),
        'w_rnn_proj': nrm(ks[16], (DEPTH, RNN_WIDTH, D_MODEL), RNN_WIDTH ** -0.5),
        'w_out': nrm(ks[17], (DEPTH, D_MODEL, D_MODEL), D_MODEL ** -0.5),
        'final_norm_gain': 1.0 + nrm(ks[18], (D_MODEL,), 0.01),
    }


def reference(x, norm_gain, w_in, b_merge, kv_norm_gain, w_uk, w_uv, idx_ln_gain, idx_ln_bias,
              w_attn_proj, conv_w, conv_b, w_rg_a, b_rg_a, w_rg_x, b_rg_x, lru_lambda,
              w_rnn_proj, w_out, final_norm_gain):
    for l in range(DEPTH):
        x = hybrid_layer(x, norm_gain[l], w_in[l], b_merge[l], kv_norm_gain[l], w_uk[l], w_uv[l],
                         idx_ln_gain[l], idx_ln_bias[l], w_attn_proj[l], conv_w[l], conv_b[l],
                         w_rg_a[l], b_rg_a[l], w_rg_x[l], b_rg_x[l], lru_lambda[l],
                         w_rnn_proj[l], w_out[l])
    return rms_norm(x, final_norm_gain)
```

```python
import numpy as np
import concourse.bass as bass
import concourse.mybir as mybir
from concourse.bass_utils import run_bass_kernel_spmd

F32 = mybir.dt.float32
BF16 = mybir.dt.bfloat16
ALU = mybir.AluOpType
AF = mybir.ActivationFunctionType
AX = mybir.AxisListType

D = 1024
NCORES = 8
EPS = 1e-6
TOPK = 256
NBIS = 12


class Buf:
    __slots__ = ("name", "w", "r")

    def __init__(self, name):
        self.name = name
        self.w = None
        self.r = []


class Sched:
    def __init__(self, nc):
        self.nc = nc
        self.eng = {"pe": nc.tensor, "act": nc.scalar, "dve": nc.vector, "pool": nc.gpsimd, "sp": nc.sync}
        self.sem, self.cnt, self.waited, self.dsem = {}, {}, {}, {}
        for k in self.eng:
            self.sem[k] = nc.semaphore("s_" + k).__enter__()
            self.cnt[k] = 0
            self.waited[k] = {}

    def _wait(self, e, dep):
        de, val = dep
        if de == e and e == "pe":
            return
        if self.waited[e].get(de, 0) >= val:
            return
        self.waited[e][de] = val
        s = self.dsem[de][0] if de.startswith("dma:") else self.sem[de]
        self.eng[e].wait_ge(s, val)

    def _deps(self, e, reads, writes):
        deps = set()
        for b in reads:
            if b.w is not None:
                deps.add(b.w)
        for b in writes:
            if b.w is not None:
                deps.add(b.w)
            deps.update(b.r)
        for d in sorted(deps):
            self._wait(e, d)

    def _mark(self, me, reads, writes):
        for b in reads:
            if len(b.r) > 24:
                last = {}
                for (q, v) in b.r:
                    last[q] = max(last.get(q, 0), v)
                b.r = list(last.items())
            b.r.append(me)
        for b in writes:
            b.w = me
            b.r = []

    def op(self, e, fn, reads=(), writes=()):
        self._deps(e, reads, writes)
        ins = fn(self.eng[e])
        self.cnt[e] += 1
        ins.then_inc(self.sem[e], 1)
        me = (e, self.cnt[e])
        self._mark(me, reads, writes)
        return me

    def dma(self, e, q, out, in_, reads=(), writes=()):
        key = "dma:" + q
        if key not in self.dsem:
            self.dsem[key] = [self.nc.semaphore("d_" + q).__enter__(), 0]
        self._deps(e, reads, writes)
        ins = self.eng[e].dma_start(out=out, in_=in_)
        self.dsem[key][1] += 16
        ins.then_inc(self.dsem[key][0], 16)
        me = (key, self.dsem[key][1])
        self._mark(me, reads, writes)
        return me


def _bf16_split(v):
    import ml_dtypes
    hi = float(np.float32(v).astype(ml_dtypes.bfloat16))
    lo = float(np.float32(v - hi).astype(ml_dtypes.bfloat16))
    return hi, lo


class _Stop(Exception):
    pass


def build(nseq, S, debug=False, stop=None):
    NBLK = S // 512
    NTS = S // 128
    nc = bass.Bass("TRN2", target_bir_lowering=False)
    S_ = Sched(nc)

    def din(name, shape):
        return nc.dram_tensor(name, list(shape), F32, kind="ExternalInput").ap()

    x = din("x", [nseq, S, D])
    norm_gain = din("norm_gain", [D])
    w_in = din("w_in", [D, 6856])
    b_merge = din("b_merge", [2048])
    kv_norm_gain = din("kv_norm_gain", [128])
    w_uk = din("w_uk", [16, 128, 64])
    w_uv = din("w_uv", [16, 128, 64])
    idx_ln_gain = din("idx_ln_gain", [64])
    idx_ln_bias = din("idx_ln_bias", [64])
    w_attn_proj = din("w_attn_proj", [D, D])
    conv_w = din("conv_w", [4, D])
    conv_b = din("conv_b", [D])
    w_rg_a = din("w_rg_a", [16, 64, 64])
    b_rg_a = din("b_rg_a", [D])
    w_rg_x = din("w_rg_x", [16, 64, 64])
    b_rg_x = din("b_rg_x", [D])
    lru_lambda = din("lru_lambda", [D])
    w_rnn_proj = din("w_rnn_proj", [D, D])
    w_out = din("w_out", [D, D])
    final_norm_gain = din("final_norm_gain", [D])
    out = nc.dram_tensor("out", [nseq, S, D], F32, kind="ExternalOutput").ap()
    wbf = nc.dram_tensor("wbf", [70, 128, 1024], BF16, kind="Internal").ap()

    def sb(name, shape, dt=F32):
        return nc.sbuf_tensor(name, list(shape), dt).__enter__()

    CH = {}
    chunk_src = []

    def add_chunks(name, src, col0, n, width=128):
        CH[name] = []
        for c in range(n):
            CH[name].append(len(chunk_src))
            chunk_src.append((src, col0 + c * 128, width))

    add_chunks("q", w_in, 0, 8)
    add_chunks("ckv", w_in, 1024, 1)
    add_chunks("qidx", w_in, 1152, 4)
    add_chunks("kw", w_in, 1664, 1, 72)
    add_chunks("gattn", w_in, 1736, 8)
    add_chunks("xrnn", w_in, 2760, 8)
    add_chunks("grnn", w_in, 3784, 8)
    add_chunks("mga", w_in, 4808, 8)
    add_chunks("mgr", w_in, 5832, 8)
    add_chunks("wap", w_attn_proj, 0, 8)
    add_chunks("wrp", w_rnn_proj, 0, 8)
    assert len(chunk_src) == 70
    Bwbf = [Buf("wbf%d" % i) for i in range(70)]

    ident = sb("ident", [128, 128], BF16); Bident = Buf("ident")
    ones_bf = sb("ones_bf", [128, 128], BF16)
    triT = sb("triT", [128, 128], BF16)
    causal_neg = sb("causal_neg", [128, 128])
    iota_row = sb("iota_row", [128, 128])
    pidx = sb("pidx", [128, 1])
    Bconst = Buf("const")

    wout_sb = sb("wout_sb", [128, 8, 1024], BF16)
    wuvP = sb("wuvP", [128, 16, 128], BF16)
    wuk_nat = sb("wuk_nat", [128, 16, 64], BF16)
    wukTz = sb("wukTz", [128, 16, 128], BF16)
    BDa = sb("BDa", [128, 8, 128], BF16)
    BDx = sb("BDx", [128, 8, 128], BF16)
    gcol = sb("gcol", [128, 8])
    bmcol = sb("bmcol", [128, 16])
    cwcol = sb("cwcol", [128, 4, 8])
    cbcol = sb("cbcol", [128, 8])
    bacol = sb("bacol", [128, 8])
    bxcol = sb("bxcol", [128, 8])
    lamcol = sb("lamcol", [128, 8])
    kap = sb("kap", [128, 8])
    kap2 = sb("kap2", [128, 8])
    fg_bc = sb("fg_bc", [128, 1024])
    gkv_bc = sb("gkv_bc", [128, 128])
    lng_bc = sb("lng_bc", [128, 64])
    lnb_bc = sb("lnb_bc", [128, 64])
    A_all = sb("A_all", [32, 16, 128], BF16)
    Bcol_hi = sb("Bcol_hi", [32, 16])
    Bcol_lo = sb("Bcol_lo", [32, 16])
    Bcol = sb("Bcol", [32, 16])
    e0 = sb("e0", [32, 1]); e12 = sb("e12", [32, 1]); e34 = sb("e34", [32, 1])
    e13 = sb("e13", [32, 1]); e24 = sb("e24", [32, 1]); etmp = sb("etmp", [32, 1])
    cold = sb("cold", [32, 16])
    Bwork = [sb("Bwork%d" % g, [32, 512], BF16) for g in range(4)]
    BBwork = [Buf("Bwork%d" % g) for g in range(4)]
    pow2 = sb("pow2", [128, NBIS])
    nhc = sb("nhc", [128, 1]); nhc_bf = sb("nhc_bf", [128, 1], BF16)

    cT = sb("cT", [128, S], BF16); BcT = Buf("cT")
    c_tm = sb("c_tm", [128, NTS, 128], BF16); Bctm = Buf("c_tm")
    kz = [sb("kz%d" % i, [128, S], BF16) for i in range(2)]; BkT = Buf("kidxT")
    w_tm = sb("w_tm", [128, NTS, 8]); Bwtm = Buf("w_tm")
    cabs = sb("cabs", [128, 1]); cabs_b = sb("cabs_b", [128, 1]); ncabs = sb("ncabs", [128, 1], BF16)
    Bcabs = Buf("cabs")
    xnT = sb("xnT", [128, 8, 512], BF16); BxnT = Buf("xnT")
    qT = sb("qT", [128, 8, 512], BF16); BqT = Buf("qT")
    sgT = sb("sgT", [128, 8, 512], BF16); BsgT = [Buf("sgT%d" % c) for c in range(8)]
    hgT = sb("hgT", [128, 8, 512], BF16); BhgT = Buf("hgT")
    qidxT = sb("qidxT", [128, 4, 512], BF16); BqidxT = Buf("qidxT")
    wch = [sb("wch%d" % i, [128, 8, 128], BF16) for i in range(4)]
    Bwch = [Buf("wch%d" % i) for i in range(4)]
    xt = [sb("xt%d" % i, [128, 1024]) for i in range(2)]
    Bxt = [Buf("xt%d" % i) for i in range(2)]
    xs = sb("xs", [128, 1024], BF16); Bxs = Buf("xs")
    junk = sb("junk", [128, 2048], BF16); Bjunk_act = Buf("junk_act"); Bjunk_dve = Buf("junk_dve")
    st = sb("st", [128, 16]); Bst = Buf("st")
    st2 = sb("st2", [128, 16]); Bst2 = Buf("st2")
    kc = sb("kc", [128, 64]); Bkc = Buf("kc")
    kn2 = sb("kn2", [128, 128], BF16); Bkn2 = Buf("kn2")
    ub = [sb("ub%d" % k, [128, 515], BF16) for k in range(2)]; Bub = [Buf("ub%d" % k) for k in range(2)]
    ucar = sb("ucar", [128, 8, 3], BF16); Bucar = [Buf("ucar%d" % c) for c in range(8)]
    xc_s = [sb("xc%d" % k, [128, 512]) for k in range(2)]; Bxc_s = [Buf("xc%d" % k) for k in range(2)]
    xcb_s = [sb("xcb%d" % k, [128, 512], BF16) for k in range(2)]; Bxcb_s = [Buf("xcb%d" % k) for k in range(2)]
    r_s = [sb("r%d" % k, [128, 512]) for k in range(2)]; Br_s = [Buf("r%d" % k) for k in range(2)]
    i_s = [sb("i%d" % k, [128, 512]) for k in range(2)]; Bi_s = [Buf("i%d" % k) for k in range(2)]
    a_s = [sb("a%d" % k, [128, 512]) for k in range(2)]; Ba_s = [Buf("a%d" % k) for k in range(2)]
    s_s = [sb("s%d" % k, [128, 512]) for k in range(2)]; Bs_s = [Buf("s%d" % k) for k in range(2)]
    sgr_s = [sb("sgr%d" % k, [128, 512], BF16) for k in range(2)]; Bsgr_s = [Buf("sgr%d" % k) for k in range(2)]
    r_t, Br, i_t, Bi, a_t, Ba, s_t, Bs = r_s[0], Br_s[0], i_s[0], Bi_s[0], a_s[0], Ba_s[0], s_s[0], Bs_s[0]
    ptmp, Bptmp = s_s[1], Bs_s[1]
    hcar = sb("hcar", [128, 8]); Bhcar = [Buf("hcar%d" % c) for c in range(8)]
    I_t = sb("I_t", [128, 2048]); BI = Buf("I")
    rj = [sb("rj%d" % i, [128, 512]) for i in range(2)]; Brj = [Buf("rj%d" % i) for i in range(2)]
    mask = sb("mask", [128, 2048], BF16); Bmask = Buf("mask")
    nmask = sb("nmask", [128, 16, 512], BF16); Bnmask = [Buf("nmask%d" % j) for j in range(16)]
    identrep4 = sb("identrep4", [128, 512], BF16)
    SI = sb("SI", [128, 4, 512], BF16)
    iota1_full = sb("iota1_full", [128, 2048], BF16)
    ntri_rep = sb("ntri_rep", [128, 512], BF16)
    nsm_s = [sb("nsm%d" % k, [128, 1], BF16) for k in range(2)]; Bnsm_s = [Buf("nsm%d" % k) for k in range(2)]
    bis_s = [sb("bis%d" % k, [128, 8]) for k in range(2)]; Bbis_s = [Buf("bis%d" % k) for k in range(2)]
    steps = sb("steps", [128, NBIS]); steps2 = sb("steps2", [128, NBIS]); Bsteps = Buf("steps")
    qlb_s = [sb("qlb%d" % k, [128, 512], BF16) for k in range(4)]; Bqlb_s = [Buf("qlb%d" % k) for k in range(4)]
    absq = sb("absq", [128, 512], BF16); Babsq = Buf("absq")
    p_t = [sb("p%d" % i, [128, 512], BF16) for i in range(2)]; Bp = [Buf("p%d" % i) for i in range(2)]
    rs_t = sb("rs_t", [128, 512]); Brs = Buf("rs")
    oTn = sb("oTn", [128, 512], BF16); BoTn = Buf("oTn")
    ga_t, Bga = r_t, Br
    gr_t, Bgr = i_t, Bi
    m1_t, Bm1 = a_t, Ba
    m2_t, Bm2 = s_t, Bs
    Bres = [Buf("res0"), Buf("res1")]

    ps = [nc.psum_tensor("ps%d" % i, [128, 512], F32).__enter__() for i in range(8)]
    Bps = [Buf("ps%d" % i) for i in range(8)]

    def psbf(i):
        return ps[i][:, :].bitcast(BF16)

    op = S_.op

    with nc.allow_non_contiguous_dma(reason="one-time small parameter layouts"):
        for ci, (src, col0, wd) in enumerate(chunk_src):
            S_.dma("pool", "cv", wbf[ci].rearrange("p (k j) -> p k j", k=8)[:, :, 0:wd],
                   src.rearrange("(k p) n -> p k n", p=128)[:, :, col0:col0 + wd], writes=[Bwbf[ci]])
        for b_ in Bwbf:
            b_.w = ("dma:cv", S_.dsem["dma:cv"][1])
        S_.dma("pool", "cs", wout_sb[:, :, :], w_out.rearrange("(k p) n -> p k n", p=128), writes=[Bconst])
        op("pool", lambda e: e.memset(wuvP[:, :, :], 0.0), writes=[Bconst])
        for par in range(2):
            S_.dma("pool", "cs", wuvP[:, :, :].rearrange("c (hp two) d -> c hp two d", two=2)[:, :, par, par * 64:par * 64 + 64],
                   w_uv.rearrange("(hp two) c d -> c hp two d", two=2)[:, :, par, :], reads=[Bconst], writes=[Bconst])
        S_.dma("pool", "cs", wuk_nat[:, :, :], w_uk.rearrange("h c d -> c h d"), writes=[Bconst])
        op("pool", lambda e: e.memset(BDa[:, :, :], 0.0), writes=[Bconst])
        op("pool", lambda e: e.memset(BDx[:, :, :], 0.0), writes=[Bconst])
        for (bd, wsrc) in ((BDa, w_rg_a), (BDx, w_rg_x)):
            for par in range(2):
                S_.dma("pool", "cs", bd[par * 64:par * 64 + 64, :, par * 64:par * 64 + 64],
                       wsrc.rearrange("(p two) i j -> two i p j", two=2)[par], reads=[Bconst], writes=[Bconst])
        for (dst, src, n) in ((gcol, norm_gain, 8), (bmcol, b_merge, 16), (cbcol, conv_b, 8), (bacol, b_rg_a, 8),
                              (bxcol, b_rg_x, 8), (lamcol, lru_lambda, 8)):
            S_.dma("sp", "cs2", dst[:, :], src.rearrange("(c p) -> p c", p=128), writes=[Bconst])
        S_.dma("sp", "cs2", cwcol[:, :, :], conv_w.rearrange("k (c p) -> p k c", p=128), writes=[Bconst])
        S_.dma("sp", "cs2", fg_bc[:, :], final_norm_gain.partition_broadcast(128), writes=[Bconst])
        S_.dma("sp", "cs2", gkv_bc[:, :], kv_norm_gain.partition_broadcast(128), writes=[Bconst])
        S_.dma("sp", "cs2", lng_bc[:, :], idx_ln_gain.partition_broadcast(128), writes=[Bconst])
        S_.dma("sp", "cs2", lnb_bc[:, :], idx_ln_bias.partition_broadcast(128), writes=[Bconst])

    for par in range(2):
        op("pool", lambda e, par=par: e.memset(kz[par][:, :], 0.0), writes=[BkT])
    op("pool", lambda e: e.iota(iota_row[:, :], [[1, 128]], base=0, channel_multiplier=0,
                                allow_small_or_imprecise_dtypes=True), writes=[Bconst])
    op("pool", lambda e: e.iota(pidx[:, :], [[0, 1]], base=0, channel_multiplier=1,
                                allow_small_or_imprecise_dtypes=True), reads=[Bconst], writes=[Bconst])
    op("dve", lambda e: e.tensor_scalar(out=ident[:, :], in0=iota_row[:, :], scalar1=pidx[:, 0:1], scalar2=None,
                                        op0=ALU.is_equal), reads=[Bconst], writes=[Bconst, Bident])
    op("dve", lambda e: e.tensor_scalar(out=triT[:, :], in0=iota_row[:, :], scalar1=pidx[:, 0:1], scalar2=None,
                                        op0=ALU.is_ge), reads=[Bconst], writes=[Bconst])
    op("dve", lambda e: e.tensor_scalar(out=causal_neg[:, :], in0=iota_row[:, :], scalar1=pidx[:, 0:1], scalar2=-1e30,
                                        op0=ALU.is_gt, op1=ALU.mult), reads=[Bconst], writes=[Bconst])
    op("dve", lambda e: e.memset(ones_bf[:, :], 1.0), reads=[Bconst], writes=[Bconst])
    for k in range(NBIS):
        op("dve", lambda e, k=k: e.memset(pow2[:, k:k + 1], 2.0 ** -(k + 1)), reads=[Bconst], writes=[Bconst])
    op("dve", lambda e: e.tensor_reduce(out=nhc[:, 0:1], in_=gkv_bc[:, :], axis=AX.X, op=ALU.max, apply_absolute_value=True),
       reads=[Bconst], writes=[Bconst])
    op("dve", lambda e: e.tensor_scalar(out=nhc[:, 0:1], in0=nhc[:, 0:1], scalar1=-0.5 * (128.0 ** 0.5), scalar2=None, op0=ALU.mult),
       reads=[Bconst], writes=[Bconst])
    op("dve", lambda e: e.tensor_copy(out=nhc_bf[:, 0:1], in_=nhc[:, 0:1]), reads=[Bconst], writes=[Bconst])
    op("act", lambda e: e.activation(out=kap[:, :], in_=lamcol[:, :], func=AF.Exp, scale=-1.0), reads=[Bconst], writes=[Bconst])
    op("act", lambda e: e.activation(out=kap[:, :], in_=kap[:, :], func=AF.Ln, bias=1.0, scale=1.0), reads=[Bconst], writes=[Bconst])
    op("dve", lambda e: e.tensor_scalar(out=kap2[:, :], in0=kap[:, :], scalar1=-16.0, scalar2=None, op0=ALU.mult), reads=[Bconst], writes=[Bconst])
    op("dve", lambda e: e.tensor_scalar(out=kap[:, :], in0=kap[:, :], scalar1=-8.0, scalar2=None, op0=ALU.mult), reads=[Bconst], writes=[Bconst])
    for pr in range(8):
        op("pe", lambda e, pr=pr: e.transpose(out=psbf(0)[:, pr * 128:(pr + 1) * 128],
                                              in_=wuk_nat[:, 2 * pr:2 * pr + 2, :].rearrange("c h d -> c (h d)"),
                                              identity=ident[:, :]), reads=[Bconst], writes=[Bps[0]])
    op("dve", lambda e: e.memset(wukTz[:, :, :], 0.0), reads=[Bconst], writes=[Bconst])
    for par in range(2):
        op("dve", lambda e, par=par: e.tensor_copy(
            out=wukTz[par * 64:par * 64 + 64, :, :].rearrange("p (pr two) c -> p pr two c", two=2)[:, :, par, :],
            in_=psbf(0)[par * 64:par * 64 + 64, :].rearrange("p (pr c) -> p pr c", c=128)), reads=[Bps[0], Bconst], writes=[Bconst])
    def sel(dst, ks):
        for n, k in enumerate(ks):
            tgt = dst if n == 0 else etmp
            op("dve", lambda e, k=k, tgt=tgt: e.tensor_scalar(out=tgt[:, :], in0=pidx[0:32, :], scalar1=float(k), scalar2=None,
                                                             op0=ALU.is_equal), reads=[Bconst], writes=[Bconst])
            if n > 0:
                op("dve", lambda e: e.tensor_tensor(out=dst[:, :], in0=dst[:, :], in1=etmp[:, :], op=ALU.add), reads=[Bconst], writes=[Bconst])
    sel(e0, [0]); sel(e12, [1, 2]); sel(e34, [3, 4]); sel(e13, [1, 3]); sel(e24, [2, 4])
    slopes = [2.0 ** (-8.0 * (h + 1) / 16.0) for h in range(16)]
    for h in range(16):
        hi, lo = _bf16_split(slopes[h])
        op("dve", lambda e, h=h, hi=hi: e.memset(Bcol_hi[:, h:h + 1], hi), reads=[Bconst], writes=[Bconst])
        op("dve", lambda e, h=h, lo=lo: e.memset(Bcol_lo[:, h:h + 1], lo), reads=[Bconst], writes=[Bconst])
    op("dve", lambda e: e.tensor_scalar(out=Bcol[:, :], in0=Bcol_hi[:, :], scalar1=e13[:, 0:1], scalar2=None, op0=ALU.mult), reads=[Bconst], writes=[Bconst])
    op("dve", lambda e: e.scalar_tensor_tensor(out=Bcol[:, :], in0=Bcol_lo[:, :], scalar=e24[:, 0:1], in1=Bcol[:, :],
                                               op0=ALU.mult, op1=ALU.add), reads=[Bconst], writes=[Bconst])
    for g in range(4):
        for hl in range(4):
            h = 4 * g + hl
            op("dve", lambda e, g=g, hl=hl, h=h: e.tensor_scalar(out=Bwork[g][:, hl * 128:(hl + 1) * 128], in0=ones_bf[0:32, :],
                                                                  scalar1=Bcol[:, h:h + 1], scalar2=None, op0=ALU.mult),
               reads=[Bconst], writes=[BBwork[g]])
    for dl in range(16):
        op("dve", lambda e, dl=dl: e.tensor_scalar(out=cold[:, dl:dl + 1], in0=e12[:, :], scalar1=128.0 * dl, scalar2=None,
                                                   op0=ALU.mult), reads=[Bconst], writes=[Bconst])
        op("dve", lambda e, dl=dl: e.tensor_tensor(out=cold[:, dl:dl + 1], in0=cold[:, dl:dl + 1], in1=e0[:, :], op=ALU.add), reads=[Bconst], writes=[Bconst])
        op("dve", lambda e, dl=dl: e.tensor_scalar(out=A_all[:, dl, :], in0=iota_row[0:32, :], scalar1=e34[:, 0:1], scalar2=cold[:, dl:dl + 1],
                                                   op0=ALU.mult, op1=ALU.add), reads=[Bconst], writes=[Bconst])

    for r4 in range(4):
        op("dve", lambda e, r4=r4: e.tensor_copy(out=identrep4[:, r4 * 128:(r4 + 1) * 128], in_=ident[:, :]), reads=[Bconst], writes=[Bconst])
        op("dve", lambda e, r4=r4: e.tensor_scalar(out=ntri_rep[:, r4 * 128:(r4 + 1) * 128], in0=triT[:, :], scalar1=-1.0, scalar2=32768.0,
                                                   op0=ALU.add, op1=ALU.mult), reads=[Bconst], writes=[Bconst])
    for h in range(16):
        op("dve", lambda e, h=h: e.tensor_scalar(out=SI[:, h // 4, (h % 4) * 128:(h % 4 + 1) * 128], in0=ident[:, :], scalar1=slopes[h], scalar2=None,
                                                 op0=ALU.mult), reads=[Bconst], writes=[Bconst])
    op("pool", lambda e: e.iota(iota1_full[:, :], [[1, 2048]], base=1, channel_multiplier=0,
                                allow_small_or_imprecise_dtypes=True), reads=[Bconst], writes=[Bconst])

    wslot = [0]

    def load_chunk(ci):
        j = wslot[0] % 4
        wslot[0] += 1
        wd = chunk_src[ci][2]
        if wd == 128:
            S_.dma("sp", "wl%d" % j, wch[j][:, :, :].rearrange("p k j -> p (k j)"), wbf[ci], reads=[Bwbf[ci]], writes=[Bwch[j]])
        else:
            S_.dma("sp", "wl%d" % j, wch[j][:, :, 0:wd], wbf[ci].rearrange("p (k j) -> p k j", k=8)[:, :, 0:wd],
                   reads=[Bwbf[ci]], writes=[Bwch[j]])
        return j

    def proj_fm(j, wd, bank, rhs_t, rhs_b):
        def f(e):
            ins = None
            for k in range(8):
                ins = e.matmul(ps[bank][0:wd, :], lhsT=wch[j][:, k, 0:wd], rhs=rhs_t[:, k, :], start=(k == 0), stop=(k == 7))
            return ins
        op("pe", f, reads=[Bwch[j], rhs_b], writes=[Bps[bank]])

    def rstd_from_ss(stt, Bstt, col_ss, col_out, n):
        op("act", lambda e: e.activation(out=stt[:, col_out:col_out + 1], in_=stt[:, col_ss:col_ss + 1], func=AF.Sqrt,
                                         bias=EPS, scale=1.0 / n), reads=[Bstt], writes=[Bstt])
        op("dve", lambda e: e.reciprocal(out=stt[:, col_out:col_out + 1], in_=stt[:, col_out:col_out + 1]), reads=[Bstt], writes=[Bstt])

    bankrr = [0]

    def next_bank():
        b = bankrr[0] % 2
        bankrr[0] += 1
        return b

    dbg = {}

    def chk(n):
        if stop is not None and stop == n:
            raise _Stop()

    try:
        chk(0)
        for sq in range(nseq):
            for tb in range(NBLK):
                t0 = tb * 512
                for i in range(4):
                    xb = i % 2
                    S_.dma("sp", "xl%d" % xb, xt[xb][:, :], x[sq, t0 + i * 128:t0 + (i + 1) * 128, :], writes=[Bxt[xb]])
                    op("act", lambda e, xb=xb: e.activation(out=junk[:, 0:1024], in_=xt[xb][:, :], func=AF.Square, accum_out=st[:, 0:1]),
                       reads=[Bxt[xb]], writes=[Bjunk_act, Bst])
                    rstd_from_ss(st, Bst, 0, 1, 1024.0)
                    op("dve", lambda e, xb=xb: e.tensor_scalar(out=xs[:, :], in0=xt[xb][:, :], scalar1=st[:, 1:2], scalar2=None, op0=ALU.mult),
                       reads=[Bxt[xb], Bst], writes=[Bxs])
                    def ftr(e):
                        ins = None
                        for k in range(8):
                            ins = e.transpose(out=psbf(2)[:, k * 128:(k + 1) * 128], in_=xs[:, k * 128:(k + 1) * 128], identity=ident[:, :])
                        return ins
                    op("pe", ftr, reads=[Bxs, Bident], writes=[Bps[2]])
                    for k in range(8):
                        op("dve", lambda e, k=k, i=i: e.tensor_scalar(out=xnT[:, k, i * 128:(i + 1) * 128], in0=psbf(2)[:, k * 128:(k + 1) * 128],
                                                                       scalar1=gcol[:, k:k + 1], scalar2=None, op0=ALU.mult),
                           reads=[Bps[2], Bconst], writes=[BxnT])

                chk(1)
                for c in range(8):
                    j = load_chunk(CH["q"][c]); bk = next_bank()
                    proj_fm(j, 128, bk, xnT, BxnT)
                    op("act", lambda e, c=c, bk=bk: e.copy(out=qT[:, c, :], in_=ps[bk][:, :]), reads=[Bps[bk]], writes=[BqT])
                for c in range(4):
                    j = load_chunk(CH["qidx"][c]); bk = next_bank()
                    proj_fm(j, 128, bk, xnT, BxnT)
                    op("act", lambda e, c=c, bk=bk: e.mul(out=qidxT[:, c, :], in_=ps[bk][:, :], mul=0.125),
                       reads=[Bps[bk]], writes=[BqidxT])
                j = load_chunk(CH["ckv"][0])
                for i in range(4):
                    gt = tb * 4 + i
                    def fc(e, i=i, j=j):
                        ins = None
                        for k in range(8):
                            ins = e.matmul(ps[2][:, 0:128], lhsT=xnT[:, k, i * 128:(i + 1) * 128], rhs=wch[j][:, k, :], start=(k == 0), stop=(k == 7))
                        return ins
                    op("pe", fc, reads=[Bwch[j], BxnT], writes=[Bps[2]])
                    op("act", lambda e: e.activation(out=junk[:, 0:128], in_=ps[2][:, 0:128], func=AF.Square, accum_out=st2[:, 0:1]),
                       reads=[Bps[2]], writes=[Bjunk_act, Bst2])
                    rstd_from_ss(st2, Bst2, 0, 1, 128.0)
                    op("dve", lambda e, gt=gt: e.scalar_tensor_tensor(out=c_tm[:, gt, :], in0=ps[2][:, 0:128], scalar=st2[:, 1:2], in1=gkv_bc[:, :],
                                                                       op0=ALU.mult, op1=ALU.mult), reads=[Bps[2], Bst2, Bconst], writes=[Bctm])
                    op("pe", lambda e, gt=gt: e.transpose(out=psbf(3)[:, 0:128], in_=c_tm[:, gt, :], identity=ident[:, :]),
                       reads=[Bctm, Bident], writes=[Bps[3]])
                    op("dve", lambda e, gt=gt: e.tensor_copy(out=cT[:, gt * 128:(gt + 1) * 128], in_=psbf(3)[:, 0:128]), reads=[Bps[3]], writes=[BcT])
                j = load_chunk(CH["kw"][0])
                for i in range(4):
                    gt = tb * 4 + i
                    def fk(e, i=i, j=j):
                        ins = None
                        for k in range(8):
                            ins = e.matmul(ps[2][:, 0:72], lhsT=xnT[:, k, i * 128:(i + 1) * 128], rhs=wch[j][:, k, 0:72], start=(k == 0), stop=(k == 7))
                        return ins
                    op("pe", fk, reads=[Bwch[j], BxnT], writes=[Bps[2]])
                    op("dve", lambda e, gt=gt: e.tensor_scalar(out=w_tm[:, gt, :], in0=ps[2][:, 64:72], scalar1=8.0 ** -0.5, scalar2=None, op0=ALU.mult),
                       reads=[Bps[2]], writes=[Bwtm])
                    op("dve", lambda e: e.tensor_reduce(out=st2[:, 2:3], in_=ps[2][:, 0:64], axis=AX.X, op=ALU.add), reads=[Bps[2]], writes=[Bst2])
                    op("dve", lambda e: e.tensor_scalar(out=st2[:, 2:3], in0=st2[:, 2:3], scalar1=-1.0 / 64.0, scalar2=None, op0=ALU.mult), reads=[Bst2], writes=[Bst2])
                    op("dve", lambda e: e.tensor_scalar(out=kc[:, :], in0=ps[2][:, 0:64], scalar1=st2[:, 2:3], scalar2=None, op0=ALU.add),
                       reads=[Bps[2], Bst2], writes=[Bkc])
                    op("act", lambda e: e.activation(out=junk[:, 0:64], in_=kc[:, :], func=AF.Square, accum_out=st2[:, 3:4]),
                       reads=[Bkc], writes=[Bjunk_act, Bst2])
                    rstd_from_ss(st2, Bst2, 3, 4, 64.0)
                    op("dve", lambda e: e.scalar_tensor_tensor(out=kc[:, :], in0=kc[:, :], scalar=st2[:, 4:5], in1=lng_bc[:, :], op0=ALU.mult, op1=ALU.mult),
                       reads=[Bkc, Bst2, Bconst], writes=[Bkc])
                    op("dve", lambda e: e.tensor_tensor(out=kn2[:, 0:64], in0=kc[:, :], in1=lnb_bc[:, :], op=ALU.add), reads=[Bkc, Bconst], writes=[Bkn2])
                    op("dve", lambda e: e.tensor_copy(out=kn2[:, 64:128], in_=kn2[:, 0:64]), reads=[Bkn2], writes=[Bkn2])
                    op("pe", lambda e: e.transpose(out=psbf(3)[:, 0:128], in_=kn2[:, :], identity=ident[:, :]), reads=[Bkn2, Bident], writes=[Bps[3]])
                    for par in range(2):
                        op("dve", lambda e, gt=gt, par=par: e.tensor_copy(out=kz[par][par * 64:par * 64 + 64, gt * 128:(gt + 1) * 128],
                                                                           in_=psbf(3)[par * 64:par * 64 + 64, 0:128]), reads=[Bps[3]], writes=[BkT])
                for c in range(8):
                    j = load_chunk(CH["gattn"][c]); bk = next_bank()
                    proj_fm(j, 128, bk, xnT, BxnT)
                    op("act", lambda e, c=c, bk=bk: e.activation(out=sgT[:, c, :], in_=ps[bk][:, :], func=AF.Silu), reads=[Bps[bk]], writes=[BsgT[c]])

                chk(2)
                def rnn_steps(c, k):
                    xc, Bxc, xcb, Bxcb = xc_s[k], Bxc_s[k], xcb_s[k], Bxcb_s[k]
                    rr, Brr, ii, Bii, aa, Baa, ss, Bss = r_s[k], Br_s[k], i_s[k], Bi_s[k], a_s[k], Ba_s[k], s_s[k], Bs_s[k]
                    sg, Bsg, uu, Buu = sgr_s[k], Bsgr_s[k], ub[k], Bub[k]
                    j = load_chunk(CH["xrnn"][c]); bk = next_bank()
                    proj_fm(j, 128, bk, xnT, BxnT)
                    if tb == 0:
                        op("pool", lambda e: e.memset(uu[:, 0:3], 0.0), reads=[], writes=[Buu])
                        op("dve", lambda e: e.memset(hcar[:, c:c + 1], 0.0), reads=[], writes=[Bhcar[c]])
                    else:
                        op("pool", lambda e: e.tensor_copy(out=uu[:, 0:3], in_=ucar[:, c, :]), reads=[Bucar[c]], writes=[Buu])
                    op("act", lambda e: e.copy(out=uu[:, 3:515], in_=ps[bk][:, :]), reads=[Bps[bk]], writes=[Buu])
                    yield
                    op("dve", lambda e: e.tensor_scalar(out=xc[:, :], in0=uu[:, 3:515], scalar1=cwcol[:, 3, c:c + 1], scalar2=cbcol[:, c:c + 1],
                                                        op0=ALU.mult, op1=ALU.add), reads=[Buu, Bconst], writes=[Bxc])
                    for kk in range(3):
                        op("dve", lambda e, kk=kk: e.scalar_tensor_tensor(out=xc[:, :], in0=uu[:, kk:kk + 512], scalar=cwcol[:, kk, c:c + 1],
                                                                          in1=xc[:, :], op0=ALU.mult, op1=ALU.add),
                           reads=[Buu, Bconst, Bxc], writes=[Bxc])
                    op("pool", lambda e: e.tensor_copy(out=ucar[:, c, :], in_=uu[:, 512:515]), reads=[Buu], writes=[Bucar[c]])
                    op("pool", lambda e: e.tensor_copy(out=xcb[:, :], in_=xc[:, :]), reads=[Bxc], writes=[Bxcb])
                    yield
                    bk1 = next_bank()
                    op("pe", lambda e: e.matmul(ps[bk1][:, :], lhsT=BDa[:, c, :], rhs=xcb[:, :], start=True, stop=True),
                       reads=[Bxcb, Bconst], writes=[Bps[bk1]])
                    op("act", lambda e: e.activation(out=rr[:, :], in_=ps[bk1][:, :], func=AF.Sigmoid, bias=bacol[:, c:c + 1], scale=1.0),
                       reads=[Bps[bk1], Bconst], writes=[Brr])
                    bk2 = next_bank()
                    op("pe", lambda e: e.matmul(ps[bk2][:, :], lhsT=BDx[:, c, :], rhs=xcb[:, :], start=True, stop=True),
                       reads=[Bxcb, Bconst], writes=[Bps[bk2]])
                    op("act", lambda e: e.activation(out=ii[:, :], in_=ps[bk2][:, :], func=AF.Sigmoid, bias=bxcol[:, c:c + 1], scale=1.0),
                       reads=[Bps[bk2], Bconst], writes=[Bii])
                    yield
                    op("act", lambda e: e.activation(out=aa[:, :], in_=rr[:, :], func=AF.Exp, scale=kap[:, c:c + 1]), reads=[Brr, Bconst], writes=[Baa])
                    op("act", lambda e: e.activation(out=ss[:, :], in_=rr[:, :], func=AF.Exp, scale=kap2[:, c:c + 1]), reads=[Brr, Bconst], writes=[Bss])
                    yield
                    op("act", lambda e: e.activation(out=ss[:, :], in_=ss[:, :], func=AF.Sqrt, bias=1.0, scale=-1.0), reads=[Bss], writes=[Bss])
                    op("dve", lambda e: e.tensor_tensor(out=ii[:, :], in0=ii[:, :], in1=xc[:, :], op=ALU.mult), reads=[Bii, Bxc], writes=[Bii])
                    yield
                    op("dve", lambda e: e.tensor_tensor(out=ii[:, :], in0=ii[:, :], in1=ss[:, :], op=ALU.mult), reads=[Bii, Bss], writes=[Bii])
                    op("dve", lambda e: e.tensor_tensor_scan(out=rr[:, :], data0=aa[:, :], data1=ii[:, :], initial=hcar[:, c:c + 1],
                                                             op0=ALU.mult, op1=ALU.add), reads=[Baa, Bii, Bhcar[c]], writes=[Brr])
                    op("dve", lambda e: e.tensor_copy(out=hcar[:, c:c + 1], in_=rr[:, 511:512]), reads=[Brr], writes=[Bhcar[c]])
                    j2 = load_chunk(CH["grnn"][c]); bk3 = next_bank()
                    proj_fm(j2, 128, bk3, xnT, BxnT)
                    yield
                    op("act", lambda e: e.activation(out=sg[:, :], in_=ps[bk3][:, :], func=AF.Silu), reads=[Bps[bk3]], writes=[Bsg])
                    op("dve", lambda e: e.tensor_tensor(out=hgT[:, c, :], in0=rr[:, :], in1=sg[:, :], op=ALU.mult), reads=[Brr, Bsg], writes=[BhgT])

                for c in range(0, 8, 2):
                    g0, g1 = rnn_steps(c, 0), rnn_steps(c + 1, 1)
                    alive = [g0, g1]
                    while alive:
                        for g in list(alive):
                            try:
                                next(g)
                            except StopIteration:
                                alive.remove(g)

                chk(3)
                def topk_gen(i, gt):
                    ns = gt + 1
                    ncols = ns * 128
                    bis, Bbis, nsm, Bnsm = bis_s[i % 2], Bbis_s[i % 2], nsm_s[i % 2], Bnsm_s[i % 2]
                    nsb = (ncols + 511) // 512
                    for sbk in range(nsb):
                        c0 = sbk * 512
                        cw = min(512, ncols - c0)
                        for jh in range(8):
                            par = jh % 2
                            bk = 7
                            op("pe", lambda e, jh=jh, par=par, bk=bk, c0=c0, cw=cw: e.matmul(
                                ps[bk][:, 0:cw], lhsT=qidxT[:, jh // 2, i * 128:(i + 1) * 128],
                                rhs=kz[par][:, c0:c0 + cw], start=True, stop=True),
                               reads=[BqidxT, BkT], writes=[Bps[bk]])
                            rb = jh % 2
                            op("act", lambda e, bk=bk, rb=rb, cw=cw: e.activation(out=rj[rb][:, 0:cw], in_=ps[bk][:, 0:cw], func=AF.Relu),
                               reads=[Bps[bk]], writes=[Brj[rb]])
                            if jh == 0:
                                op("dve", lambda e, rb=rb, c0=c0, cw=cw: e.tensor_scalar(
                                    out=I_t[:, c0:c0 + cw], in0=rj[rb][:, 0:cw], scalar1=w_tm[:, gt, 0:1], scalar2=None, op0=ALU.mult),
                                   reads=[Brj[rb], Bwtm], writes=[BI, Bres[0], Bres[1]])
                            else:
                                op("dve", lambda e, rb=rb, c0=c0, cw=cw, jh=jh: e.scalar_tensor_tensor(
                                    out=I_t[:, c0:c0 + cw], in0=rj[rb][:, 0:cw], scalar=w_tm[:, gt, jh:jh + 1], in1=I_t[:, c0:c0 + cw],
                                    op0=ALU.mult, op1=ALU.add), reads=[Brj[rb], Bwtm, BI], writes=[BI])
                    yield
                    op("dve", lambda e: e.tensor_reduce(out=bis[:, 0:1], in_=I_t[:, 0:ncols], axis=AX.X, op=ALU.max,
                                                        apply_absolute_value=True), reads=[BI], writes=[Bbis])
                    op("dve", lambda e: e.tensor_scalar(out=bis[:, 0:1], in0=bis[:, 0:1], scalar1=1.01, scalar2=1e-6, op0=ALU.mult, op1=ALU.add),
                       reads=[Bbis], writes=[Bbis])
                    op("dve", lambda e: e.tensor_scalar(out=steps[:, :], in0=pow2[:, :], scalar1=bis[:, 0:1], scalar2=None, op0=ALU.mult),
                       reads=[Bbis, Bconst], writes=[Bsteps])
                    op("dve", lambda e: e.tensor_scalar(out=steps2[:, :], in0=steps[:, :], scalar1=2.0, scalar2=None, op0=ALU.mult),
                       reads=[Bsteps], writes=[Bsteps])
                    op("dve", lambda e: e.tensor_tensor(out=I_t[:, gt * 128:(gt + 1) * 128], in0=I_t[:, gt * 128:(gt + 1) * 128],
                                                        in1=causal_neg[:, :], op=ALU.add), reads=[BI, Bconst, Bbis], writes=[BI])
                    op("dve", lambda e: e.memset(bis[:, 1:2], 0.0), reads=[Bbis], writes=[Bbis])
                    for kb in range(NBIS):
                        op("dve", lambda e: e.tensor_scalar(out=mask[:, 0:ncols], in0=I_t[:, 0:ncols], scalar1=bis[:, 1:2], scalar2=None,
                                                            op0=ALU.is_ge, op1=ALU.add, accum_out=bis[:, 2:3]),
                           reads=[BI, Bbis], writes=[Bmask, Bbis])
                        op("dve", lambda e, kb=kb: e.tensor_scalar(out=bis[:, 3:4], in0=bis[:, 2:3], scalar1=TOPK - 0.5, scalar2=steps2[:, kb:kb + 1],
                                                                   op0=ALU.is_ge, op1=ALU.mult), reads=[Bbis, Bsteps], writes=[Bbis])
                        op("dve", lambda e, kb=kb: e.scalar_tensor_tensor(out=bis[:, 1:2], in0=bis[:, 1:2], scalar=steps[:, kb:kb + 1], in1=bis[:, 3:4],
                                                                          op0=ALU.subtract, op1=ALU.add), reads=[Bbis, Bsteps], writes=[Bbis])
                        if kb in (3, 7):
                            yield
                    op("dve", lambda e: e.tensor_scalar(out=mask[:, 0:ncols], in0=I_t[:, 0:ncols], scalar1=bis[:, 1:2], scalar2=None,
                                                        op0=ALU.is_ge), reads=[BI, Bbis], writes=[Bmask])
                    op("dve", lambda e: e.scalar_tensor_tensor(out=I_t[:, 0:ncols], in0=I_t[:, 0:ncols], scalar=bis[:, 1:2],
                                                               in1=iota1_full[:, 0:ncols], op0=ALU.is_ge, op1=ALU.mult),
                       reads=[BI, Bbis, Bconst], writes=[BI])
                    op("dve", lambda e: e.tensor_reduce(out=bis[:, 4:5], in_=I_t[:, 0:ncols], axis=AX.X, op=ALU.max), reads=[BI], writes=[Bbis])
                    op("dve", lambda e: e.tensor_scalar(out=nsm[:, :], in0=bis[:, 4:5], scalar1=-1.0, scalar2=1.0, op0=ALU.mult, op1=ALU.add),
                       reads=[Bbis], writes=[Bnsm])

                def nmask_build(i, gt):
                    for jj in range(gt + 1):
                        mbk = 6 + (jj % 2)
                        op("pe", lambda e, jj=jj, mbk=mbk: e.matmul(ps[mbk][:, :], lhsT=mask[:, jj * 128:(jj + 1) * 128], rhs=identrep4[:, :],
                                                                     start=True, stop=True), reads=[Bmask, Bconst], writes=[Bps[mbk]])
                        op("dve", lambda e, jj=jj, mbk=mbk: e.tensor_scalar(out=nmask[:, jj, :], in0=ps[mbk][:, :], scalar1=-1.0, scalar2=32768.0,
                                                                             op0=ALU.add, op1=ALU.mult), reads=[Bps[mbk]], writes=[Bnmask[jj]])

                def attn_prologue(i, gt):
                    nsm, Bnsm = nsm_s[i % 2], Bnsm_s[i % 2]
                    if gt < 2:
                        op("dve", lambda e: e.tensor_scalar(out=nsm[:, :], in0=pidx[:, :], scalar1=-1.0, scalar2=-128.0 * gt, op0=ALU.mult, op1=ALU.add),
                           reads=[Bconst], writes=[Bnsm])
                    for hg in range(4):
                        def fql(e, hg=hg):
                            ins = None
                            for hl in range(4):
                                h = 4 * hg + hl
                                ins = e.matmul(ps[6][:, hl * 128:(hl + 1) * 128], lhsT=wukTz[:, h, :],
                                               rhs=qT[:, h // 2, i * 128:(i + 1) * 128], start=True, stop=True)
                            return ins
                        op("pe", fql, reads=[BqT, Bconst], writes=[Bps[6]])
                        op("act", lambda e, hg=hg: e.mul(out=qlb_s[hg][:, :], in_=ps[6][:, :], mul=0.125), reads=[Bps[6]], writes=[Bqlb_s[hg]])
                        op("act", lambda e: e.activation(out=absq[:, :], in_=ps[6][:, :], func=AF.Square, scale=0.125), reads=[Bps[6]], writes=[Babsq])
                        def fnm(e, hg=hg):
                            e.matmul(ps[4][0:1, :], lhsT=nhc_bf[:, 0:1], rhs=absq[:, :], start=True, stop=False)
                            return e.matmul(ps[4][0:1, :], lhsT=nsm[:, 0:1], rhs=SI[:, hg, :], start=False, stop=True)
                        op("pe", fnm, reads=[Babsq, Bnsm, Bconst], writes=[Bps[4]])
                        op("act", lambda e, hg=hg: e.activation(out=Bwork[hg][0:1, :], in_=ps[4][0:1, :], func=AF.Identity, bias=nhc[0:1, 0:1], scale=1.0),
                           reads=[Bps[4], Bconst], writes=[BBwork[hg]])

                def attn_hg(i, gt, hg):
                    ns = gt + 1
                    qlb, Bqlb = qlb_s[hg], Bqlb_s[hg]
                    def flg_exp(js):
                        lb = js % 2
                        if gt >= 2:
                            mrhs, mbuf = nmask[:, js, :], Bnmask[js]
                        elif js == gt:
                            mrhs, mbuf = ntri_rep[:, :], Bconst
                        else:
                            mrhs, mbuf = None, None
                        def flg(e):
                            e.matmul(ps[lb][:, :], lhsT=cT[:, js * 128:(js + 1) * 128], rhs=qlb[:, :], start=True, stop=False)
                            if mrhs is None:
                                return e.matmul(ps[lb][:, :], lhsT=A_all[:, js, :], rhs=Bwork[hg][:, :], start=False, stop=True)
                            e.matmul(ps[lb][:, :], lhsT=A_all[:, js, :], rhs=Bwork[hg][:, :], start=False, stop=False)
                            return e.matmul(ps[lb][:, :], lhsT=ident[:, :], rhs=mrhs, start=False, stop=True)
                        op("pe", flg, reads=[BcT, Bqlb, BBwork[hg], Bconst] + ([mbuf] if mbuf is not None else []), writes=[Bps[lb]])
                        op("act", lambda e: e.activation(out=p_t[lb][:, :], in_=ps[lb][:, :], func=AF.Exp), reads=[Bps[lb]], writes=[Bp[lb]])
                    bo, bs_ = (2, 3) if hg % 2 == 0 else (4, 5)
                    def fpv_(js):
                        lb = js % 2
                        def fpv(e):
                            e.matmul(ps[bo][:, :], lhsT=c_tm[:, js, :], rhs=p_t[lb][:, :], start=(js == 0), stop=(js == ns - 1))
                            return e.matmul(ps[bs_][:, :], lhsT=ones_bf[:, :], rhs=p_t[lb][:, :], start=(js == 0), stop=(js == ns - 1))
                        op("pe", fpv, reads=[Bctm, Bp[lb], Bconst], writes=[Bps[bo], Bps[bs_]])
                    flg_exp(0)
                    for js in range(ns):
                        if js + 1 < ns:
                            flg_exp(js + 1)
                        fpv_(js)
                    op("dve", lambda e: e.reciprocal(out=rs_t[:, :], in_=ps[bs_][:, :]), reads=[Bps[bs_]], writes=[Brs])
                    op("dve", lambda e: e.tensor_tensor(out=oTn[:, :], in0=ps[bo][:, :], in1=rs_t[:, :], op=ALU.mult), reads=[Bps[bo], Brs], writes=[BoTn])
                    def fy(e):
                        ins = None
                        for pp in range(2):
                            for q2 in range(2):
                                hl = 2 * pp + q2
                                h = 4 * hg + hl
                                ins = e.matmul(ps[6][:, pp * 128:(pp + 1) * 128], lhsT=wuvP[:, h, :], rhs=oTn[:, hl * 128:(hl + 1) * 128],
                                               start=(q2 == 0), stop=(q2 == 1))
                        return ins
                    op("pe", fy, reads=[BoTn, Bconst], writes=[Bps[6]])
                    for pp in range(2):
                        cc = 2 * hg + pp
                        op("dve", lambda e, pp=pp, cc=cc: e.tensor_tensor(out=sgT[:, cc, i * 128:(i + 1) * 128], in0=ps[6][:, pp * 128:(pp + 1) * 128],
                                                                           in1=sgT[:, cc, i * 128:(i + 1) * 128], op=ALU.mult),
                           reads=[Bps[6], BsgT[cc]], writes=[BsgT[cc]])

                gens = {}
                for i in range(4):
                    if tb * 4 + i >= 2:
                        gens[i] = topk_gen(i, tb * 4 + i)
                if 0 in gens:
                    for _ in gens[0]:
                        pass
                    nmask_build(0, tb * 4)
                for i in range(4):
                    gt = tb * 4 + i
                    attn_prologue(i, gt)
                    g = gens.get(i + 1)
                    for hg in range(4):
                        attn_hg(i, gt, hg)
                        if g is not None:
                            next(g, None)
                    if g is not None:
                        for _ in g:
                            pass
                        nmask_build(i + 1, gt + 1)

                chk(4)
                mixedT, BmixedT = qT, BqT
                for f in range(8):
                    ja = load_chunk(CH["wap"][f])
                    def fya(e, ja=ja):
                        ins = None
                        for k in range(8):
                            ins = e.matmul(ps[0][:, :], lhsT=wch[ja][:, k, :], rhs=sgT[:, k, :], start=(k == 0), stop=(k == 7))
                        return ins
                    op("pe", fya, reads=[Bwch[ja]] + BsgT, writes=[Bps[0]])
                    jr = load_chunk(CH["wrp"][f])
                    proj_fm(jr, 128, 1, hgT, BhgT)
                    jg = load_chunk(CH["mga"][f])
                    proj_fm(jg, 128, 2, xnT, BxnT)
                    op("act", lambda e, f=f: e.activation(out=ga_t[:, :], in_=ps[2][:, :], func=AF.Sigmoid, bias=bmcol[:, f:f + 1], scale=1.0),
                       reads=[Bps[2], Bconst], writes=[Bga])
                    jg2 = load_chunk(CH["mgr"][f])
                    proj_fm(jg2, 128, 3, xnT, BxnT)
                    op("act", lambda e, f=f: e.activation(out=gr_t[:, :], in_=ps[3][:, :], func=AF.Sigmoid, bias=bmcol[:, 8 + f:9 + f], scale=1.0),
                       reads=[Bps[3], Bconst], writes=[Bgr])
                    op("dve", lambda e: e.tensor_tensor(out=m1_t[:, :], in0=ps[0][:, :], in1=ga_t[:, :], op=ALU.mult), reads=[Bps[0], Bga], writes=[Bm1])
                    op("dve", lambda e: e.tensor_tensor(out=m2_t[:, :], in0=ps[1][:, :], in1=gr_t[:, :], op=ALU.mult), reads=[Bps[1], Bgr], writes=[Bm2])
                    op("pool", lambda e, f=f: e.tensor_tensor(out=mixedT[:, f, :], in0=m1_t[:, :], in1=m2_t[:, :], op=ALU.add), reads=[Bm1, Bm2], writes=[BmixedT])
                for i in range(4):
                    xb = i % 2
                    rk = i % 2
                    res_t = I_t[:, rk * 1024:(rk + 1) * 1024]
                    S_.dma("sp", "xl%d" % xb, xt[xb][:, :], x[sq, t0 + i * 128:t0 + (i + 1) * 128, :], writes=[Bxt[xb]])
                    for db in range(2):
                        def fo(e, i=i, db=db):
                            ins = None
                            for k in range(8):
                                ins = e.matmul(ps[4 + db][:, :], lhsT=mixedT[:, k, i * 128:(i + 1) * 128], rhs=wout_sb[:, k, db * 512:(db + 1) * 512],
                                               start=(k == 0), stop=(k == 7))
                            return ins
                        op("pe", fo, reads=[BmixedT, Bconst], writes=[Bps[4 + db]])
                        op("dve", lambda e, db=db, xb=xb, res_t=res_t: e.tensor_tensor(out=res_t[:, db * 512:(db + 1) * 512], in0=ps[4 + db][:, :],
                                                                                       in1=xt[xb][:, db * 512:(db + 1) * 512], op=ALU.add),
                           reads=[Bps[4 + db], Bxt[xb]], writes=[Bres[rk], BI])
                    op("act", lambda e, res_t=res_t: e.activation(out=junk[:, 0:1024], in_=res_t, func=AF.Square, accum_out=st[:, 2:3]),
                       reads=[Bres[rk]], writes=[Bjunk_act, Bst])
                    rstd_from_ss(st, Bst, 2, 3, 1024.0)
                    op("dve", lambda e, res_t=res_t: e.scalar_tensor_tensor(out=res_t, in0=res_t, scalar=st[:, 3:4], in1=fg_bc[:, :],
                                                                            op0=ALU.mult, op1=ALU.mult), reads=[Bres[rk], Bst, Bconst], writes=[Bres[rk]])
                    S_.dma("pool", "os%d" % rk, out[sq, t0 + i * 128:t0 + (i + 1) * 128, :], res_t, reads=[Bres[rk]])
    except _Stop:
        pass
    for key in sorted(S_.dsem):
        nc.gpsimd.wait_ge(S_.dsem[key][0], S_.dsem[key][1])
    for k_ in S_.sem:
        if S_.cnt[k_] > 0 and k_ != "pool":
            nc.gpsimd.wait_ge(S_.sem[k_], S_.cnt[k_])
    return nc


_PARAMS = ["norm_gain", "w_in", "b_merge", "kv_norm_gain", "w_uk", "w_uv", "idx_ln_gain", "idx_ln_bias", "w_attn_proj",
           "conv_w", "conv_b", "w_rg_a", "b_rg_a", "w_rg_x", "b_rg_x", "lru_lambda", "w_rnn_proj", "w_out"]


def kernel(**inputs):
    x = np.ascontiguousarray(np.asarray(inputs["x"], dtype=np.float32))
    B, S, _ = x.shape
    nseq = B // NCORES
    base = {}
    for k in _PARAMS:
        a = np.asarray(inputs[k], dtype=np.float32)
        base[k] = np.ascontiguousarray(a.reshape(a.shape[1:]))
    base["final_norm_gain"] = np.ascontiguousarray(np.asarray(inputs["final_norm_gain"], dtype=np.float32))
    nc = build(nseq, S)
    in_maps = []
    for c in range(NCORES):
        m = dict(base)
        m["x"] = np.ascontiguousarray(x[c * nseq:(c + 1) * nseq])
        in_maps.append(m)
    res = run_bass_kernel_spmd(nc, in_maps, core_ids=list(range(NCORES)))
    return np.concatenate([np.asarray(r["out"], dtype=np.float32) for r in res.results], axis=0)
```

```python
import numpy as np
import concourse.bass as bass
import concourse.mybir as mybir
from concourse.bass_utils import run_bass_kernel_spmd

F32 = mybir.dt.float32
BF16 = mybir.dt.bfloat16
ALU = mybir.AluOpType
AF = mybir.ActivationFunctionType
AX = mybir.AxisListType

D = 1024
NCORES = 8
EPS = 1e-6
TOPK = 256
NBIS = 12


class Buf:
    __slots__ = ("name", "w", "r")

    def __init__(self, name):
        self.name = name
        self.w = None
        self.r = []


class Sched:
    def __init__(self, nc):
        self.nc = nc
        self.eng = {"pe": nc.tensor, "act": nc.scalar, "dve": nc.vector, "pool": nc.gpsimd, "sp": nc.sync}
        self.sem, self.cnt, self.waited, self.dsem = {}, {}, {}, {}
        for k in self.eng:
            self.sem[k] = nc.semaphore("s_" + k).__enter__()
            self.cnt[k] = 0
            self.waited[k] = {}

    def _wait(self, e, dep):
        de, val = dep
        if de == e and e == "pe":
            return
        if self.waited[e].get(de, 0) >= val:
            return
        self.waited[e][de] = val
        s = self.dsem[de][0] if de.startswith("dma:") else self.sem[de]
        self.eng[e].wait_ge(s, val)

    def _deps(self, e, reads, writes):
        deps = set()
        for b in reads:
            if b.w is not None:
                deps.add(b.w)
        for b in writes:
            if b.w is not None:
                deps.add(b.w)
            deps.update(b.r)
        for d in sorted(deps):
            self._wait(e, d)

    def _mark(self, me, reads, writes):
        for b in reads:
            if len(b.r) > 24:
                last = {}
                for (q, v) in b.r:
                    last[q] = max(last.get(q, 0), v)
                b.r = list(last.items())
            b.r.append(me)
        for b in writes:
            b.w = me
            b.r = []

    def op(self, e, fn, reads=(), writes=()):
        self._deps(e, reads, writes)
        ins = fn(self.eng[e])
        self.cnt[e] += 1
        ins.then_inc(self.sem[e], 1)
        me = (e, self.cnt[e])
        self._mark(me, reads, writes)
        return me

    def dma(self, e, q, out, in_, reads=(), writes=()):
        key = "dma:" + q
        if key not in self.dsem:
            self.dsem[key] = [self.nc.semaphore("d_" + q).__enter__(), 0]
        self._deps(e, reads, writes)
        ins = self.eng[e].dma_start(out=out, in_=in_)
        self.dsem[key][1] += 16
        ins.then_inc(self.dsem[key][0], 16)
        me = (key, self.dsem[key][1])
        self._mark(me, reads, writes)
        return me


def _bf16_split(v):
    import ml_dtypes
    hi = float(np.float32(v).astype(ml_dtypes.bfloat16))
    lo = float(np.float32(v - hi).astype(ml_dtypes.bfloat16))
    return hi, lo


class _Stop(Exception):
    pass


def build(nseq, S, debug=False, stop=None):
    NBLK = S // 512
    NTS = S // 128
    nc = bass.Bass("TRN2", target_bir_lowering=False)
    S_ = Sched(nc)

    def din(name, shape):
        return nc.dram_tensor(name, list(shape), F32, kind="ExternalInput").ap()

    x = din("x", [nseq, S, D])
    norm_gain = din("norm_gain", [D])
    w_in = din("w_in", [D, 6856])
    b_merge = din("b_merge", [2048])
    kv_norm_gain = din("kv_norm_gain", [128])
    w_uk = din("w_uk", [16, 128, 64])
    w_uv = din("w_uv", [16, 128, 64])
    idx_ln_gain = din("idx_ln_gain", [64])
    idx_ln_bias = din("idx_ln_bias", [64])
    w_attn_proj = din("w_attn_proj", [D, D])
    conv_w = din("conv_w", [4, D])
    conv_b = din("conv_b", [D])
    w_rg_a = din("w_rg_a", [16, 64, 64])
    b_rg_a = din("b_rg_a", [D])
    w_rg_x = din("w_rg_x", [16, 64, 64])
    b_rg_x = din("b_rg_x", [D])
    lru_lambda = din("lru_lambda", [D])
    w_rnn_proj = din("w_rnn_proj", [D, D])
    w_out = din("w_out", [D, D])
    final_norm_gain = din("final_norm_gain", [D])
    out = nc.dram_tensor("out", [nseq, S, D], F32, kind="ExternalOutput").ap()
    wbf = nc.dram_tensor("wbf", [70, 128, 1024], BF16, kind="Internal").ap()

    def sb(name, shape, dt=F32):
        return nc.sbuf_tensor(name, list(shape), dt).__enter__()

    CH = {}
    chunk_src = []

    def add_chunks(name, src, col0, n, width=128):
        CH[name] = []
        for c in range(n):
            CH[name].append(len(chunk_src))
            chunk_src.append((src, col0 + c * 128, width))

    add_chunks("q", w_in, 0, 8)
    add_chunks("ckv", w_in, 1024, 1)
    add_chunks("qidx", w_in, 1152, 4)
    add_chunks("kw", w_in, 1664, 1, 72)
    add_chunks("gattn", w_in, 1736, 8)
    add_chunks("xrnn", w_in, 2760, 8)
    add_chunks("grnn", w_in, 3784, 8)
    add_chunks("mga", w_in, 4808, 8)
    add_chunks("mgr", w_in, 5832, 8)
    add_chunks("wap", w_attn_proj, 0, 8)
    add_chunks("wrp", w_rnn_proj, 0, 8)
    assert len(chunk_src) == 70
    Bwbf = [Buf("wbf%d" % i) for i in range(70)]

    ident = sb("ident", [128, 128], BF16); Bident = Buf("ident")
    ones_bf = sb("ones_bf", [128, 128], BF16)
    triT = sb("triT", [128, 128], BF16)
    causal_neg = sb("causal_neg", [128, 128])
    iota_row = sb("iota_row", [128, 128])
    pidx = sb("pidx", [128, 1])
    Bconst = Buf("const")

    wout_sb = sb("wout_sb", [128, 8, 1024], BF16)
    wuvP = sb("wuvP", [128, 16, 128], BF16)
    wuk_nat = sb("wuk_nat", [128, 16, 64], BF16)
    wukTz = sb("wukTz", [128, 16, 128], BF16)
    BDa = sb("BDa", [128, 8, 128], BF16)
    BDx = sb("BDx", [128, 8, 128], BF16)
    gcol = sb("gcol", [128, 8])
    bmcol = sb("bmcol", [128, 16])
    cwcol = sb("cwcol", [128, 4, 8])
    cbcol = sb("cbcol", [128, 8])
    bacol = sb("bacol", [128, 8])
    bxcol = sb("bxcol", [128, 8])
    lamcol = sb("lamcol", [128, 8])
    kap = sb("kap", [128, 8])
    kap2 = sb("kap2", [128, 8])
    fg_bc = sb("fg_bc", [128, 1024])
    gkv_bc = sb("gkv_bc", [128, 128])
    lng_bc = sb("lng_bc", [128, 64])
    lnb_bc = sb("lnb_bc", [128, 64])
    A_all = sb("A_all", [32, 16, 128], BF16)
    Bcol_hi = sb("Bcol_hi", [32, 16])
    Bcol_lo = sb("Bcol_lo", [32, 16])
    Bcol = sb("Bcol", [32, 16])
    e0 = sb("e0", [32, 1]); e12 = sb("e12", [32, 1]); e34 = sb("e34", [32, 1])
    e13 = sb("e13", [32, 1]); e24 = sb("e24", [32, 1]); etmp = sb("etmp", [32, 1])
    cold = sb("cold", [32, 16])
    Bwork = [sb("Bwork%d" % g, [32, 512], BF16) for g in range(4)]
    BBwork = [Buf("Bwork%d" % g) for g in range(4)]
    pow2 = sb("pow2", [128, NBIS])
    nhc = sb("nhc", [128, 1]); nhc_bf = sb("nhc_bf", [128, 1], BF16)

    cT = sb("cT", [128, S], BF16); BcT = Buf("cT")
    c_tm = sb("c_tm", [128, NTS, 128], BF16); Bctm = Buf("c_tm")
    kz = [sb("kz%d" % i, [128, S], BF16) for i in range(2)]; BkT = Buf("kidxT")
    w_tm = sb("w_tm", [128, NTS, 8]); Bwtm = Buf("w_tm")
    cabs = sb("cabs", [128, 1]); cabs_b = sb("cabs_b", [128, 1]); ncabs = sb("ncabs", [128, 1], BF16)
    Bcabs = Buf("cabs")
    xnT = sb("xnT", [128, 8, 512], BF16); BxnT = Buf("xnT")
    qT = sb("qT", [128, 8, 512], BF16); BqT = Buf("qT")
    sgT = sb("sgT", [128, 8, 512], BF16); BsgT = [Buf("sgT%d" % c) for c in range(8)]
    hgT = sb("hgT", [128, 8, 512], BF16); BhgT = Buf("hgT")
    qidxT = sb("qidxT", [128, 4, 512], BF16); BqidxT = Buf("qidxT")
    wch = [sb("wch%d" % i, [128, 8, 128], BF16) for i in range(4)]
    Bwch = [Buf("wch%d" % i) for i in range(4)]
    xt = [sb("xt%d" % i, [128, 1024]) for i in range(2)]
    Bxt = [Buf("xt%d" % i) for i in range(2)]
    xs = sb("xs", [128, 1024], BF16); Bxs = Buf("xs")
    junk = sb("junk", [128, 2048], BF16); Bjunk_act = Buf("junk_act"); Bjunk_dve = Buf("junk_dve")
    st = sb("st", [128, 16]); Bst = Buf("st")
    st2 = sb("st2", [128, 16]); Bst2 = Buf("st2")
    kc = sb("kc", [128, 64]); Bkc = Buf("kc")
    kn2 = sb("kn2", [128, 128], BF16); Bkn2 = Buf("kn2")
    ub = [sb("ub%d" % k, [128, 515], BF16) for k in range(2)]; Bub = [Buf("ub%d" % k) for k in range(2)]
    ucar = sb("ucar", [128, 8, 3], BF16); Bucar = [Buf("ucar%d" % c) for c in range(8)]
    xc_s = [sb("xc%d" % k, [128, 512]) for k in range(2)]; Bxc_s = [Buf("xc%d" % k) for k in range(2)]
    xcb_s = [sb("xcb%d" % k, [128, 512], BF16) for k in range(2)]; Bxcb_s = [Buf("xcb%d" % k) for k in range(2)]
    r_s = [sb("r%d" % k, [128, 512]) for k in range(2)]; Br_s = [Buf("r%d" % k) for k in range(2)]
    i_s = [sb("i%d" % k, [128, 512]) for k in range(2)]; Bi_s = [Buf("i%d" % k) for k in range(2)]
    a_s = [sb("a%d" % k, [128, 512]) for k in range(2)]; Ba_s = [Buf("a%d" % k) for k in range(2)]
    s_s = [sb("s%d" % k, [128, 512]) for k in range(2)]; Bs_s = [Buf("s%d" % k) for k in range(2)]
    sgr_s = [sb("sgr%d" % k, [128, 512], BF16) for k in range(2)]; Bsgr_s = [Buf("sgr%d" % k) for k in range(2)]
    r_t, Br, i_t, Bi, a_t, Ba, s_t, Bs = r_s[0], Br_s[0], i_s[0], Bi_s[0], a_s[0], Ba_s[0], s_s[0], Bs_s[0]
    ptmp, Bptmp = s_s[1], Bs_s[1]
    hcar = sb("hcar", [128, 8]); Bhcar = [Buf("hcar%d" % c) for c in range(8)]
    I_t = sb("I_t", [128, 2048]); BI = Buf("I")
    rj = [sb("rj%d" % i, [128, 512]) for i in range(2)]; Brj = [Buf("rj%d" % i) for i in range(2)]
    mask = sb("mask", [128, 2048], BF16); Bmask = Buf("mask")
    nmask = sb("nmask", [128, 16, 512], BF16); Bnmask = [Buf("nmask%d" % j) for j in range(16)]
    identrep4 = sb("identrep4", [128, 512], BF16)
    SI = sb("SI", [128, 4, 512], BF16)
    iota1_full = sb("iota1_full", [128, 2048], BF16)
    ntri_rep = sb("ntri_rep", [128, 512], BF16)
    nsm_s = [sb("nsm%d" % k, [128, 1], BF16) for k in range(2)]; Bnsm_s = [Buf("nsm%d" % k) for k in range(2)]
    bis_s = [sb("bis%d" % k, [128, 8]) for k in range(2)]; Bbis_s = [Buf("bis%d" % k) for k in range(2)]
    steps = sb("steps", [128, NBIS]); steps2 = sb("steps2", [128, NBIS]); Bsteps = Buf("steps")
    qlb_s = [sb("qlb%d" % k, [128, 512], BF16) for k in range(4)]; Bqlb_s = [Buf("qlb%d" % k) for k in range(4)]
    absq = sb("absq", [128, 512], BF16); Babsq = Buf("absq")
    p_t = [sb("p%d" % i, [128, 512], BF16) for i in range(2)]; Bp = [Buf("p%d" % i) for i in range(2)]
    rs_s = [sb("rs%d" % k, [128, 512]) for k in range(2)]; Brs_s = [Buf("rs%d" % k) for k in range(2)]
    oTn_s = [sb("oTn%d" % k, [128, 512], BF16) for k in range(2)]; BoTn_s = [Buf("oTn%d" % k) for k in range(2)]
    ga_t, Bga = r_t, Br
    gr_t, Bgr = i_t, Bi
    m1_t, Bm1 = a_t, Ba
    m2_t, Bm2 = s_t, Bs
    Bres = [Buf("res0"), Buf("res1")]

    ps = [nc.psum_tensor("ps%d" % i, [128, 512], F32).__enter__() for i in range(8)]
    Bps = [Buf("ps%d" % i) for i in range(8)]

    def psbf(i):
        return ps[i][:, :].bitcast(BF16)

    op = S_.op

    with nc.allow_non_contiguous_dma(reason="one-time small parameter layouts"):
        for ci, (src, col0, wd) in enumerate(chunk_src):
            S_.dma("pool", "cv", wbf[ci].rearrange("p (k j) -> p k j", k=8)[:, :, 0:wd],
                   src.rearrange("(k p) n -> p k n", p=128)[:, :, col0:col0 + wd], writes=[Bwbf[ci]])
        for b_ in Bwbf:
            b_.w = ("dma:cv", S_.dsem["dma:cv"][1])
        S_.dma("pool", "cs", wout_sb[:, :, :], w_out.rearrange("(k p) n -> p k n", p=128), writes=[Bconst])
        op("pool", lambda e: e.memset(wuvP[:, :, :], 0.0), writes=[Bconst])
        for par in range(2):
            S_.dma("pool", "cs", wuvP[:, :, :].rearrange("c (hp two) d -> c hp two d", two=2)[:, :, par, par * 64:par * 64 + 64],
                   w_uv.rearrange("(hp two) c d -> c hp two d", two=2)[:, :, par, :], reads=[Bconst], writes=[Bconst])
        S_.dma("pool", "cs", wuk_nat[:, :, :], w_uk.rearrange("h c d -> c h d"), writes=[Bconst])
        op("pool", lambda e: e.memset(BDa[:, :, :], 0.0), writes=[Bconst])
        op("pool", lambda e: e.memset(BDx[:, :, :], 0.0), writes=[Bconst])
        for (bd, wsrc) in ((BDa, w_rg_a), (BDx, w_rg_x)):
            for par in range(2):
                S_.dma("pool", "cs", bd[par * 64:par * 64 + 64, :, par * 64:par * 64 + 64],
                       wsrc.rearrange("(p two) i j -> two i p j", two=2)[par], reads=[Bconst], writes=[Bconst])
        for (dst, src, n) in ((gcol, norm_gain, 8), (bmcol, b_merge, 16), (cbcol, conv_b, 8), (bacol, b_rg_a, 8),
                              (bxcol, b_rg_x, 8), (lamcol, lru_lambda, 8)):
            S_.dma("sp", "cs2", dst[:, :], src.rearrange("(c p) -> p c", p=128), writes=[Bconst])
        S_.dma("sp", "cs2", cwcol[:, :, :], conv_w.rearrange("k (c p) -> p k c", p=128), writes=[Bconst])
        S_.dma("sp", "cs2", fg_bc[:, :], final_norm_gain.partition_broadcast(128), writes=[Bconst])
        S_.dma("sp", "cs2", gkv_bc[:, :], kv_norm_gain.partition_broadcast(128), writes=[Bconst])
        S_.dma("sp", "cs2", lng_bc[:, :], idx_ln_gain.partition_broadcast(128), writes=[Bconst])
        S_.dma("sp", "cs2", lnb_bc[:, :], idx_ln_bias.partition_broadcast(128), writes=[Bconst])

    for par in range(2):
        op("pool", lambda e, par=par: e.memset(kz[par][:, :], 0.0), writes=[BkT])
    op("pool", lambda e: e.iota(iota_row[:, :], [[1, 128]], base=0, channel_multiplier=0,
                                allow_small_or_imprecise_dtypes=True), writes=[Bconst])
    op("pool", lambda e: e.iota(pidx[:, :], [[0, 1]], base=0, channel_multiplier=1,
                                allow_small_or_imprecise_dtypes=True), reads=[Bconst], writes=[Bconst])
    op("dve", lambda e: e.tensor_scalar(out=ident[:, :], in0=iota_row[:, :], scalar1=pidx[:, 0:1], scalar2=None,
                                        op0=ALU.is_equal), reads=[Bconst], writes=[Bconst, Bident])
    op("dve", lambda e: e.tensor_scalar(out=triT[:, :], in0=iota_row[:, :], scalar1=pidx[:, 0:1], scalar2=None,
                                        op0=ALU.is_ge), reads=[Bconst], writes=[Bconst])
    op("dve", lambda e: e.tensor_scalar(out=causal_neg[:, :], in0=iota_row[:, :], scalar1=pidx[:, 0:1], scalar2=-1e30,
                                        op0=ALU.is_gt, op1=ALU.mult), reads=[Bconst], writes=[Bconst])
    op("dve", lambda e: e.memset(ones_bf[:, :], 1.0), reads=[Bconst], writes=[Bconst])
    for k in range(NBIS):
        op("dve", lambda e, k=k: e.memset(pow2[:, k:k + 1], 2.0 ** -(k + 1)), reads=[Bconst], writes=[Bconst])
    op("dve", lambda e: e.tensor_reduce(out=nhc[:, 0:1], in_=gkv_bc[:, :], axis=AX.X, op=ALU.max, apply_absolute_value=True),
       reads=[Bconst], writes=[Bconst])
    op("dve", lambda e: e.tensor_scalar(out=nhc[:, 0:1], in0=nhc[:, 0:1], scalar1=-0.5 * (128.0 ** 0.5), scalar2=None, op0=ALU.mult),
       reads=[Bconst], writes=[Bconst])
    op("dve", lambda e: e.tensor_copy(out=nhc_bf[:, 0:1], in_=nhc[:, 0:1]), reads=[Bconst], writes=[Bconst])
    op("act", lambda e: e.activation(out=kap[:, :], in_=lamcol[:, :], func=AF.Exp, scale=-1.0), reads=[Bconst], writes=[Bconst])
    op("act", lambda e: e.activation(out=kap[:, :], in_=kap[:, :], func=AF.Ln, bias=1.0, scale=1.0), reads=[Bconst], writes=[Bconst])
    op("dve", lambda e: e.tensor_scalar(out=kap2[:, :], in0=kap[:, :], scalar1=-16.0, scalar2=None, op0=ALU.mult), reads=[Bconst], writes=[Bconst])
    op("dve", lambda e: e.tensor_scalar(out=kap[:, :], in0=kap[:, :], scalar1=-8.0, scalar2=None, op0=ALU.mult), reads=[Bconst], writes=[Bconst])
    for pr in range(8):
        op("pe", lambda e, pr=pr: e.transpose(out=psbf(0)[:, pr * 128:(pr + 1) * 128],
                                              in_=wuk_nat[:, 2 * pr:2 * pr + 2, :].rearrange("c h d -> c (h d)"),
                                              identity=ident[:, :]), reads=[Bconst], writes=[Bps[0]])
    op("dve", lambda e: e.memset(wukTz[:, :, :], 0.0), reads=[Bconst], writes=[Bconst])
    for par in range(2):
        op("dve", lambda e, par=par: e.tensor_copy(
            out=wukTz[par * 64:par * 64 + 64, :, :].rearrange("p (pr two) c -> p pr two c", two=2)[:, :, par, :],
            in_=psbf(0)[par * 64:par * 64 + 64, :].rearrange("p (pr c) -> p pr c", c=128)), reads=[Bps[0], Bconst], writes=[Bconst])
    def sel(dst, ks):
        for n, k in enumerate(ks):
            tgt = dst if n == 0 else etmp
            op("dve", lambda e, k=k, tgt=tgt: e.tensor_scalar(out=tgt[:, :], in0=pidx[0:32, :], scalar1=float(k), scalar2=None,
                                                             op0=ALU.is_equal), reads=[Bconst], writes=[Bconst])
            if n > 0:
                op("dve", lambda e: e.tensor_tensor(out=dst[:, :], in0=dst[:, :], in1=etmp[:, :], op=ALU.add), reads=[Bconst], writes=[Bconst])
    sel(e0, [0]); sel(e12, [1, 2]); sel(e34, [3, 4]); sel(e13, [1, 3]); sel(e24, [2, 4])
    slopes = [2.0 ** (-8.0 * (h + 1) / 16.0) for h in range(16)]
    for h in range(16):
        hi, lo = _bf16_split(slopes[h])
        op("dve", lambda e, h=h, hi=hi: e.memset(Bcol_hi[:, h:h + 1], hi), reads=[Bconst], writes=[Bconst])
        op("dve", lambda e, h=h, lo=lo: e.memset(Bcol_lo[:, h:h + 1], lo), reads=[Bconst], writes=[Bconst])
    op("dve", lambda e: e.tensor_scalar(out=Bcol[:, :], in0=Bcol_hi[:, :], scalar1=e13[:, 0:1], scalar2=None, op0=ALU.mult), reads=[Bconst], writes=[Bconst])
    op("dve", lambda e: e.scalar_tensor_tensor(out=Bcol[:, :], in0=Bcol_lo[:, :], scalar=e24[:, 0:1], in1=Bcol[:, :],
                                               op0=ALU.mult, op1=ALU.add), reads=[Bconst], writes=[Bconst])
    for g in range(4):
        for hl in range(4):
            h = 4 * g + hl
            op("dve", lambda e, g=g, hl=hl, h=h: e.tensor_scalar(out=Bwork[g][:, hl * 128:(hl + 1) * 128], in0=ones_bf[0:32, :],
                                                                  scalar1=Bcol[:, h:h + 1], scalar2=None, op0=ALU.mult),
               reads=[Bconst], writes=[BBwork[g]])
    for dl in range(16):
        op("dve", lambda e, dl=dl: e.tensor_scalar(out=cold[:, dl:dl + 1], in0=e12[:, :], scalar1=128.0 * dl, scalar2=None,
                                                   op0=ALU.mult), reads=[Bconst], writes=[Bconst])
        op("dve", lambda e, dl=dl: e.tensor_tensor(out=cold[:, dl:dl + 1], in0=cold[:, dl:dl + 1], in1=e0[:, :], op=ALU.add), reads=[Bconst], writes=[Bconst])
        op("dve", lambda e, dl=dl: e.tensor_scalar(out=A_all[:, dl, :], in0=iota_row[0:32, :], scalar1=e34[:, 0:1], scalar2=cold[:, dl:dl + 1],
                                                   op0=ALU.mult, op1=ALU.add), reads=[Bconst], writes=[Bconst])

    for r4 in range(4):
        op("dve", lambda e, r4=r4: e.tensor_copy(out=identrep4[:, r4 * 128:(r4 + 1) * 128], in_=ident[:, :]), reads=[Bconst], writes=[Bconst])
        op("dve", lambda e, r4=r4: e.tensor_scalar(out=ntri_rep[:, r4 * 128:(r4 + 1) * 128], in0=triT[:, :], scalar1=-1.0, scalar2=32768.0,
                                                   op0=ALU.add, op1=ALU.mult), reads=[Bconst], writes=[Bconst])
    for h in range(16):
        op("dve", lambda e, h=h: e.tensor_scalar(out=SI[:, h // 4, (h % 4) * 128:(h % 4 + 1) * 128], in0=ident[:, :], scalar1=slopes[h], scalar2=None,
                                                 op0=ALU.mult), reads=[Bconst], writes=[Bconst])
    op("pool", lambda e: e.iota(iota1_full[:, :], [[1, 2048]], base=1, channel_multiplier=0,
                                allow_small_or_imprecise_dtypes=True), reads=[Bconst], writes=[Bconst])

    wslot = [0]

    def load_chunk(ci):
        j = wslot[0] % 4
        wslot[0] += 1
        wd = chunk_src[ci][2]
        if wd == 128:
            S_.dma("sp", "wl%d" % j, wch[j][:, :, :].rearrange("p k j -> p (k j)"), wbf[ci], reads=[Bwbf[ci]], writes=[Bwch[j]])
        else:
            S_.dma("sp", "wl%d" % j, wch[j][:, :, 0:wd], wbf[ci].rearrange("p (k j) -> p k j", k=8)[:, :, 0:wd],
                   reads=[Bwbf[ci]], writes=[Bwch[j]])
        return j

    def proj_fm(j, wd, bank, rhs_t, rhs_b):
        def f(e):
            ins = None
            for k in range(8):
                ins = e.matmul(ps[bank][0:wd, :], lhsT=wch[j][:, k, 0:wd], rhs=rhs_t[:, k, :], start=(k == 0), stop=(k == 7))
            return ins
        op("pe", f, reads=[Bwch[j], rhs_b], writes=[Bps[bank]])

    def rstd_from_ss(stt, Bstt, col_ss, col_out, n):
        op("act", lambda e: e.activation(out=stt[:, col_out:col_out + 1], in_=stt[:, col_ss:col_ss + 1], func=AF.Sqrt,
                                         bias=EPS, scale=1.0 / n), reads=[Bstt], writes=[Bstt])
        op("dve", lambda e: e.reciprocal(out=stt[:, col_out:col_out + 1], in_=stt[:, col_out:col_out + 1]), reads=[Bstt], writes=[Bstt])

    bankrr = [0]

    def next_bank():
        b = bankrr[0] % 2
        bankrr[0] += 1
        return b

    dbg = {}

    def chk(n):
        if stop is not None and stop == n:
            raise _Stop()

    try:
        chk(0)
        for sq in range(nseq):
            for tb in range(NBLK):
                t0 = tb * 512
                for i in range(4):
                    xb = i % 2
                    S_.dma("sp", "xl%d" % xb, xt[xb][:, :], x[sq, t0 + i * 128:t0 + (i + 1) * 128, :], writes=[Bxt[xb]])
                    op("act", lambda e, xb=xb: e.activation(out=junk[:, 0:1024], in_=xt[xb][:, :], func=AF.Square, accum_out=st[:, 0:1]),
                       reads=[Bxt[xb]], writes=[Bjunk_act, Bst])
                    rstd_from_ss(st, Bst, 0, 1, 1024.0)
                    op("dve", lambda e, xb=xb: e.tensor_scalar(out=xs[:, :], in0=xt[xb][:, :], scalar1=st[:, 1:2], scalar2=None, op0=ALU.mult),
                       reads=[Bxt[xb], Bst], writes=[Bxs])
                    def ftr(e):
                        ins = None
                        for k in range(8):
                            ins = e.transpose(out=psbf(2)[:, k * 128:(k + 1) * 128], in_=xs[:, k * 128:(k + 1) * 128], identity=ident[:, :])
                        return ins
                    op("pe", ftr, reads=[Bxs, Bident], writes=[Bps[2]])
                    for k in range(8):
                        op("dve", lambda e, k=k, i=i: e.tensor_scalar(out=xnT[:, k, i * 128:(i + 1) * 128], in0=psbf(2)[:, k * 128:(k + 1) * 128],
                                                                       scalar1=gcol[:, k:k + 1], scalar2=None, op0=ALU.mult),
                           reads=[Bps[2], Bconst], writes=[BxnT])

                chk(1)
                for c in range(8):
                    j = load_chunk(CH["q"][c]); bk = next_bank()
                    proj_fm(j, 128, bk, xnT, BxnT)
                    op("act", lambda e, c=c, bk=bk: e.copy(out=qT[:, c, :], in_=ps[bk][:, :]), reads=[Bps[bk]], writes=[BqT])
                for c in range(4):
                    j = load_chunk(CH["qidx"][c]); bk = next_bank()
                    proj_fm(j, 128, bk, xnT, BxnT)
                    op("act", lambda e, c=c, bk=bk: e.mul(out=qidxT[:, c, :], in_=ps[bk][:, :], mul=0.125),
                       reads=[Bps[bk]], writes=[BqidxT])
                j = load_chunk(CH["ckv"][0])
                for i in range(4):
                    gt = tb * 4 + i
                    def fc(e, i=i, j=j):
                        ins = None
                        for k in range(8):
                            ins = e.matmul(ps[2][:, 0:128], lhsT=xnT[:, k, i * 128:(i + 1) * 128], rhs=wch[j][:, k, :], start=(k == 0), stop=(k == 7))
                        return ins
                    op("pe", fc, reads=[Bwch[j], BxnT], writes=[Bps[2]])
                    op("act", lambda e: e.activation(out=junk[:, 0:128], in_=ps[2][:, 0:128], func=AF.Square, accum_out=st2[:, 0:1]),
                       reads=[Bps[2]], writes=[Bjunk_act, Bst2])
                    rstd_from_ss(st2, Bst2, 0, 1, 128.0)
                    op("dve", lambda e, gt=gt: e.scalar_tensor_tensor(out=c_tm[:, gt, :], in0=ps[2][:, 0:128], scalar=st2[:, 1:2], in1=gkv_bc[:, :],
                                                                       op0=ALU.mult, op1=ALU.mult), reads=[Bps[2], Bst2, Bconst], writes=[Bctm])
                    op("pe", lambda e, gt=gt: e.transpose(out=psbf(3)[:, 0:128], in_=c_tm[:, gt, :], identity=ident[:, :]),
                       reads=[Bctm, Bident], writes=[Bps[3]])
                    op("dve", lambda e, gt=gt: e.tensor_copy(out=cT[:, gt * 128:(gt + 1) * 128], in_=psbf(3)[:, 0:128]), reads=[Bps[3]], writes=[BcT])
                j = load_chunk(CH["kw"][0])
                for i in range(4):
                    gt = tb * 4 + i
                    def fk(e, i=i, j=j):
                        ins = None
                        for k in range(8):
                            ins = e.matmul(ps[2][:, 0:72], lhsT=xnT[:, k, i * 128:(i + 1) * 128], rhs=wch[j][:, k, 0:72], start=(k == 0), stop=(k == 7))
                        return ins
                    op("pe", fk, reads=[Bwch[j], BxnT], writes=[Bps[2]])
                    op("dve", lambda e, gt=gt: e.tensor_scalar(out=w_tm[:, gt, :], in0=ps[2][:, 64:72], scalar1=8.0 ** -0.5, scalar2=None, op0=ALU.mult),
                       reads=[Bps[2]], writes=[Bwtm])
                    op("dve", lambda e: e.tensor_reduce(out=st2[:, 2:3], in_=ps[2][:, 0:64], axis=AX.X, op=ALU.add), reads=[Bps[2]], writes=[Bst2])
                    op("dve", lambda e: e.tensor_scalar(out=st2[:, 2:3], in0=st2[:, 2:3], scalar1=-1.0 / 64.0, scalar2=None, op0=ALU.mult), reads=[Bst2], writes=[Bst2])
                    op("dve", lambda e: e.tensor_scalar(out=kc[:, :], in0=ps[2][:, 0:64], scalar1=st2[:, 2:3], scalar2=None, op0=ALU.add),
                       reads=[Bps[2], Bst2], writes=[Bkc])
                    op("act", lambda e: e.activation(out=junk[:, 0:64], in_=kc[:, :], func=AF.Square, accum_out=st2[:, 3:4]),
                       reads=[Bkc], writes=[Bjunk_act, Bst2])
                    rstd_from_ss(st2, Bst2, 3, 4, 64.0)
                    op("dve", lambda e: e.scalar_tensor_tensor(out=kc[:, :], in0=kc[:, :], scalar=st2[:, 4:5], in1=lng_bc[:, :], op0=ALU.mult, op1=ALU.mult),
                       reads=[Bkc, Bst2, Bconst], writes=[Bkc])
                    op("dve", lambda e: e.tensor_tensor(out=kn2[:, 0:64], in0=kc[:, :], in1=lnb_bc[:, :], op=ALU.add), reads=[Bkc, Bconst], writes=[Bkn2])
                    op("dve", lambda e: e.tensor_copy(out=kn2[:, 64:128], in_=kn2[:, 0:64]), reads=[Bkn2], writes=[Bkn2])
                    op("pe", lambda e: e.transpose(out=psbf(3)[:, 0:128], in_=kn2[:, :], identity=ident[:, :]), reads=[Bkn2, Bident], writes=[Bps[3]])
                    for par in range(2):
                        op("dve", lambda e, gt=gt, par=par: e.tensor_copy(out=kz[par][par * 64:par * 64 + 64, gt * 128:(gt + 1) * 128],
                                                                           in_=psbf(3)[par * 64:par * 64 + 64, 0:128]), reads=[Bps[3]], writes=[BkT])
                for c in range(8):
                    j = load_chunk(CH["gattn"][c]); bk = next_bank()
                    proj_fm(j, 128, bk, xnT, BxnT)
                    op("act", lambda e, c=c, bk=bk: e.activation(out=sgT[:, c, :], in_=ps[bk][:, :], func=AF.Silu), reads=[Bps[bk]], writes=[BsgT[c]])

                chk(2)
                def topk_gen(i, gt):
                    ns = gt + 1
                    ncols = ns * 128
                    bis, Bbis, nsm, Bnsm = bis_s[i % 2], Bbis_s[i % 2], nsm_s[i % 2], Bnsm_s[i % 2]
                    nsb = (ncols + 511) // 512
                    for sbk in range(nsb):
                        c0 = sbk * 512
                        cw = min(512, ncols - c0)
                        for jh in range(8):
                            par = jh % 2
                            bk = 7
                            op("pe", lambda e, jh=jh, par=par, bk=bk, c0=c0, cw=cw: e.matmul(
                                ps[bk][:, 0:cw], lhsT=qidxT[:, jh // 2, i * 128:(i + 1) * 128],
                                rhs=kz[par][:, c0:c0 + cw], start=True, stop=True),
                               reads=[BqidxT, BkT], writes=[Bps[bk]])
                            rb = jh % 2
                            op("act", lambda e, bk=bk, rb=rb, cw=cw: e.activation(out=rj[rb][:, 0:cw], in_=ps[bk][:, 0:cw], func=AF.Relu),
                               reads=[Bps[bk]], writes=[Brj[rb]])
                            if jh == 0:
                                op("dve", lambda e, rb=rb, c0=c0, cw=cw: e.tensor_scalar(
                                    out=I_t[:, c0:c0 + cw], in0=rj[rb][:, 0:cw], scalar1=w_tm[:, gt, 0:1], scalar2=None, op0=ALU.mult),
                                   reads=[Brj[rb], Bwtm], writes=[BI, Bres[0], Bres[1]])
                            else:
                                op("dve", lambda e, rb=rb, c0=c0, cw=cw, jh=jh: e.scalar_tensor_tensor(
                                    out=I_t[:, c0:c0 + cw], in0=rj[rb][:, 0:cw], scalar=w_tm[:, gt, jh:jh + 1], in1=I_t[:, c0:c0 + cw],
                                    op0=ALU.mult, op1=ALU.add), reads=[Brj[rb], Bwtm, BI], writes=[BI])
                    yield
                    op("dve", lambda e: e.tensor_reduce(out=bis[:, 0:1], in_=I_t[:, 0:ncols], axis=AX.X, op=ALU.max,
                                                        apply_absolute_value=True), reads=[BI], writes=[Bbis])
                    op("dve", lambda e: e.tensor_scalar(out=bis[:, 0:1], in0=bis[:, 0:1], scalar1=1.01, scalar2=1e-6, op0=ALU.mult, op1=ALU.add),
                       reads=[Bbis], writes=[Bbis])
                    op("dve", lambda e: e.tensor_scalar(out=steps[:, :], in0=pow2[:, :], scalar1=bis[:, 0:1], scalar2=None, op0=ALU.mult),
                       reads=[Bbis, Bconst], writes=[Bsteps])
                    op("dve", lambda e: e.tensor_scalar(out=steps2[:, :], in0=steps[:, :], scalar1=2.0, scalar2=None, op0=ALU.mult),
                       reads=[Bsteps], writes=[Bsteps])
                    op("dve", lambda e: e.tensor_tensor(out=I_t[:, gt * 128:(gt + 1) * 128], in0=I_t[:, gt * 128:(gt + 1) * 128],
                                                        in1=causal_neg[:, :], op=ALU.add), reads=[BI, Bconst, Bbis], writes=[BI])
                    op("dve", lambda e: e.memset(bis[:, 1:2], 0.0), reads=[Bbis], writes=[Bbis])
                    for kb in range(NBIS):
                        op("dve", lambda e: e.tensor_scalar(out=mask[:, 0:ncols], in0=I_t[:, 0:ncols], scalar1=bis[:, 1:2], scalar2=None,
                                                            op0=ALU.is_ge, op1=ALU.add, accum_out=bis[:, 2:3]),
                           reads=[BI, Bbis], writes=[Bmask, Bbis])
                        op("dve", lambda e, kb=kb: e.tensor_scalar(out=bis[:, 3:4], in0=bis[:, 2:3], scalar1=TOPK - 0.5, scalar2=steps2[:, kb:kb + 1],
                                                                   op0=ALU.is_ge, op1=ALU.mult), reads=[Bbis, Bsteps], writes=[Bbis])
                        op("dve", lambda e, kb=kb: e.scalar_tensor_tensor(out=bis[:, 1:2], in0=bis[:, 1:2], scalar=steps[:, kb:kb + 1], in1=bis[:, 3:4],
                                                                          op0=ALU.subtract, op1=ALU.add), reads=[Bbis, Bsteps], writes=[Bbis])
                        if kb in (3, 7):
                            yield
                    op("dve", lambda e: e.tensor_scalar(out=mask[:, 0:ncols], in0=I_t[:, 0:ncols], scalar1=bis[:, 1:2], scalar2=None,
                                                        op0=ALU.is_ge), reads=[BI, Bbis], writes=[Bmask])
                    op("dve", lambda e: e.scalar_tensor_tensor(out=I_t[:, 0:ncols], in0=I_t[:, 0:ncols], scalar=bis[:, 1:2],
                                                               in1=iota1_full[:, 0:ncols], op0=ALU.is_ge, op1=ALU.mult),
                       reads=[BI, Bbis, Bconst], writes=[BI])
                    op("dve", lambda e: e.tensor_reduce(out=bis[:, 4:5], in_=I_t[:, 0:ncols], axis=AX.X, op=ALU.max), reads=[BI], writes=[Bbis])
                    op("dve", lambda e: e.tensor_scalar(out=nsm[:, :], in0=bis[:, 4:5], scalar1=-1.0, scalar2=1.0, op0=ALU.mult, op1=ALU.add),
                       reads=[Bbis], writes=[Bnsm])

                def nmask_build(i, gt):
                    for jj in range(gt + 1):
                        mbk = 6 + (jj % 2)
                        op("pe", lambda e, jj=jj, mbk=mbk: e.matmul(ps[mbk][:, :], lhsT=mask[:, jj * 128:(jj + 1) * 128], rhs=identrep4[:, :],
                                                                     start=True, stop=True), reads=[Bmask, Bconst], writes=[Bps[mbk]])
                        op("dve", lambda e, jj=jj, mbk=mbk: e.tensor_scalar(out=nmask[:, jj, :], in0=ps[mbk][:, :], scalar1=-1.0, scalar2=32768.0,
                                                                             op0=ALU.add, op1=ALU.mult), reads=[Bps[mbk]], writes=[Bnmask[jj]])

                def rnn_steps(c, k):
                    xc, Bxc, xcb, Bxcb = xc_s[k], Bxc_s[k], xcb_s[k], Bxcb_s[k]
                    rr, Brr, ii, Bii, aa, Baa, ss, Bss = r_s[k], Br_s[k], i_s[k], Bi_s[k], a_s[k], Ba_s[k], s_s[k], Bs_s[k]
                    sg, Bsg, uu, Buu = sgr_s[k], Bsgr_s[k], ub[k], Bub[k]
                    j = load_chunk(CH["xrnn"][c]); bk = next_bank()
                    proj_fm(j, 128, bk, xnT, BxnT)
                    if tb == 0:
                        op("pool", lambda e: e.memset(uu[:, 0:3], 0.0), reads=[], writes=[Buu])
                        op("dve", lambda e: e.memset(hcar[:, c:c + 1], 0.0), reads=[], writes=[Bhcar[c]])
                    else:
                        op("pool", lambda e: e.tensor_copy(out=uu[:, 0:3], in_=ucar[:, c, :]), reads=[Bucar[c]], writes=[Buu])
                    op("act", lambda e: e.copy(out=uu[:, 3:515], in_=ps[bk][:, :]), reads=[Bps[bk]], writes=[Buu])
                    yield
                    op("dve", lambda e: e.tensor_scalar(out=xc[:, :], in0=uu[:, 3:515], scalar1=cwcol[:, 3, c:c + 1], scalar2=cbcol[:, c:c + 1],
                                                        op0=ALU.mult, op1=ALU.add), reads=[Buu, Bconst], writes=[Bxc])
                    for kk in range(3):
                        op("dve", lambda e, kk=kk: e.scalar_tensor_tensor(out=xc[:, :], in0=uu[:, kk:kk + 512], scalar=cwcol[:, kk, c:c + 1],
                                                                          in1=xc[:, :], op0=ALU.mult, op1=ALU.add),
                           reads=[Buu, Bconst, Bxc], writes=[Bxc])
                    op("pool", lambda e: e.tensor_copy(out=ucar[:, c, :], in_=uu[:, 512:515]), reads=[Buu], writes=[Bucar[c]])
                    op("pool", lambda e: e.tensor_copy(out=xcb[:, :], in_=xc[:, :]), reads=[Bxc], writes=[Bxcb])
                    yield
                    bk1 = next_bank()
                    op("pe", lambda e: e.matmul(ps[bk1][:, :], lhsT=BDa[:, c, :], rhs=xcb[:, :], start=True, stop=True),
                       reads=[Bxcb, Bconst], writes=[Bps[bk1]])
                    op("act", lambda e: e.activation(out=rr[:, :], in_=ps[bk1][:, :], func=AF.Sigmoid, bias=bacol[:, c:c + 1], scale=1.0),
                       reads=[Bps[bk1], Bconst], writes=[Brr])
                    bk2 = next_bank()
                    op("pe", lambda e: e.matmul(ps[bk2][:, :], lhsT=BDx[:, c, :], rhs=xcb[:, :], start=True, stop=True),
                       reads=[Bxcb, Bconst], writes=[Bps[bk2]])
                    op("act", lambda e: e.activation(out=ii[:, :], in_=ps[bk2][:, :], func=AF.Sigmoid, bias=bxcol[:, c:c + 1], scale=1.0),
                       reads=[Bps[bk2], Bconst], writes=[Bii])
                    yield
                    op("act", lambda e: e.activation(out=aa[:, :], in_=rr[:, :], func=AF.Exp, scale=kap[:, c:c + 1]), reads=[Brr, Bconst], writes=[Baa])
                    op("act", lambda e: e.activation(out=ss[:, :], in_=rr[:, :], func=AF.Exp, scale=kap2[:, c:c + 1]), reads=[Brr, Bconst], writes=[Bss])
                    yield
                    op("act", lambda e: e.activation(out=ss[:, :], in_=ss[:, :], func=AF.Sqrt, bias=1.0, scale=-1.0), reads=[Bss], writes=[Bss])
                    op("dve", lambda e: e.tensor_tensor(out=ii[:, :], in0=ii[:, :], in1=xc[:, :], op=ALU.mult), reads=[Bii, Bxc], writes=[Bii])
                    yield
                    op("dve", lambda e: e.tensor_tensor(out=ii[:, :], in0=ii[:, :], in1=ss[:, :], op=ALU.mult), reads=[Bii, Bss], writes=[Bii])
                    op("dve", lambda e: e.tensor_tensor_scan(out=rr[:, :], data0=aa[:, :], data1=ii[:, :], initial=hcar[:, c:c + 1],
                                                             op0=ALU.mult, op1=ALU.add), reads=[Baa, Bii, Bhcar[c]], writes=[Brr])
                    op("dve", lambda e: e.tensor_copy(out=hcar[:, c:c + 1], in_=rr[:, 511:512]), reads=[Brr], writes=[Bhcar[c]])
                    j2 = load_chunk(CH["grnn"][c]); bk3 = next_bank()
                    proj_fm(j2, 128, bk3, xnT, BxnT)
                    yield
                    op("act", lambda e: e.activation(out=sg[:, :], in_=ps[bk3][:, :], func=AF.Silu), reads=[Bps[bk3]], writes=[Bsg])
                    op("dve", lambda e: e.tensor_tensor(out=hgT[:, c, :], in0=rr[:, :], in1=sg[:, :], op=ALU.mult), reads=[Brr, Bsg], writes=[BhgT])

                tk0 = topk_gen(0, tb * 4) if tb >= 1 else None
                for c in range(0, 8, 2):
                    g0, g1 = rnn_steps(c, 0), rnn_steps(c + 1, 1)
                    alive = [g0, g1]
                    while alive:
                        for g in list(alive):
                            try:
                                next(g)
                            except StopIteration:
                                alive.remove(g)
                    if tk0 is not None:
                        next(tk0, None)
                if tk0 is not None:
                    for _ in tk0:
                        pass
                    nmask_build(0, tb * 4)

                chk(3)
                def attn_prologue(i, gt):
                    nsm, Bnsm = nsm_s[i % 2], Bnsm_s[i % 2]
                    if gt < 2:
                        op("dve", lambda e: e.tensor_scalar(out=nsm[:, :], in0=pidx[:, :], scalar1=-1.0, scalar2=-128.0 * gt, op0=ALU.mult, op1=ALU.add),
                           reads=[Bconst], writes=[Bnsm])
                    for hg in range(4):
                        def fql(e, hg=hg):
                            ins = None
                            for hl in range(4):
                                h = 4 * hg + hl
                                ins = e.matmul(ps[6][:, hl * 128:(hl + 1) * 128], lhsT=wukTz[:, h, :],
                                               rhs=qT[:, h // 2, i * 128:(i + 1) * 128], start=True, stop=True)
                            return ins
                        op("pe", fql, reads=[BqT, Bconst], writes=[Bps[6]])
                        op("act", lambda e, hg=hg: e.mul(out=qlb_s[hg][:, :], in_=ps[6][:, :], mul=0.125), reads=[Bps[6]], writes=[Bqlb_s[hg]])
                        op("act", lambda e: e.activation(out=absq[:, :], in_=ps[6][:, :], func=AF.Square, scale=0.125), reads=[Bps[6]], writes=[Babsq])
                        def fnm(e, hg=hg):
                            e.matmul(ps[4][0:1, :], lhsT=nhc_bf[:, 0:1], rhs=absq[:, :], start=True, stop=False)
                            return e.matmul(ps[4][0:1, :], lhsT=nsm[:, 0:1], rhs=SI[:, hg, :], start=False, stop=True)
                        op("pe", fnm, reads=[Babsq, Bnsm, Bconst], writes=[Bps[4]])
                        op("act", lambda e, hg=hg: e.activation(out=Bwork[hg][0:1, :], in_=ps[4][0:1, :], func=AF.Identity, bias=nhc[0:1, 0:1], scale=1.0),
                           reads=[Bps[4], Bconst], writes=[BBwork[hg]])

                def attn_hg(i, gt, hg):
                    ns = gt + 1
                    qlb, Bqlb = qlb_s[hg], Bqlb_s[hg]
                    def flg_exp(js):
                        lb = js % 2
                        if gt >= 2:
                            mrhs, mbuf = nmask[:, js, :], Bnmask[js]
                        elif js == gt:
                            mrhs, mbuf = ntri_rep[:, :], Bconst
                        else:
                            mrhs, mbuf = None, None
                        def flg(e):
                            e.matmul(ps[lb][:, :], lhsT=cT[:, js * 128:(js + 1) * 128], rhs=qlb[:, :], start=True, stop=False)
                            if mrhs is None:
                                return e.matmul(ps[lb][:, :], lhsT=A_all[:, js, :], rhs=Bwork[hg][:, :], start=False, stop=True)
                            e.matmul(ps[lb][:, :], lhsT=A_all[:, js, :], rhs=Bwork[hg][:, :], start=False, stop=False)
                            return e.matmul(ps[lb][:, :], lhsT=ident[:, :], rhs=mrhs, start=False, stop=True)
                        op("pe", flg, reads=[BcT, Bqlb, BBwork[hg], Bconst] + ([mbuf] if mbuf is not None else []), writes=[Bps[lb]])
                        op("act", lambda e: e.activation(out=p_t[lb][:, :], in_=ps[lb][:, :], func=AF.Exp), reads=[Bps[lb]], writes=[Bp[lb]])
                    bo, bs_ = (2, 3) if hg % 2 == 0 else (4, 5)
                    def fpv_(js):
                        lb = js % 2
                        def fpv(e):
                            e.matmul(ps[bo][:, :], lhsT=c_tm[:, js, :], rhs=p_t[lb][:, :], start=(js == 0), stop=(js == ns - 1))
                            return e.matmul(ps[bs_][:, :], lhsT=ones_bf[:, :], rhs=p_t[lb][:, :], start=(js == 0), stop=(js == ns - 1))
                        op("pe", fpv, reads=[Bctm, Bp[lb], Bconst], writes=[Bps[bo], Bps[bs_]])
                    flg_exp(0)
                    for js in range(ns):
                        if js + 1 < ns:
                            flg_exp(js + 1)
                        fpv_(js)
                    rs_t, Brs, oTn, BoTn = rs_s[hg % 2], Brs_s[hg % 2], oTn_s[hg % 2], BoTn_s[hg % 2]
                    op("dve", lambda e: e.reciprocal(out=rs_t[:, :], in_=ps[bs_][:, :]), reads=[Bps[bs_]], writes=[Brs])
                    op("dve", lambda e: e.tensor_tensor(out=oTn[:, :], in0=ps[bo][:, :], in1=rs_t[:, :], op=ALU.mult), reads=[Bps[bo], Brs], writes=[BoTn])

                def attn_epi(i, hg):
                    oTn, BoTn = oTn_s[hg % 2], BoTn_s[hg % 2]
                    def fy(e):
                        ins = None
                        for pp in range(2):
                            for q2 in range(2):
                                hl = 2 * pp + q2
                                h = 4 * hg + hl
                                ins = e.matmul(ps[6][:, pp * 128:(pp + 1) * 128], lhsT=wuvP[:, h, :], rhs=oTn[:, hl * 128:(hl + 1) * 128],
                                               start=(q2 == 0), stop=(q2 == 1))
                        return ins
                    op("pe", fy, reads=[BoTn, Bconst], writes=[Bps[6]])
                    for pp in range(2):
                        cc = 2 * hg + pp
                        op("dve", lambda e, pp=pp, cc=cc: e.tensor_tensor(out=sgT[:, cc, i * 128:(i + 1) * 128], in0=ps[6][:, pp * 128:(pp + 1) * 128],
                                                                           in1=sgT[:, cc, i * 128:(i + 1) * 128], op=ALU.mult),
                           reads=[Bps[6], BsgT[cc]], writes=[BsgT[cc]])

                gens = {}
                for i in range(1, 4):
                    if tb * 4 + i >= 2:
                        gens[i] = topk_gen(i, tb * 4 + i)
                for i in range(4):
                    gt = tb * 4 + i
                    attn_prologue(i, gt)
                    g = gens.get(i + 1)
                    for hg in range(4):
                        attn_hg(i, gt, hg)
                        if hg > 0:
                            attn_epi(i, hg - 1)
                        if g is not None:
                            next(g, None)
                    attn_epi(i, 3)
                    if g is not None:
                        for _ in g:
                            pass
                        nmask_build(i + 1, gt + 1)

                chk(4)
                mixedT, BmixedT = qT, BqT
                for f in range(8):
                    ja = load_chunk(CH["wap"][f])
                    def fya(e, ja=ja):
                        ins = None
                        for k in range(8):
                            ins = e.matmul(ps[0][:, :], lhsT=wch[ja][:, k, :], rhs=sgT[:, k, :], start=(k == 0), stop=(k == 7))
                        return ins
                    op("pe", fya, reads=[Bwch[ja]] + BsgT, writes=[Bps[0]])
                    jr = load_chunk(CH["wrp"][f])
                    proj_fm(jr, 128, 1, hgT, BhgT)
                    jg = load_chunk(CH["mga"][f])
                    proj_fm(jg, 128, 2, xnT, BxnT)
                    op("act", lambda e, f=f: e.activation(out=ga_t[:, :], in_=ps[2][:, :], func=AF.Sigmoid, bias=bmcol[:, f:f + 1], scale=1.0),
                       reads=[Bps[2], Bconst], writes=[Bga])
                    jg2 = load_chunk(CH["mgr"][f])
                    proj_fm(jg2, 128, 3, xnT, BxnT)
                    op("act", lambda e, f=f: e.activation(out=gr_t[:, :], in_=ps[3][:, :], func=AF.Sigmoid, bias=bmcol[:, 8 + f:9 + f], scale=1.0),
                       reads=[Bps[3], Bconst], writes=[Bgr])
                    op("dve", lambda e: e.tensor_tensor(out=m1_t[:, :], in0=ps[0][:, :], in1=ga_t[:, :], op=ALU.mult), reads=[Bps[0], Bga], writes=[Bm1])
                    op("dve", lambda e: e.tensor_tensor(out=m2_t[:, :], in0=ps[1][:, :], in1=gr_t[:, :], op=ALU.mult), reads=[Bps[1], Bgr], writes=[Bm2])
                    op("pool", lambda e, f=f: e.tensor_tensor(out=mixedT[:, f, :], in0=m1_t[:, :], in1=m2_t[:, :], op=ALU.add), reads=[Bm1, Bm2], writes=[BmixedT])
                for i in range(4):
                    xb = i % 2
                    rk = i % 2
                    res_t = I_t[:, rk * 1024:(rk + 1) * 1024]
                    S_.dma("sp", "xl%d" % xb, xt[xb][:, :], x[sq, t0 + i * 128:t0 + (i + 1) * 128, :], writes=[Bxt[xb]])
                    for db in range(2):
                        def fo(e, i=i, db=db):
                            ins = None
                            for k in range(8):
                                ins = e.matmul(ps[4 + db][:, :], lhsT=mixedT[:, k, i * 128:(i + 1) * 128], rhs=wout_sb[:, k, db * 512:(db + 1) * 512],
                                               start=(k == 0), stop=(k == 7))
                            return ins
                        op("pe", fo, reads=[BmixedT, Bconst], writes=[Bps[4 + db]])
                        op("dve", lambda e, db=db, xb=xb, res_t=res_t: e.tensor_tensor(out=res_t[:, db * 512:(db + 1) * 512], in0=ps[4 + db][:, :],
                                                                                       in1=xt[xb][:, db * 512:(db + 1) * 512], op=ALU.add),
                           reads=[Bps[4 + db], Bxt[xb]], writes=[Bres[rk], BI])
                    op("act", lambda e, res_t=res_t: e.activation(out=junk[:, 0:1024], in_=res_t, func=AF.Square, accum_out=st[:, 2:3]),
                       reads=[Bres[rk]], writes=[Bjunk_act, Bst])
                    rstd_from_ss(st, Bst, 2, 3, 1024.0)
                    op("dve", lambda e, res_t=res_t: e.scalar_tensor_tensor(out=res_t, in0=res_t, scalar=st[:, 3:4], in1=fg_bc[:, :],
                                                                            op0=ALU.mult, op1=ALU.mult), reads=[Bres[rk], Bst, Bconst], writes=[Bres[rk]])
                    S_.dma("pool", "os%d" % rk, out[sq, t0 + i * 128:t0 + (i + 1) * 128, :], res_t, reads=[Bres[rk]])
    except _Stop:
        pass
    for key in sorted(S_.dsem):
        nc.gpsimd.wait_ge(S_.dsem[key][0], S_.dsem[key][1])
    for k_ in S_.sem:
        if S_.cnt[k_] > 0 and k_ != "pool":
            nc.gpsimd.wait_ge(S_.sem[k_], S_.cnt[k_])
    return nc


_PARAMS = ["norm_gain", "w_in", "b_merge", "kv_norm_gain", "w_uk", "w_uv", "idx_ln_gain", "idx_ln_bias", "w_attn_proj",
           "conv_w", "conv_b", "w_rg_a", "b_rg_a", "w_rg_x", "b_rg_x", "lru_lambda", "w_rnn_proj", "w_out"]


def kernel(**inputs):
    x = np.ascontiguousarray(np.asarray(inputs["x"], dtype=np.float32))
    B, S, _ = x.shape
    nseq = B // NCORES
    base = {}
    for k in _PARAMS:
        a = np.asarray(inputs[k], dtype=np.float32)
        base[k] = np.ascontiguousarray(a.reshape(a.shape[1:]))
    base["final_norm_gain"] = np.ascontiguousarray(np.asarray(inputs["final_norm_gain"], dtype=np.float32))
    nc = build(nseq, S)
    in_maps = []
    for c in range(NCORES):
        m = dict(base)
        m["x"] = np.ascontiguousarray(x[c * nseq:(c + 1) * nseq])
        in_maps.append(m)
    res = run_bass_kernel_spmd(nc, in_maps, core_ids=list(range(NCORES)))
    return np.concatenate([np.asarray(r["out"], dtype=np.float32) for r in res.results], axis=0)
```

```python
import numpy as np
import concourse.bass as bass
import concourse.mybir as mybir
from concourse.bass_utils import run_bass_kernel_spmd

F32 = mybir.dt.float32
BF16 = mybir.dt.bfloat16
ALU = mybir.AluOpType
AF = mybir.ActivationFunctionType
AX = mybir.AxisListType

D = 1024
NCORES = 8
EPS = 1e-6
TOPK = 256
NBIS = 12


class Buf:
    __slots__ = ("name", "w", "r")

    def __init__(self, name):
        self.name = name
        self.w = None
        self.r = []


class Sched:
    def __init__(self, nc):
        self.nc = nc
        self.eng = {"pe": nc.tensor, "act": nc.scalar, "dve": nc.vector, "pool": nc.gpsimd, "sp": nc.sync}
        self.sem, self.cnt, self.waited, self.dsem = {}, {}, {}, {}
        for k in self.eng:
            self.sem[k] = nc.semaphore("s_" + k).__enter__()
            self.cnt[k] = 0
            self.waited[k] = {}

    def _wait(self, e, dep):
        de, val = dep
        if de == e and e == "pe":
            return
        if self.waited[e].get(de, 0) >= val:
            return
        self.waited[e][de] = val
        s = self.dsem[de][0] if de.startswith("dma:") else self.sem[de]
        self.eng[e].wait_ge(s, val)

    def _deps(self, e, reads, writes):
        deps = set()
        for b in reads:
            if b.w is not None:
                deps.add(b.w)
        for b in writes:
            if b.w is not None:
                deps.add(b.w)
            deps.update(b.r)
        for d in sorted(deps):
            self._wait(e, d)

    def _mark(self, me, reads, writes):
        for b in reads:
            if len(b.r) > 24:
                last = {}
                for (q, v) in b.r:
                    last[q] = max(last.get(q, 0), v)
                b.r = list(last.items())
            b.r.append(me)
        for b in writes:
            b.w = me
            b.r = []

    def op(self, e, fn, reads=(), writes=()):
        self._deps(e, reads, writes)
        ins = fn(self.eng[e])
        self.cnt[e] += 1
        ins.then_inc(self.sem[e], 1)
        me = (e, self.cnt[e])
        self._mark(me, reads, writes)
        return me

    def dma(self, e, q, out, in_, reads=(), writes=()):
        key = "dma:" + q
        if key not in self.dsem:
            self.dsem[key] = [self.nc.semaphore("d_" + q).__enter__(), 0]
        self._deps(e, reads, writes)
        ins = self.eng[e].dma_start(out=out, in_=in_)
        self.dsem[key][1] += 16
        ins.then_inc(self.dsem[key][0], 16)
        me = (key, self.dsem[key][1])
        self._mark(me, reads, writes)
        return me


def _bf16_split(v):
    import ml_dtypes
    hi = float(np.float32(v).astype(ml_dtypes.bfloat16))
    lo = float(np.float32(v - hi).astype(ml_dtypes.bfloat16))
    return hi, lo


class _Stop(Exception):
    pass


def build(nseq, S, debug=False, stop=None):
    NBLK = S // 512
    NTS = S // 128
    nc = bass.Bass("TRN2", target_bir_lowering=False)
    S_ = Sched(nc)

    def din(name, shape):
        return nc.dram_tensor(name, list(shape), F32, kind="ExternalInput").ap()

    x = din("x", [nseq, S, D])
    norm_gain = din("norm_gain", [D])
    w_in = din("w_in", [D, 6856])
    b_merge = din("b_merge", [2048])
    kv_norm_gain = din("kv_norm_gain", [128])
    w_uk = din("w_uk", [16, 128, 64])
    w_uv = din("w_uv", [16, 128, 64])
    idx_ln_gain = din("idx_ln_gain", [64])
    idx_ln_bias = din("idx_ln_bias", [64])
    w_attn_proj = din("w_attn_proj", [D, D])
    conv_w = din("conv_w", [4, D])
    conv_b = din("conv_b", [D])
    w_rg_a = din("w_rg_a", [16, 64, 64])
    b_rg_a = din("b_rg_a", [D])
    w_rg_x = din("w_rg_x", [16, 64, 64])
    b_rg_x = din("b_rg_x", [D])
    lru_lambda = din("lru_lambda", [D])
    w_rnn_proj = din("w_rnn_proj", [D, D])
    w_out = din("w_out", [D, D])
    final_norm_gain = din("final_norm_gain", [D])
    out = nc.dram_tensor("out", [nseq, S, D], F32, kind="ExternalOutput").ap()
    wbf = nc.dram_tensor("wbf", [70, 128, 1024], BF16, kind="Internal").ap()

    def sb(name, shape, dt=F32):
        return nc.sbuf_tensor(name, list(shape), dt).__enter__()

    CH = {}
    chunk_src = []

    def add_chunks(name, src, col0, n, width=128):
        CH[name] = []
        for c in range(n):
            CH[name].append(len(chunk_src))
            chunk_src.append((src, col0 + c * 128, width))

    add_chunks("q", w_in, 0, 8)
    add_chunks("ckv", w_in, 1024, 1)
    add_chunks("qidx", w_in, 1152, 4)
    add_chunks("kw", w_in, 1664, 1, 72)
    add_chunks("gattn", w_in, 1736, 8)
    add_chunks("xrnn", w_in, 2760, 8)
    add_chunks("grnn", w_in, 3784, 8)
    add_chunks("mga", w_in, 4808, 8)
    add_chunks("mgr", w_in, 5832, 8)
    add_chunks("wap", w_attn_proj, 0, 8)
    add_chunks("wrp", w_rnn_proj, 0, 8)
    assert len(chunk_src) == 70
    Bwbf = [Buf("wbf%d" % i) for i in range(70)]

    ident = sb("ident", [128, 128], BF16); Bident = Buf("ident")
    ones_bf = sb("ones_bf", [128, 128], BF16)
    triT = sb("triT", [128, 128], BF16)
    causal_neg = sb("causal_neg", [128, 128])
    iota_row = sb("iota_row", [128, 128])
    pidx = sb("pidx", [128, 1])
    Bconst = Buf("const")

    wout_sb = sb("wout_sb", [128, 8, 1024], BF16)
    wuvP = sb("wuvP", [128, 16, 128], BF16)
    wuk_nat = sb("wuk_nat", [128, 16, 64], BF16)
    wukTz = sb("wukTz", [128, 16, 128], BF16)
    BDa = sb("BDa", [128, 8, 128], BF16)
    BDx = sb("BDx", [128, 8, 128], BF16)
    gcol = sb("gcol", [128, 8])
    bmcol = sb("bmcol", [128, 16])
    cwcol = sb("cwcol", [128, 4, 8])
    cbcol = sb("cbcol", [128, 8])
    bacol = sb("bacol", [128, 8])
    bxcol = sb("bxcol", [128, 8])
    lamcol = sb("lamcol", [128, 8])
    kap = sb("kap", [128, 8])
    kap2 = sb("kap2", [128, 8])
    fg_bc = sb("fg_bc", [128, 1024])
    gkv_bc = sb("gkv_bc", [128, 128])
    lng_bc = sb("lng_bc", [128, 64])
    lnb_bc = sb("lnb_bc", [128, 64])
    A_all = sb("A_all", [32, 16, 128], BF16)
    Bcol_hi = sb("Bcol_hi", [32, 16])
    Bcol_lo = sb("Bcol_lo", [32, 16])
    Bcol = sb("Bcol", [32, 16])
    e0 = sb("e0", [32, 1]); e12 = sb("e12", [32, 1]); e34 = sb("e34", [32, 1])
    e13 = sb("e13", [32, 1]); e24 = sb("e24", [32, 1]); etmp = sb("etmp", [32, 1])
    cold = sb("cold", [32, 16])
    Bwork = [sb("Bwork%d" % g, [32, 512], BF16) for g in range(4)]
    BBwork = [Buf("Bwork%d" % g) for g in range(4)]
    pow2 = sb("pow2", [128, NBIS])
    nhc = sb("nhc", [128, 1]); nhc_bf = sb("nhc_bf", [128, 1], BF16)
    nb32 = sb("nb32", [128, 1])

    cT = sb("cT", [128, S], BF16); BcT = Buf("cT")
    c_tm = sb("c_tm", [128, NTS, 128], BF16); Bctm = Buf("c_tm")
    kz = [sb("kz%d" % i, [128, S], BF16) for i in range(2)]; BkT = Buf("kidxT")
    w_tm = sb("w_tm", [128, NTS, 8]); Bwtm = Buf("w_tm")
    cabs = sb("cabs", [128, 1]); cabs_b = sb("cabs_b", [128, 1]); ncabs = sb("ncabs", [128, 1], BF16)
    Bcabs = Buf("cabs")
    xnT = sb("xnT", [128, 8, 512], BF16); BxnT = Buf("xnT")
    qT = sb("qT", [128, 8, 512], BF16); BqT = Buf("qT")
    sgT = sb("sgT", [128, 8, 512], BF16); BsgT = [Buf("sgT%d" % c) for c in range(8)]
    hgT = sb("hgT", [128, 8, 512], BF16); BhgT = Buf("hgT")
    qidxT = sb("qidxT", [128, 4, 512], BF16); BqidxT = Buf("qidxT")
    wch = [sb("wch%d" % i, [128, 8, 128], BF16) for i in range(4)]
    Bwch = [Buf("wch%d" % i) for i in range(4)]
    xt = [sb("xt%d" % i, [128, 1024]) for i in range(2)]
    Bxt = [Buf("xt%d" % i) for i in range(2)]
    xs = sb("xs", [128, 1024], BF16); Bxs = Buf("xs")
    junk = sb("junk", [128, 2048], BF16); Bjunk_act = Buf("junk_act"); Bjunk_dve = Buf("junk_dve")
    st = sb("st", [128, 16]); Bst = Buf("st")
    st2 = sb("st2", [128, 16]); Bst2 = Buf("st2")
    kc = sb("kc", [128, 64]); Bkc = Buf("kc")
    kn2 = sb("kn2", [128, 128], BF16); Bkn2 = Buf("kn2")
    ub = [sb("ub%d" % k, [128, 515], BF16) for k in range(2)]; Bub = [Buf("ub%d" % k) for k in range(2)]
    ucar = sb("ucar", [128, 8, 3], BF16); Bucar = [Buf("ucar%d" % c) for c in range(8)]
    xc_s = [sb("xc%d" % k, [128, 512]) for k in range(2)]; Bxc_s = [Buf("xc%d" % k) for k in range(2)]
    xcb_s = [sb("xcb%d" % k, [128, 512], BF16) for k in range(2)]; Bxcb_s = [Buf("xcb%d" % k) for k in range(2)]
    r_s = [sb("r%d" % k, [128, 512]) for k in range(2)]; Br_s = [Buf("r%d" % k) for k in range(2)]
    i_s = [sb("i%d" % k, [128, 512]) for k in range(2)]; Bi_s = [Buf("i%d" % k) for k in range(2)]
    a_s = [sb("a%d" % k, [128, 512]) for k in range(2)]; Ba_s = [Buf("a%d" % k) for k in range(2)]
    s_s = [sb("s%d" % k, [128, 512]) for k in range(2)]; Bs_s = [Buf("s%d" % k) for k in range(2)]
    sgr_s = [sb("sgr%d" % k, [128, 512], BF16) for k in range(2)]; Bsgr_s = [Buf("sgr%d" % k) for k in range(2)]
    r_t, Br, i_t, Bi, a_t, Ba, s_t, Bs = r_s[0], Br_s[0], i_s[0], Bi_s[0], a_s[0], Ba_s[0], s_s[0], Bs_s[0]
    ptmp, Bptmp = s_s[1], Bs_s[1]
    hcar = sb("hcar", [128, 8]); Bhcar = [Buf("hcar%d" % c) for c in range(8)]
    I_t = sb("I_t", [128, 2048]); BI = Buf("I")
    rj = [sb("rj%d" % i, [128, 512]) for i in range(2)]; Brj = [Buf("rj%d" % i) for i in range(2)]
    mask = sb("mask", [128, 2048], BF16); Bmask = Buf("mask")
    nmask = sb("nmask", [128, 16, 512], BF16); Bnmask = [Buf("nmask%d" % j) for j in range(16)]
    identrep4 = sb("identrep4", [128, 512], BF16)
    SI = sb("SI", [128, 4, 512], BF16)
    iota1_full = sb("iota1_full", [128, 2048], BF16)
    ntri_rep = sb("ntri_rep", [128, 512], BF16)
    nsm_s = [sb("nsm%d" % k, [128, 1], BF16) for k in range(2)]; Bnsm_s = [Buf("nsm%d" % k) for k in range(2)]
    bis_s = [sb("bis%d" % k, [128, 8]) for k in range(2)]; Bbis_s = [Buf("bis%d" % k) for k in range(2)]
    steps = sb("steps", [128, NBIS]); steps2 = sb("steps2", [128, NBIS]); Bsteps = Buf("steps")
    qlb_s = [sb("qlb%d" % k, [128, 512], BF16) for k in range(4)]; Bqlb_s = [Buf("qlb%d" % k) for k in range(4)]
    absq = sb("absq", [128, 512], BF16); Babsq = Buf("absq")
    p_t = [sb("p%d" % i, [128, 512], BF16) for i in range(2)]; Bp = [Buf("p%d" % i) for i in range(2)]
    rs_s = [sb("rs%d" % k, [128, 512]) for k in range(2)]; Brs_s = [Buf("rs%d" % k) for k in range(2)]
    oTn_s = [sb("oTn%d" % k, [128, 512], BF16) for k in range(2)]; BoTn_s = [Buf("oTn%d" % k) for k in range(2)]
    ga_t, Bga = r_t, Br
    gr_t, Bgr = i_t, Bi
    m1_t, Bm1 = a_t, Ba
    m2_t, Bm2 = s_t, Bs
    Bres = [Buf("res0"), Buf("res1")]

    ps = [nc.psum_tensor("ps%d" % i, [128, 512], F32).__enter__() for i in range(8)]
    Bps = [Buf("ps%d" % i) for i in range(8)]

    def psbf(i):
        return ps[i][:, :].bitcast(BF16)

    op = S_.op

    with nc.allow_non_contiguous_dma(reason="one-time small parameter layouts"):
        for ci, (src, col0, wd) in enumerate(chunk_src):
            S_.dma("pool", "cv", wbf[ci].rearrange("p (k j) -> p k j", k=8)[:, :, 0:wd],
                   src.rearrange("(k p) n -> p k n", p=128)[:, :, col0:col0 + wd], writes=[Bwbf[ci]])
        for b_ in Bwbf:
            b_.w = ("dma:cv", S_.dsem["dma:cv"][1])
        S_.dma("pool", "cs", wout_sb[:, :, :], w_out.rearrange("(k p) n -> p k n", p=128), writes=[Bconst])
        op("pool", lambda e: e.memset(wuvP[:, :, :], 0.0), writes=[Bconst])
        for par in range(2):
            S_.dma("pool", "cs", wuvP[:, :, :].rearrange("c (hp two) d -> c hp two d", two=2)[:, :, par, par * 64:par * 64 + 64],
                   w_uv.rearrange("(hp two) c d -> c hp two d", two=2)[:, :, par, :], reads=[Bconst], writes=[Bconst])
        S_.dma("pool", "cs", wuk_nat[:, :, :], w_uk.rearrange("h c d -> c h d"), writes=[Bconst])
        op("pool", lambda e: e.memset(BDa[:, :, :], 0.0), writes=[Bconst])
        op("pool", lambda e: e.memset(BDx[:, :, :], 0.0), writes=[Bconst])
        for (bd, wsrc) in ((BDa, w_rg_a), (BDx, w_rg_x)):
            for par in range(2):
                S_.dma("pool", "cs", bd[par * 64:par * 64 + 64, :, par * 64:par * 64 + 64],
                       wsrc.rearrange("(p two) i j -> two i p j", two=2)[par], reads=[Bconst], writes=[Bconst])
        for (dst, src, n) in ((gcol, norm_gain, 8), (bmcol, b_merge, 16), (cbcol, conv_b, 8), (bacol, b_rg_a, 8),
                              (bxcol, b_rg_x, 8), (lamcol, lru_lambda, 8)):
            S_.dma("sp", "cs2", dst[:, :], src.rearrange("(c p) -> p c", p=128), writes=[Bconst])
        S_.dma("sp", "cs2", cwcol[:, :, :], conv_w.rearrange("k (c p) -> p k c", p=128), writes=[Bconst])
        S_.dma("sp", "cs2", fg_bc[:, :], final_norm_gain.partition_broadcast(128), writes=[Bconst])
        S_.dma("sp", "cs2", gkv_bc[:, :], kv_norm_gain.partition_broadcast(128), writes=[Bconst])
        S_.dma("sp", "cs2", lng_bc[:, :], idx_ln_gain.partition_broadcast(128), writes=[Bconst])
        S_.dma("sp", "cs2", lnb_bc[:, :], idx_ln_bias.partition_broadcast(128), writes=[Bconst])

    for par in range(2):
        op("pool", lambda e, par=par: e.memset(kz[par][:, :], 0.0), writes=[BkT])
    op("pool", lambda e: e.iota(iota_row[:, :], [[1, 128]], base=0, channel_multiplier=0,
                                allow_small_or_imprecise_dtypes=True), writes=[Bconst])
    op("pool", lambda e: e.iota(pidx[:, :], [[0, 1]], base=0, channel_multiplier=1,
                                allow_small_or_imprecise_dtypes=True), reads=[Bconst], writes=[Bconst])
    op("dve", lambda e: e.tensor_scalar(out=ident[:, :], in0=iota_row[:, :], scalar1=pidx[:, 0:1], scalar2=None,
                                        op0=ALU.is_equal), reads=[Bconst], writes=[Bconst, Bident])
    op("dve", lambda e: e.tensor_scalar(out=triT[:, :], in0=iota_row[:, :], scalar1=pidx[:, 0:1], scalar2=None,
                                        op0=ALU.is_ge), reads=[Bconst], writes=[Bconst])
    op("dve", lambda e: e.tensor_scalar(out=causal_neg[:, :], in0=iota_row[:, :], scalar1=pidx[:, 0:1], scalar2=-1e30,
                                        op0=ALU.is_gt, op1=ALU.mult), reads=[Bconst], writes=[Bconst])
    op("dve", lambda e: e.memset(ones_bf[:, :], 1.0), reads=[Bconst], writes=[Bconst])
    for k in range(NBIS):
        op("dve", lambda e, k=k: e.memset(pow2[:, k:k + 1], 2.0 ** -(k + 1)), reads=[Bconst], writes=[Bconst])
    op("dve", lambda e: e.tensor_reduce(out=nhc[:, 0:1], in_=gkv_bc[:, :], axis=AX.X, op=ALU.max, apply_absolute_value=True),
       reads=[Bconst], writes=[Bconst])
    op("dve", lambda e: e.tensor_scalar(out=nhc[:, 0:1], in0=nhc[:, 0:1], scalar1=-0.5 * (128.0 ** 0.5), scalar2=None, op0=ALU.mult),
       reads=[Bconst], writes=[Bconst])
    op("dve", lambda e: e.tensor_copy(out=nhc_bf[:, 0:1], in_=nhc[:, 0:1]), reads=[Bconst], writes=[Bconst])
    op("dve", lambda e: e.memset(nb32[:, :], -32768.0), reads=[Bconst], writes=[Bconst])
    op("act", lambda e: e.activation(out=kap[:, :], in_=lamcol[:, :], func=AF.Exp, scale=-1.0), reads=[Bconst], writes=[Bconst])
    op("act", lambda e: e.activation(out=kap[:, :], in_=kap[:, :], func=AF.Ln, bias=1.0, scale=1.0), reads=[Bconst], writes=[Bconst])
    op("dve", lambda e: e.tensor_scalar(out=kap2[:, :], in0=kap[:, :], scalar1=-16.0, scalar2=None, op0=ALU.mult), reads=[Bconst], writes=[Bconst])
    op("dve", lambda e: e.tensor_scalar(out=kap[:, :], in0=kap[:, :], scalar1=-8.0, scalar2=None, op0=ALU.mult), reads=[Bconst], writes=[Bconst])
    for pr in range(8):
        op("pe", lambda e, pr=pr: e.transpose(out=psbf(0)[:, pr * 128:(pr + 1) * 128],
                                              in_=wuk_nat[:, 2 * pr:2 * pr + 2, :].rearrange("c h d -> c (h d)"),
                                              identity=ident[:, :]), reads=[Bconst], writes=[Bps[0]])
    op("dve", lambda e: e.memset(wukTz[:, :, :], 0.0), reads=[Bconst], writes=[Bconst])
    for par in range(2):
        op("dve", lambda e, par=par: e.tensor_copy(
            out=wukTz[par * 64:par * 64 + 64, :, :].rearrange("p (pr two) c -> p pr two c", two=2)[:, :, par, :],
            in_=psbf(0)[par * 64:par * 64 + 64, :].rearrange("p (pr c) -> p pr c", c=128)), reads=[Bps[0], Bconst], writes=[Bconst])
    def sel(dst, ks):
        for n, k in enumerate(ks):
            tgt = dst if n == 0 else etmp
            op("dve", lambda e, k=k, tgt=tgt: e.tensor_scalar(out=tgt[:, :], in0=pidx[0:32, :], scalar1=float(k), scalar2=None,
                                                             op0=ALU.is_equal), reads=[Bconst], writes=[Bconst])
            if n > 0:
                op("dve", lambda e: e.tensor_tensor(out=dst[:, :], in0=dst[:, :], in1=etmp[:, :], op=ALU.add), reads=[Bconst], writes=[Bconst])
    sel(e0, [0]); sel(e12, [1, 2]); sel(e34, [3, 4]); sel(e13, [1, 3]); sel(e24, [2, 4])
    slopes = [2.0 ** (-8.0 * (h + 1) / 16.0) for h in range(16)]
    for h in range(16):
        hi, lo = _bf16_split(slopes[h])
        op("dve", lambda e, h=h, hi=hi: e.memset(Bcol_hi[:, h:h + 1], hi), reads=[Bconst], writes=[Bconst])
        op("dve", lambda e, h=h, lo=lo: e.memset(Bcol_lo[:, h:h + 1], lo), reads=[Bconst], writes=[Bconst])
    op("dve", lambda e: e.tensor_scalar(out=Bcol[:, :], in0=Bcol_hi[:, :], scalar1=e13[:, 0:1], scalar2=None, op0=ALU.mult), reads=[Bconst], writes=[Bconst])
    op("dve", lambda e: e.scalar_tensor_tensor(out=Bcol[:, :], in0=Bcol_lo[:, :], scalar=e24[:, 0:1], in1=Bcol[:, :],
                                               op0=ALU.mult, op1=ALU.add), reads=[Bconst], writes=[Bconst])
    for g in range(4):
        for hl in range(4):
            h = 4 * g + hl
            op("dve", lambda e, g=g, hl=hl, h=h: e.tensor_scalar(out=Bwork[g][:, hl * 128:(hl + 1) * 128], in0=ones_bf[0:32, :],
                                                                  scalar1=Bcol[:, h:h + 1], scalar2=None, op0=ALU.mult),
               reads=[Bconst], writes=[BBwork[g]])
    for dl in range(16):
        op("dve", lambda e, dl=dl: e.tensor_scalar(out=cold[:, dl:dl + 1], in0=e12[:, :], scalar1=128.0 * dl, scalar2=None,
                                                   op0=ALU.mult), reads=[Bconst], writes=[Bconst])
        op("dve", lambda e, dl=dl: e.tensor_tensor(out=cold[:, dl:dl + 1], in0=cold[:, dl:dl + 1], in1=e0[:, :], op=ALU.add), reads=[Bconst], writes=[Bconst])
        op("dve", lambda e, dl=dl: e.tensor_scalar(out=A_all[:, dl, :], in0=iota_row[0:32, :], scalar1=e34[:, 0:1], scalar2=cold[:, dl:dl + 1],
                                                   op0=ALU.mult, op1=ALU.add), reads=[Bconst], writes=[Bconst])

    for r4 in range(4):
        op("dve", lambda e, r4=r4: e.tensor_copy(out=identrep4[:, r4 * 128:(r4 + 1) * 128], in_=ident[:, :]), reads=[Bconst], writes=[Bconst])
        op("dve", lambda e, r4=r4: e.tensor_scalar(out=ntri_rep[:, r4 * 128:(r4 + 1) * 128], in0=triT[:, :], scalar1=-1.0, scalar2=32768.0,
                                                   op0=ALU.add, op1=ALU.mult), reads=[Bconst], writes=[Bconst])
    for h in range(16):
        op("dve", lambda e, h=h: e.tensor_scalar(out=SI[:, h // 4, (h % 4) * 128:(h % 4 + 1) * 128], in0=ident[:, :], scalar1=slopes[h], scalar2=None,
                                                 op0=ALU.mult), reads=[Bconst], writes=[Bconst])
    op("pool", lambda e: e.iota(iota1_full[:, :], [[1, 2048]], base=1, channel_multiplier=0,
                                allow_small_or_imprecise_dtypes=True), reads=[Bconst], writes=[Bconst])

    wslot = [0]

    def load_chunk(ci):
        j = wslot[0] % 4
        wslot[0] += 1
        wd = chunk_src[ci][2]
        if wd == 128:
            S_.dma("sp", "wl%d" % j, wch[j][:, :, :].rearrange("p k j -> p (k j)"), wbf[ci], reads=[Bwbf[ci]], writes=[Bwch[j]])
        else:
            S_.dma("sp", "wl%d" % j, wch[j][:, :, 0:wd], wbf[ci].rearrange("p (k j) -> p k j", k=8)[:, :, 0:wd],
                   reads=[Bwbf[ci]], writes=[Bwch[j]])
        return j

    def proj_fm(j, wd, bank, rhs_t, rhs_b):
        def f(e):
            ins = None
            for k in range(8):
                ins = e.matmul(ps[bank][0:wd, :], lhsT=wch[j][:, k, 0:wd], rhs=rhs_t[:, k, :], start=(k == 0), stop=(k == 7))
            return ins
        op("pe", f, reads=[Bwch[j], rhs_b], writes=[Bps[bank]])

    def rstd_from_ss(stt, Bstt, col_ss, col_out, n):
        op("act", lambda e: e.activation(out=stt[:, col_out:col_out + 1], in_=stt[:, col_ss:col_ss + 1], func=AF.Sqrt,
                                         bias=EPS, scale=1.0 / n), reads=[Bstt], writes=[Bstt])
        op("dve", lambda e: e.reciprocal(out=stt[:, col_out:col_out + 1], in_=stt[:, col_out:col_out + 1]), reads=[Bstt], writes=[Bstt])

    bankrr = [0]

    def next_bank():
        b = (0, 1, 4, 5)[bankrr[0] % 4]
        bankrr[0] += 1
        return b

    dbg = {}

    def chk(n):
        if stop is not None and stop == n:
            raise _Stop()

    try:
        chk(0)
        for sq in range(nseq):
            for tb in range(NBLK):
                t0 = tb * 512
                for i in range(4):
                    xb = i % 2
                    S_.dma("sp", "xl%d" % xb, xt[xb][:, :], x[sq, t0 + i * 128:t0 + (i + 1) * 128, :], writes=[Bxt[xb]])
                    op("act", lambda e, xb=xb: e.activation(out=junk[:, 0:1024], in_=xt[xb][:, :], func=AF.Square, accum_out=st[:, 0:1]),
                       reads=[Bxt[xb]], writes=[Bjunk_act, Bst])
                    rstd_from_ss(st, Bst, 0, 1, 1024.0)
                    op("dve", lambda e, xb=xb: e.tensor_scalar(out=xs[:, :], in0=xt[xb][:, :], scalar1=st[:, 1:2], scalar2=None, op0=ALU.mult),
                       reads=[Bxt[xb], Bst], writes=[Bxs])
                    def ftr(e):
                        ins = None
                        for k in range(8):
                            ins = e.transpose(out=psbf(2)[:, k * 128:(k + 1) * 128], in_=xs[:, k * 128:(k + 1) * 128], identity=ident[:, :])
                        return ins
                    op("pe", ftr, reads=[Bxs, Bident], writes=[Bps[2]])
                    for k in range(8):
                        op("dve", lambda e, k=k, i=i: e.tensor_scalar(out=xnT[:, k, i * 128:(i + 1) * 128], in0=psbf(2)[:, k * 128:(k + 1) * 128],
                                                                       scalar1=gcol[:, k:k + 1], scalar2=None, op0=ALU.mult),
                           reads=[Bps[2], Bconst], writes=[BxnT])

                chk(1)
                def b_steps():
                    for c in range(8):
                        j = load_chunk(CH["q"][c]); bk = next_bank()
                        proj_fm(j, 128, bk, xnT, BxnT)
                        op("act", lambda e, c=c, bk=bk: e.copy(out=qT[:, c, :], in_=ps[bk][:, :]), reads=[Bps[bk]], writes=[BqT])
                        yield
                    for c in range(4):
                        j = load_chunk(CH["qidx"][c]); bk = next_bank()
                        proj_fm(j, 128, bk, xnT, BxnT)
                        op("act", lambda e, c=c, bk=bk: e.mul(out=qidxT[:, c, :], in_=ps[bk][:, :], mul=0.125),
                           reads=[Bps[bk]], writes=[BqidxT])
                        yield
                    j = load_chunk(CH["ckv"][0])
                    for i in range(4):
                        gt = tb * 4 + i
                        def fc(e, i=i, j=j):
                            ins = None
                            for k in range(8):
                                ins = e.matmul(ps[2][:, 0:128], lhsT=xnT[:, k, i * 128:(i + 1) * 128], rhs=wch[j][:, k, :], start=(k == 0), stop=(k == 7))
                            return ins
                        op("pe", fc, reads=[Bwch[j], BxnT], writes=[Bps[2]])
                        op("act", lambda e: e.activation(out=junk[:, 0:128], in_=ps[2][:, 0:128], func=AF.Square, accum_out=st2[:, 0:1]),
                           reads=[Bps[2]], writes=[Bjunk_act, Bst2])
                        rstd_from_ss(st2, Bst2, 0, 1, 128.0)
                        op("dve", lambda e, gt=gt: e.scalar_tensor_tensor(out=c_tm[:, gt, :], in0=ps[2][:, 0:128], scalar=st2[:, 1:2], in1=gkv_bc[:, :],
                                                                           op0=ALU.mult, op1=ALU.mult), reads=[Bps[2], Bst2, Bconst], writes=[Bctm])
                        op("pe", lambda e, gt=gt: e.transpose(out=psbf(3)[:, 0:128], in_=c_tm[:, gt, :], identity=ident[:, :]),
                           reads=[Bctm, Bident], writes=[Bps[3]])
                        op("dve", lambda e, gt=gt: e.tensor_copy(out=cT[:, gt * 128:(gt + 1) * 128], in_=psbf(3)[:, 0:128]), reads=[Bps[3]], writes=[BcT])
                    yield
                    j = load_chunk(CH["kw"][0])
                    for i in range(4):
                        gt = tb * 4 + i
                        def fk(e, i=i, j=j):
                            ins = None
                            for k in range(8):
                                ins = e.matmul(ps[2][:, 0:72], lhsT=xnT[:, k, i * 128:(i + 1) * 128], rhs=wch[j][:, k, 0:72], start=(k == 0), stop=(k == 7))
                            return ins
                        op("pe", fk, reads=[Bwch[j], BxnT], writes=[Bps[2]])
                        op("dve", lambda e, gt=gt: e.tensor_scalar(out=w_tm[:, gt, :], in0=ps[2][:, 64:72], scalar1=8.0 ** -0.5, scalar2=None, op0=ALU.mult),
                           reads=[Bps[2]], writes=[Bwtm])
                        op("dve", lambda e: e.tensor_reduce(out=st2[:, 2:3], in_=ps[2][:, 0:64], axis=AX.X, op=ALU.add), reads=[Bps[2]], writes=[Bst2])
                        op("dve", lambda e: e.tensor_scalar(out=st2[:, 2:3], in0=st2[:, 2:3], scalar1=-1.0 / 64.0, scalar2=None, op0=ALU.mult), reads=[Bst2], writes=[Bst2])
                        op("dve", lambda e: e.tensor_scalar(out=kc[:, :], in0=ps[2][:, 0:64], scalar1=st2[:, 2:3], scalar2=None, op0=ALU.add),
                           reads=[Bps[2], Bst2], writes=[Bkc])
                        op("act", lambda e: e.activation(out=junk[:, 0:64], in_=kc[:, :], func=AF.Square, accum_out=st2[:, 3:4]),
                           reads=[Bkc], writes=[Bjunk_act, Bst2])
                        rstd_from_ss(st2, Bst2, 3, 4, 64.0)
                        op("dve", lambda e: e.scalar_tensor_tensor(out=kc[:, :], in0=kc[:, :], scalar=st2[:, 4:5], in1=lng_bc[:, :], op0=ALU.mult, op1=ALU.mult),
                           reads=[Bkc, Bst2, Bconst], writes=[Bkc])
                        op("dve", lambda e: e.tensor_tensor(out=kn2[:, 0:64], in0=kc[:, :], in1=lnb_bc[:, :], op=ALU.add), reads=[Bkc, Bconst], writes=[Bkn2])
                        op("dve", lambda e: e.tensor_copy(out=kn2[:, 64:128], in_=kn2[:, 0:64]), reads=[Bkn2], writes=[Bkn2])
                        op("pe", lambda e: e.transpose(out=psbf(3)[:, 0:128], in_=kn2[:, :], identity=ident[:, :]), reads=[Bkn2, Bident], writes=[Bps[3]])
                        for par in range(2):
                            op("dve", lambda e, gt=gt, par=par: e.tensor_copy(out=kz[par][par * 64:par * 64 + 64, gt * 128:(gt + 1) * 128],
                                                                               in_=psbf(3)[par * 64:par * 64 + 64, 0:128]), reads=[Bps[3]], writes=[BkT])
                    yield
                def b_gattn():
                    for c in range(8):
                        j = load_chunk(CH["gattn"][c]); bk = next_bank()
                        proj_fm(j, 128, bk, xnT, BxnT)
                        op("act", lambda e, c=c, bk=bk: e.activation(out=sgT[:, c, :], in_=ps[bk][:, :], func=AF.Silu), reads=[Bps[bk]], writes=[BsgT[c]])

                chk(2)
                def topk_gen(i, gt):
                    ns = gt + 1
                    ncols = ns * 128
                    bis, Bbis, nsm, Bnsm = bis_s[i % 2], Bbis_s[i % 2], nsm_s[i % 2], Bnsm_s[i % 2]
                    nsb = (ncols + 511) // 512
                    for sbk in range(nsb):
                        c0 = sbk * 512
                        cw = min(512, ncols - c0)
                        for jh in range(8):
                            par = jh % 2
                            bk = 6 + (jh % 2)
                            op("pe", lambda e, jh=jh, par=par, bk=bk, c0=c0, cw=cw: e.matmul(
                                ps[bk][:, 0:cw], lhsT=qidxT[:, jh // 2, i * 128:(i + 1) * 128],
                                rhs=kz[par][:, c0:c0 + cw], start=True, stop=True),
                               reads=[BqidxT, BkT], writes=[Bps[bk]])
                            rb = jh % 2
                            op("act", lambda e, bk=bk, rb=rb, cw=cw: e.activation(out=rj[rb][:, 0:cw], in_=ps[bk][:, 0:cw], func=AF.Relu),
                               reads=[Bps[bk]], writes=[Brj[rb]])
                            if jh == 0:
                                op("dve", lambda e, rb=rb, c0=c0, cw=cw: e.tensor_scalar(
                                    out=I_t[:, c0:c0 + cw], in0=rj[rb][:, 0:cw], scalar1=w_tm[:, gt, 0:1], scalar2=None, op0=ALU.mult),
                                   reads=[Brj[rb], Bwtm], writes=[BI, Bres[0], Bres[1]])
                            else:
                                op("dve", lambda e, rb=rb, c0=c0, cw=cw, jh=jh: e.scalar_tensor_tensor(
                                    out=I_t[:, c0:c0 + cw], in0=rj[rb][:, 0:cw], scalar=w_tm[:, gt, jh:jh + 1], in1=I_t[:, c0:c0 + cw],
                                    op0=ALU.mult, op1=ALU.add), reads=[Brj[rb], Bwtm, BI], writes=[BI])
                    yield
                    op("dve", lambda e: e.tensor_reduce(out=bis[:, 0:1], in_=I_t[:, 0:ncols], axis=AX.X, op=ALU.max,
                                                        apply_absolute_value=True), reads=[BI], writes=[Bbis])
                    op("dve", lambda e: e.tensor_scalar(out=bis[:, 0:1], in0=bis[:, 0:1], scalar1=1.01, scalar2=1e-6, op0=ALU.mult, op1=ALU.add),
                       reads=[Bbis], writes=[Bbis])
                    op("dve", lambda e: e.tensor_scalar(out=steps[:, :], in0=pow2[:, :], scalar1=bis[:, 0:1], scalar2=None, op0=ALU.mult),
                       reads=[Bbis, Bconst], writes=[Bsteps])
                    op("dve", lambda e: e.tensor_scalar(out=steps2[:, :], in0=steps[:, :], scalar1=2.0, scalar2=None, op0=ALU.mult),
                       reads=[Bsteps], writes=[Bsteps])
                    op("dve", lambda e: e.tensor_tensor(out=I_t[:, gt * 128:(gt + 1) * 128], in0=I_t[:, gt * 128:(gt + 1) * 128],
                                                        in1=causal_neg[:, :], op=ALU.add), reads=[BI, Bconst, Bbis], writes=[BI])
                    op("dve", lambda e: e.memset(bis[:, 1:2], 0.0), reads=[Bbis], writes=[Bbis])
                    for kb in range(NBIS):
                        op("dve", lambda e: e.tensor_scalar(out=mask[:, 0:ncols], in0=I_t[:, 0:ncols], scalar1=bis[:, 1:2], scalar2=None,
                                                            op0=ALU.is_ge, op1=ALU.add, accum_out=bis[:, 2:3]),
                           reads=[BI, Bbis], writes=[Bmask, Bbis])
                        op("dve", lambda e, kb=kb: e.tensor_scalar(out=bis[:, 3:4], in0=bis[:, 2:3], scalar1=TOPK - 0.5, scalar2=steps2[:, kb:kb + 1],
                                                                   op0=ALU.is_ge, op1=ALU.mult), reads=[Bbis, Bsteps], writes=[Bbis])
                        op("dve", lambda e, kb=kb: e.scalar_tensor_tensor(out=bis[:, 1:2], in0=bis[:, 1:2], scalar=steps[:, kb:kb + 1], in1=bis[:, 3:4],
                                                                          op0=ALU.subtract, op1=ALU.add), reads=[Bbis, Bsteps], writes=[Bbis])
                        if kb in (3, 7):
                            yield
                    op("dve", lambda e: e.tensor_scalar(out=mask[:, 0:ncols], in0=I_t[:, 0:ncols], scalar1=bis[:, 1:2], scalar2=None,
                                                        op0=ALU.is_ge), reads=[BI, Bbis], writes=[Bmask])
                    op("dve", lambda e: e.scalar_tensor_tensor(out=I_t[:, 0:ncols], in0=I_t[:, 0:ncols], scalar=bis[:, 1:2],
                                                               in1=iota1_full[:, 0:ncols], op0=ALU.is_ge, op1=ALU.mult),
                       reads=[BI, Bbis, Bconst], writes=[BI])
                    op("dve", lambda e: e.tensor_reduce(out=bis[:, 4:5], in_=I_t[:, 0:ncols], axis=AX.X, op=ALU.max), reads=[BI], writes=[Bbis])
                    op("dve", lambda e: e.tensor_scalar(out=nsm[:, :], in0=bis[:, 4:5], scalar1=-1.0, scalar2=1.0, op0=ALU.mult, op1=ALU.add),
                       reads=[Bbis], writes=[Bnsm])

                def nmask_build(i, gt):
                    for jj in range(gt + 1):
                        mbk = 6 + (jj % 2)
                        op("pe", lambda e, jj=jj, mbk=mbk: e.matmul(ps[mbk][:, :], lhsT=mask[:, jj * 128:(jj + 1) * 128], rhs=identrep4[:, :],
                                                                     start=True, stop=True), reads=[Bmask, Bconst], writes=[Bps[mbk]])
                        op("act", lambda e, jj=jj, mbk=mbk: e.activation(out=nmask[:, jj, :], in_=ps[mbk][:, :], func=AF.Identity,
                                                                          bias=nb32[:, 0:1], scale=32768.0), reads=[Bps[mbk], Bconst], writes=[Bnmask[jj]])

                def rnn_steps(c, k):
                    xc, Bxc, xcb, Bxcb = xc_s[k], Bxc_s[k], xcb_s[k], Bxcb_s[k]
                    rr, Brr, ii, Bii, aa, Baa, ss, Bss = r_s[k], Br_s[k], i_s[k], Bi_s[k], a_s[k], Ba_s[k], s_s[k], Bs_s[k]
                    sg, Bsg, uu, Buu = sgr_s[k], Bsgr_s[k], ub[k], Bub[k]
                    j = load_chunk(CH["xrnn"][c]); bk = next_bank()
                    proj_fm(j, 128, bk, xnT, BxnT)
                    if tb == 0:
                        op("pool", lambda e: e.memset(uu[:, 0:3], 0.0), reads=[], writes=[Buu])
                        op("dve", lambda e: e.memset(hcar[:, c:c + 1], 0.0), reads=[], writes=[Bhcar[c]])
                    else:
                        op("pool", lambda e: e.tensor_copy(out=uu[:, 0:3], in_=ucar[:, c, :]), reads=[Bucar[c]], writes=[Buu])
                    op("act", lambda e: e.copy(out=uu[:, 3:515], in_=ps[bk][:, :]), reads=[Bps[bk]], writes=[Buu])
                    yield
                    op("dve", lambda e: e.tensor_scalar(out=xc[:, :], in0=uu[:, 3:515], scalar1=cwcol[:, 3, c:c + 1], scalar2=cbcol[:, c:c + 1],
                                                        op0=ALU.mult, op1=ALU.add), reads=[Buu, Bconst], writes=[Bxc])
                    for kk in range(3):
                        op("dve", lambda e, kk=kk: e.scalar_tensor_tensor(out=xc[:, :], in0=uu[:, kk:kk + 512], scalar=cwcol[:, kk, c:c + 1],
                                                                          in1=xc[:, :], op0=ALU.mult, op1=ALU.add),
                           reads=[Buu, Bconst, Bxc], writes=[Bxc])
                    op("pool", lambda e: e.tensor_copy(out=ucar[:, c, :], in_=uu[:, 512:515]), reads=[Buu], writes=[Bucar[c]])
                    op("pool", lambda e: e.tensor_copy(out=xcb[:, :], in_=xc[:, :]), reads=[Bxc], writes=[Bxcb])
                    yield
                    bk1 = next_bank()
                    op("pe", lambda e: e.matmul(ps[bk1][:, :], lhsT=BDa[:, c, :], rhs=xcb[:, :], start=True, stop=True),
                       reads=[Bxcb, Bconst], writes=[Bps[bk1]])
                    op("act", lambda e: e.activation(out=rr[:, :], in_=ps[bk1][:, :], func=AF.Sigmoid, bias=bacol[:, c:c + 1], scale=1.0),
                       reads=[Bps[bk1], Bconst], writes=[Brr])
                    bk2 = next_bank()
                    op("pe", lambda e: e.matmul(ps[bk2][:, :], lhsT=BDx[:, c, :], rhs=xcb[:, :], start=True, stop=True),
                       reads=[Bxcb, Bconst], writes=[Bps[bk2]])
                    op("act", lambda e: e.activation(out=ii[:, :], in_=ps[bk2][:, :], func=AF.Sigmoid, bias=bxcol[:, c:c + 1], scale=1.0),
                       reads=[Bps[bk2], Bconst], writes=[Bii])
                    yield
                    op("act", lambda e: e.activation(out=aa[:, :], in_=rr[:, :], func=AF.Exp, scale=kap[:, c:c + 1]), reads=[Brr, Bconst], writes=[Baa])
                    op("act", lambda e: e.activation(out=ss[:, :], in_=rr[:, :], func=AF.Exp, scale=kap2[:, c:c + 1]), reads=[Brr, Bconst], writes=[Bss])
                    yield
                    op("act", lambda e: e.activation(out=ss[:, :], in_=ss[:, :], func=AF.Sqrt, bias=1.0, scale=-1.0), reads=[Bss], writes=[Bss])
                    op("dve", lambda e: e.tensor_tensor(out=ii[:, :], in0=ii[:, :], in1=xc[:, :], op=ALU.mult), reads=[Bii, Bxc], writes=[Bii])
                    yield
                    op("dve", lambda e: e.tensor_tensor(out=ii[:, :], in0=ii[:, :], in1=ss[:, :], op=ALU.mult), reads=[Bii, Bss], writes=[Bii])
                    op("dve", lambda e: e.tensor_tensor_scan(out=rr[:, :], data0=aa[:, :], data1=ii[:, :], initial=hcar[:, c:c + 1],
                                                             op0=ALU.mult, op1=ALU.add), reads=[Baa, Bii, Bhcar[c]], writes=[Brr])
                    op("dve", lambda e: e.tensor_copy(out=hcar[:, c:c + 1], in_=rr[:, 511:512]), reads=[Brr], writes=[Bhcar[c]])
                    j2 = load_chunk(CH["grnn"][c]); bk3 = next_bank()
                    proj_fm(j2, 128, bk3, xnT, BxnT)
                    yield
                    op("act", lambda e: e.activation(out=sg[:, :], in_=ps[bk3][:, :], func=AF.Silu), reads=[Bps[bk3]], writes=[Bsg])
                    op("dve", lambda e: e.tensor_tensor(out=hgT[:, c, :], in0=rr[:, :], in1=sg[:, :], op=ALU.mult), reads=[Brr, Bsg], writes=[BhgT])

                def c_rounds():
                    for c in range(0, 8, 2):
                        g0, g1 = rnn_steps(c, 0), rnn_steps(c + 1, 1)
                        alive = [g0, g1]
                        while alive:
                            for g in list(alive):
                                try:
                                    next(g)
                                except StopIteration:
                                    alive.remove(g)
                            yield

                tk0 = topk_gen(0, tb * 4) if tb >= 1 else None
                bg, cg = b_steps(), c_rounds()
                b_done, c_done, rnd = False, False, 0
                _END = object()
                while not (b_done and c_done):
                    if not b_done:
                        b_done = next(bg, _END) is _END
                    if not c_done:
                        c_done = next(cg, _END) is _END
                    rnd += 1
                    if b_done and tk0 is not None and rnd % 6 == 0:
                        next(tk0, None)
                b_gattn()
                if tk0 is not None:
                    for _ in tk0:
                        pass
                    nmask_build(0, tb * 4)
                chk(3)
                def attn_prologue(i, gt):
                    nsm, Bnsm = nsm_s[i % 2], Bnsm_s[i % 2]
                    if gt < 2:
                        op("dve", lambda e: e.tensor_scalar(out=nsm[:, :], in0=pidx[:, :], scalar1=-1.0, scalar2=-128.0 * gt, op0=ALU.mult, op1=ALU.add),
                           reads=[Bconst], writes=[Bnsm])
                    for hg in range(4):
                        def fql(e, hg=hg):
                            ins = None
                            for hl in range(4):
                                h = 4 * hg + hl
                                ins = e.matmul(ps[6][:, hl * 128:(hl + 1) * 128], lhsT=wukTz[:, h, :],
                                               rhs=qT[:, h // 2, i * 128:(i + 1) * 128], start=True, stop=True)
                            return ins
                        op("pe", fql, reads=[BqT, Bconst], writes=[Bps[6]])
                        op("act", lambda e, hg=hg: e.mul(out=qlb_s[hg][:, :], in_=ps[6][:, :], mul=0.125), reads=[Bps[6]], writes=[Bqlb_s[hg]])
                        op("act", lambda e: e.activation(out=absq[:, :], in_=ps[6][:, :], func=AF.Square, scale=0.125), reads=[Bps[6]], writes=[Babsq])
                        def fnm(e, hg=hg):
                            e.matmul(ps[4][0:1, :], lhsT=nhc_bf[:, 0:1], rhs=absq[:, :], start=True, stop=False)
                            return e.matmul(ps[4][0:1, :], lhsT=nsm[:, 0:1], rhs=SI[:, hg, :], start=False, stop=True)
                        op("pe", fnm, reads=[Babsq, Bnsm, Bconst], writes=[Bps[4]])
                        op("act", lambda e, hg=hg: e.activation(out=Bwork[hg][0:1, :], in_=ps[4][0:1, :], func=AF.Identity, bias=nhc[0:1, 0:1], scale=1.0),
                           reads=[Bps[4], Bconst], writes=[BBwork[hg]])

                def attn_hg(i, gt, hg):
                    ns = gt + 1
                    qlb, Bqlb = qlb_s[hg], Bqlb_s[hg]
                    def flg_exp(js):
                        lb = js % 2
                        if gt >= 2:
                            mrhs, mbuf = nmask[:, js, :], Bnmask[js]
                        elif js == gt:
                            mrhs, mbuf = ntri_rep[:, :], Bconst
                        else:
                            mrhs, mbuf = None, None
                        def flg(e):
                            e.matmul(ps[lb][:, :], lhsT=cT[:, js * 128:(js + 1) * 128], rhs=qlb[:, :], start=True, stop=False)
                            if mrhs is None:
                                return e.matmul(ps[lb][:, :], lhsT=A_all[:, js, :], rhs=Bwork[hg][:, :], start=False, stop=True)
                            e.matmul(ps[lb][:, :], lhsT=A_all[:, js, :], rhs=Bwork[hg][:, :], start=False, stop=False)
                            return e.matmul(ps[lb][:, :], lhsT=ident[:, :], rhs=mrhs, start=False, stop=True)
                        op("pe", flg, reads=[BcT, Bqlb, BBwork[hg], Bconst] + ([mbuf] if mbuf is not None else []), writes=[Bps[lb]])
                        op("act", lambda e: e.activation(out=p_t[lb][:, :], in_=ps[lb][:, :], func=AF.Exp), reads=[Bps[lb]], writes=[Bp[lb]])
                    bo, bs_ = (2, 3) if hg % 2 == 0 else (4, 5)
                    def fpv_(js):
                        lb = js % 2
                        def fpv(e):
                            e.matmul(ps[bo][:, :], lhsT=c_tm[:, js, :], rhs=p_t[lb][:, :], start=(js == 0), stop=(js == ns - 1))
                            return e.matmul(ps[bs_][:, :], lhsT=ones_bf[:, :], rhs=p_t[lb][:, :], start=(js == 0), stop=(js == ns - 1))
                        op("pe", fpv, reads=[Bctm, Bp[lb], Bconst], writes=[Bps[bo], Bps[bs_]])
                    flg_exp(0)
                    for js in range(ns):
                        if js + 1 < ns:
                            flg_exp(js + 1)
                        fpv_(js)
                    rs_t, Brs, oTn, BoTn = rs_s[hg % 2], Brs_s[hg % 2], oTn_s[hg % 2], BoTn_s[hg % 2]
                    op("dve", lambda e: e.reciprocal(out=rs_t[:, :], in_=ps[bs_][:, :]), reads=[Bps[bs_]], writes=[Brs])
                    op("dve", lambda e: e.tensor_tensor(out=oTn[:, :], in0=ps[bo][:, :], in1=rs_t[:, :], op=ALU.mult), reads=[Bps[bo], Brs], writes=[BoTn])

                def attn_epi(i, hg):
                    oTn, BoTn = oTn_s[hg % 2], BoTn_s[hg % 2]
                    def fy(e):
                        ins = None
                        for pp in range(2):
                            for q2 in range(2):
                                hl = 2 * pp + q2
                                h = 4 * hg + hl
                                ins = e.matmul(ps[6][:, pp * 128:(pp + 1) * 128], lhsT=wuvP[:, h, :], rhs=oTn[:, hl * 128:(hl + 1) * 128],
                                               start=(q2 == 0), stop=(q2 == 1))
                        return ins
                    op("pe", fy, reads=[BoTn, Bconst], writes=[Bps[6]])
                    for pp in range(2):
                        cc = 2 * hg + pp
                        op("dve", lambda e, pp=pp, cc=cc: e.tensor_tensor(out=sgT[:, cc, i * 128:(i + 1) * 128], in0=ps[6][:, pp * 128:(pp + 1) * 128],
                                                                           in1=sgT[:, cc, i * 128:(i + 1) * 128], op=ALU.mult),
                           reads=[Bps[6], BsgT[cc]], writes=[BsgT[cc]])

                gens = {}
                for i in range(1, 4):
                    if tb * 4 + i >= 2:
                        gens[i] = topk_gen(i, tb * 4 + i)
                attn_prologue(0, tb * 4)
                for i in range(4):
                    gt = tb * 4 + i
                    g = gens.get(i + 1)
                    for hg in range(4):
                        attn_hg(i, gt, hg)
                        if hg > 0:
                            attn_epi(i, hg - 1)
                        if g is not None and hg < 3:
                            next(g, None)
                            if hg == 0:
                                next(g, None)
                    if g is not None:
                        for _ in g:
                            pass
                    if i + 1 < 4:
                        attn_prologue(i + 1, gt + 1)
                    if g is not None:
                        nmask_build(i + 1, gt + 1)
                    attn_epi(i, 3)

                chk(4)
                mixedT, BmixedT = qT, BqT
                for f in range(8):
                    ja = load_chunk(CH["wap"][f])
                    def fya(e, ja=ja):
                        ins = None
                        for k in range(8):
                            ins = e.matmul(ps[0][:, :], lhsT=wch[ja][:, k, :], rhs=sgT[:, k, :], start=(k == 0), stop=(k == 7))
                        return ins
                    op("pe", fya, reads=[Bwch[ja]] + BsgT, writes=[Bps[0]])
                    jr = load_chunk(CH["wrp"][f])
                    proj_fm(jr, 128, 1, hgT, BhgT)
                    jg = load_chunk(CH["mga"][f])
                    proj_fm(jg, 128, 2, xnT, BxnT)
                    op("act", lambda e, f=f: e.activation(out=ga_t[:, :], in_=ps[2][:, :], func=AF.Sigmoid, bias=bmcol[:, f:f + 1], scale=1.0),
                       reads=[Bps[2], Bconst], writes=[Bga])
                    jg2 = load_chunk(CH["mgr"][f])
                    proj_fm(jg2, 128, 3, xnT, BxnT)
                    op("act", lambda e, f=f: e.activation(out=gr_t[:, :], in_=ps[3][:, :], func=AF.Sigmoid, bias=bmcol[:, 8 + f:9 + f], scale=1.0),
                       reads=[Bps[3], Bconst], writes=[Bgr])
                    op("dve", lambda e: e.tensor_tensor(out=m1_t[:, :], in0=ps[0][:, :], in1=ga_t[:, :], op=ALU.mult), reads=[Bps[0], Bga], writes=[Bm1])
                    op("dve", lambda e: e.tensor_tensor(out=m2_t[:, :], in0=ps[1][:, :], in1=gr_t[:, :], op=ALU.mult), reads=[Bps[1], Bgr], writes=[Bm2])
                    op("pool", lambda e, f=f: e.tensor_tensor(out=mixedT[:, f, :], in0=m1_t[:, :], in1=m2_t[:, :], op=ALU.add), reads=[Bm1, Bm2], writes=[BmixedT])
                for i in range(4):
                    xb = i % 2
                    rk = i % 2
                    res_t = I_t[:, rk * 1024:(rk + 1) * 1024]
                    S_.dma("sp", "xl%d" % xb, xt[xb][:, :], x[sq, t0 + i * 128:t0 + (i + 1) * 128, :], writes=[Bxt[xb]])
                    for db in range(2):
                        def fo(e, i=i, db=db):
                            ins = None
                            for k in range(8):
                                ins = e.matmul(ps[4 + db][:, :], lhsT=mixedT[:, k, i * 128:(i + 1) * 128], rhs=wout_sb[:, k, db * 512:(db + 1) * 512],
                                               start=(k == 0), stop=(k == 7))
                            return ins
                        op("pe", fo, reads=[BmixedT, Bconst], writes=[Bps[4 + db]])
                        op("dve", lambda e, db=db, xb=xb, res_t=res_t: e.tensor_tensor(out=res_t[:, db * 512:(db + 1) * 512], in0=ps[4 + db][:, :],
                                                                                       in1=xt[xb][:, db * 512:(db + 1) * 512], op=ALU.add),
                           reads=[Bps[4 + db], Bxt[xb]], writes=[Bres[rk], BI])
                    op("act", lambda e, res_t=res_t: e.activation(out=junk[:, 0:1024], in_=res_t, func=AF.Square, accum_out=st[:, 2:3]),
                       reads=[Bres[rk]], writes=[Bjunk_act, Bst])
                    rstd_from_ss(st, Bst, 2, 3, 1024.0)
                    op("dve", lambda e, res_t=res_t: e.scalar_tensor_tensor(out=res_t, in0=res_t, scalar=st[:, 3:4], in1=fg_bc[:, :],
                                                                            op0=ALU.mult, op1=ALU.mult), reads=[Bres[rk], Bst, Bconst], writes=[Bres[rk]])
                    S_.dma("pool", "os%d" % rk, out[sq, t0 + i * 128:t0 + (i + 1) * 128, :], res_t, reads=[Bres[rk]])
    except _Stop:
        pass
    for key in sorted(S_.dsem):
        nc.gpsimd.wait_ge(S_.dsem[key][0], S_.dsem[key][1])
    for k_ in S_.sem:
        if S_.cnt[k_] > 0 and k_ != "pool":
            nc.gpsimd.wait_ge(S_.sem[k_], S_.cnt[k_])
    return nc


_PARAMS = ["norm_gain", "w_in", "b_merge", "kv_norm_gain", "w_uk", "w_uv", "idx_ln_gain", "idx_ln_bias", "w_attn_proj",
           "conv_w", "conv_b", "w_rg_a", "b_rg_a", "w_rg_x", "b_rg_x", "lru_lambda", "w_rnn_proj", "w_out"]


def kernel(**inputs):
    x = np.ascontiguousarray(np.asarray(inputs["x"], dtype=np.float32))
    B, S, _ = x.shape
    nseq = B // NCORES
    base = {}
    for k in _PARAMS:
        a = np.asarray(inputs[k], dtype=np.float32)
        base[k] = np.ascontiguousarray(a.reshape(a.shape[1:]))
    base["final_norm_gain"] = np.ascontiguousarray(np.asarray(inputs["final_norm_gain"], dtype=np.float32))
    nc = build(nseq, S)
    in_maps = []
    for c in range(NCORES):
        m = dict(base)
        m["x"] = np.ascontiguousarray(x[c * nseq:(c + 1) * nseq])
        in_maps.append(m)
    res = run_bass_kernel_spmd(nc, in_maps, core_ids=list(range(NCORES)))
    return np.concatenate([np.asarray(r["out"], dtype=np.float32) for r in res.results], axis=0)
```

```python
import numpy as np
import concourse.bass as bass
import concourse.mybir as mybir
from concourse.bass_utils import run_bass_kernel_spmd

F32 = mybir.dt.float32
BF16 = mybir.dt.bfloat16
ALU = mybir.AluOpType
AF = mybir.ActivationFunctionType
AX = mybir.AxisListType

D = 1024
NCORES = 8
EPS = 1e-6
TOPK = 256
NBIS = 12


class Buf:
    __slots__ = ("name", "w", "r")

    def __init__(self, name):
        self.name = name
        self.w = None
        self.r = []


class Sched:
    def __init__(self, nc):
        self.nc = nc
        self.eng = {"pe": nc.tensor, "act": nc.scalar, "dve": nc.vector, "pool": nc.gpsimd, "sp": nc.sync}
        self.sem, self.cnt, self.waited, self.dsem = {}, {}, {}, {}
        for k in self.eng:
            self.sem[k] = nc.semaphore("s_" + k).__enter__()
            self.cnt[k] = 0
            self.waited[k] = {}

    def _wait(self, e, dep):
        de, val = dep
        if de == e and e == "pe":
            return
        if self.waited[e].get(de, 0) >= val:
            return
        self.waited[e][de] = val
        s = self.dsem[de][0] if de.startswith("dma:") else self.sem[de]
        self.eng[e].wait_ge(s, val)

    def _deps(self, e, reads, writes):
        deps = set()
        for b in reads:
            if b.w is not None:
                deps.add(b.w)
        for b in writes:
            if b.w is not None:
                deps.add(b.w)
            deps.update(b.r)
        for d in sorted(deps):
            self._wait(e, d)

    def _mark(self, me, reads, writes):
        for b in reads:
            if len(b.r) > 24:
                last = {}
                for (q, v) in b.r:
                    last[q] = max(last.get(q, 0), v)
                b.r = list(last.items())
            b.r.append(me)
        for b in writes:
            b.w = me
            b.r = []

    def op(self, e, fn, reads=(), writes=()):
        self._deps(e, reads, writes)
        ins = fn(self.eng[e])
        self.cnt[e] += 1
        ins.then_inc(self.sem[e], 1)
        me = (e, self.cnt[e])
        self._mark(me, reads, writes)
        return me

    def dma(self, e, q, out, in_, reads=(), writes=()):
        key = "dma:" + q
        if key not in self.dsem:
            self.dsem[key] = [self.nc.semaphore("d_" + q).__enter__(), 0]
        self._deps(e, reads, writes)
        ins = self.eng[e].dma_start(out=out, in_=in_)
        self.dsem[key][1] += 16
        ins.then_inc(self.dsem[key][0], 16)
        me = (key, self.dsem[key][1])
        self._mark(me, reads, writes)
        return me


class Stepper:
    def __init__(self, g):
        self.g, self.idx = g, True
    def tick(self):
        if self.g is not None and self.idx:
            if next(self.g, None) != "idx":
                self.idx = False
    def seg(self):
        if self.g is None:
            return
        while self.idx:
            self.tick()
        next(self.g, None)
    def drain(self):
        if self.g is not None:
            for _ in self.g:
                pass


def _bf16_split(v):
    import ml_dtypes
    hi = float(np.float32(v).astype(ml_dtypes.bfloat16))
    lo = float(np.float32(v - hi).astype(ml_dtypes.bfloat16))
    return hi, lo


class _Stop(Exception):
    pass


def build(nseq, S, debug=False, stop=None):
    NBLK = S // 512
    NTS = S // 128
    nc = bass.Bass("TRN2", target_bir_lowering=False)
    S_ = Sched(nc)

    def din(name, shape):
        return nc.dram_tensor(name, list(shape), F32, kind="ExternalInput").ap()

    x = din("x", [nseq, S, D])
    norm_gain = din("norm_gain", [D])
    w_in = din("w_in", [D, 6856])
    b_merge = din("b_merge", [2048])
    kv_norm_gain = din("kv_norm_gain", [128])
    w_uk = din("w_uk", [16, 128, 64])
    w_uv = din("w_uv", [16, 128, 64])
    idx_ln_gain = din("idx_ln_gain", [64])
    idx_ln_bias = din("idx_ln_bias", [64])
    w_attn_proj = din("w_attn_proj", [D, D])
    conv_w = din("conv_w", [4, D])
    conv_b = din("conv_b", [D])
    w_rg_a = din("w_rg_a", [16, 64, 64])
    b_rg_a = din("b_rg_a", [D])
    w_rg_x = din("w_rg_x", [16, 64, 64])
    b_rg_x = din("b_rg_x", [D])
    lru_lambda = din("lru_lambda", [D])
    w_rnn_proj = din("w_rnn_proj", [D, D])
    w_out = din("w_out", [D, D])
    final_norm_gain = din("final_norm_gain", [D])
    out = nc.dram_tensor("out", [nseq, S, D], F32, kind="ExternalOutput").ap()
    wbf = nc.dram_tensor("wbf", [70, 128, 1024], BF16, kind="Internal").ap()

    def sb(name, shape, dt=F32):
        return nc.sbuf_tensor(name, list(shape), dt).__enter__()

    CH = {}
    chunk_src = []

    def add_chunks(name, src, col0, n, width=128):
        CH[name] = []
        for c in range(n):
            CH[name].append(len(chunk_src))
            chunk_src.append((src, col0 + c * 128, width))

    add_chunks("q", w_in, 0, 8)
    add_chunks("ckv", w_in, 1024, 1)
    add_chunks("qidx", w_in, 1152, 4)
    add_chunks("kw", w_in, 1664, 1, 72)
    add_chunks("gattn", w_in, 1736, 8)
    add_chunks("xrnn", w_in, 2760, 8)
    add_chunks("grnn", w_in, 3784, 8)
    add_chunks("mga", w_in, 4808, 8)
    add_chunks("mgr", w_in, 5832, 8)
    add_chunks("wap", w_attn_proj, 0, 8)
    add_chunks("wrp", w_rnn_proj, 0, 8)
    assert len(chunk_src) == 70
    Bwbf = [Buf("wbf%d" % i) for i in range(70)]

    ident = sb("ident", [128, 128], BF16); Bident = Buf("ident")
    ones_bf = sb("ones_bf", [128, 128], BF16)
    triT = sb("triT", [128, 128], BF16)
    causal_neg = sb("causal_neg", [128, 128])
    iota_row = sb("iota_row", [128, 128])
    pidx = sb("pidx", [128, 1])
    Bconst = Buf("const")

    wout_sb = sb("wout_sb", [128, 8, 1024], BF16)
    wuvP = sb("wuvP", [128, 16, 128], BF16)
    wuk_nat = sb("wuk_nat", [128, 16, 64], BF16)
    wukTz = sb("wukTz", [128, 16, 128], BF16)
    BDa = sb("BDa", [128, 8, 128], BF16)
    BDx = sb("BDx", [128, 8, 128], BF16)
    gcol = sb("gcol", [128, 8])
    bmcol = sb("bmcol", [128, 16])
    cwcol = sb("cwcol", [128, 4, 8])
    cbcol = sb("cbcol", [128, 8])
    bacol = sb("bacol", [128, 8])
    bxcol = sb("bxcol", [128, 8])
    lamcol = sb("lamcol", [128, 8])
    kap = sb("kap", [128, 8])
    kap2 = sb("kap2", [128, 8])
    fg_bc = sb("fg_bc", [128, 1024])
    gkv_bc = sb("gkv_bc", [128, 128])
    lng_bc = sb("lng_bc", [128, 64])
    lnb_bc = sb("lnb_bc", [128, 64])
    A_all = sb("A_all", [32, 16, 128], BF16)
    Bcol_hi = sb("Bcol_hi", [32, 16])
    Bcol_lo = sb("Bcol_lo", [32, 16])
    Bcol = sb("Bcol", [32, 16])
    e0 = sb("e0", [32, 1]); e12 = sb("e12", [32, 1]); e34 = sb("e34", [32, 1])
    e13 = sb("e13", [32, 1]); e24 = sb("e24", [32, 1]); etmp = sb("etmp", [32, 1])
    cold = sb("cold", [32, 16])
    Bwork = [sb("Bwork%d" % g, [32, 512], BF16) for g in range(4)]
    BBwork = [Buf("Bwork%d" % g) for g in range(4)]
    pow2 = sb("pow2", [128, NBIS])
    nhc = sb("nhc", [128, 1]); nhc_bf = sb("nhc_bf", [128, 1], BF16)
    nb32 = sb("nb32", [128, 1])

    cT = sb("cT", [128, S], BF16); BcT = Buf("cT")
    c_tm = sb("c_tm", [128, NTS, 128], BF16); Bctm = Buf("c_tm")
    kz = [sb("kz%d" % i, [128, S], BF16) for i in range(2)]; BkT = Buf("kidxT")
    w_tm = sb("w_tm", [128, NTS, 8]); Bwtm = Buf("w_tm")
    cabs = sb("cabs", [128, 1]); cabs_b = sb("cabs_b", [128, 1]); ncabs = sb("ncabs", [128, 1], BF16)
    Bcabs = Buf("cabs")
    xnT = sb("xnT", [128, 8, 512], BF16); BxnT = Buf("xnT")
    qT = sb("qT", [128, 8, 512], BF16); BqT = Buf("qT")
    sgT = sb("sgT", [128, 8, 512], BF16); BsgT = [Buf("sgT%d" % c) for c in range(8)]
    hgT = sb("hgT", [128, 8, 512], BF16); BhgT = Buf("hgT")
    qidxT = sb("qidxT", [128, 4, 512], BF16); BqidxT = Buf("qidxT")
    wch = [sb("wch%d" % i, [128, 8, 128], BF16) for i in range(4)]
    Bwch = [Buf("wch%d" % i) for i in range(4)]
    xt = [sb("xt%d" % i, [128, 1024]) for i in range(2)]
    Bxt = [Buf("xt%d" % i) for i in range(2)]
    xs = sb("xs", [128, 1024], BF16); Bxs = Buf("xs")
    junk = sb("junk", [128, 2048], BF16); Bjunk_act = Buf("junk_act"); Bjunk_dve = Buf("junk_dve")
    st = sb("st", [128, 16]); Bst = Buf("st")
    st2 = sb("st2", [128, 16]); Bst2 = Buf("st2")
    kc = sb("kc", [128, 64]); Bkc = Buf("kc")
    kn2 = sb("kn2", [128, 128], BF16); Bkn2 = Buf("kn2")
    ub = [sb("ub%d" % k, [128, 515], BF16) for k in range(2)]; Bub = [Buf("ub%d" % k) for k in range(2)]
    ucar = sb("ucar", [128, 8, 3], BF16); Bucar = [Buf("ucar%d" % c) for c in range(8)]
    xc_s = [sb("xc%d" % k, [128, 512]) for k in range(2)]; Bxc_s = [Buf("xc%d" % k) for k in range(2)]
    xcb_s = [sb("xcb%d" % k, [128, 512], BF16) for k in range(2)]; Bxcb_s = [Buf("xcb%d" % k) for k in range(2)]
    r_s = [sb("r%d" % k, [128, 512]) for k in range(2)]; Br_s = [Buf("r%d" % k) for k in range(2)]
    i_s = [sb("i%d" % k, [128, 512]) for k in range(2)]; Bi_s = [Buf("i%d" % k) for k in range(2)]
    a_s = [sb("a%d" % k, [128, 512]) for k in range(2)]; Ba_s = [Buf("a%d" % k) for k in range(2)]
    s_s = [sb("s%d" % k, [128, 512]) for k in range(2)]; Bs_s = [Buf("s%d" % k) for k in range(2)]
    sgr_s = [sb("sgr%d" % k, [128, 512], BF16) for k in range(2)]; Bsgr_s = [Buf("sgr%d" % k) for k in range(2)]
    r_t, Br, i_t, Bi, a_t, Ba, s_t, Bs = r_s[0], Br_s[0], i_s[0], Bi_s[0], a_s[0], Ba_s[0], s_s[0], Bs_s[0]
    ptmp, Bptmp = s_s[1], Bs_s[1]
    hcar = sb("hcar", [128, 8]); Bhcar = [Buf("hcar%d" % c) for c in range(8)]
    I_t = sb("I_t", [128, 2048]); BI = Buf("I")
    rj = [sb("rj%d" % i, [128, 512]) for i in range(2)]; Brj = [Buf("rj%d" % i) for i in range(2)]
    mask = sb("mask", [128, 2048], BF16); Bmask = Buf("mask")
    nmask = sb("nmask", [128, 16, 512], BF16); Bnmask = [Buf("nmask%d" % j) for j in range(16)]
    identrep4 = sb("identrep4", [128, 512], BF16)
    SI = sb("SI", [128, 4, 512], BF16)
    iota1_full = sb("iota1_full", [128, 2048], BF16)
    ntri_rep = sb("ntri_rep", [128, 512], BF16)
    nsm_s = [sb("nsm%d" % k, [128, 1], BF16) for k in range(2)]; Bnsm_s = [Buf("nsm%d" % k) for k in range(2)]
    bis_s = [sb("bis%d" % k, [128, 8]) for k in range(2)]; Bbis_s = [Buf("bis%d" % k) for k in range(2)]
    steps = sb("steps", [128, NBIS]); steps2 = sb("steps2", [128, NBIS]); Bsteps = Buf("steps")
    qlb_s = [sb("qlb%d" % k, [128, 512], BF16) for k in range(4)]; Bqlb_s = [Buf("qlb%d" % k) for k in range(4)]
    absq = sb("absq", [128, 512], BF16); Babsq = Buf("absq")
    p_t = [sb("p%d" % i, [128, 512], BF16) for i in range(2)]; Bp = [Buf("p%d" % i) for i in range(2)]
    rs_s = [sb("rs%d" % k, [128, 512]) for k in range(2)]; Brs_s = [Buf("rs%d" % k) for k in range(2)]
    oTn_s = [sb("oTn%d" % k, [128, 512], BF16) for k in range(2)]; BoTn_s = [Buf("oTn%d" % k) for k in range(2)]
    ga_t, Bga = r_t, Br
    gr_t, Bgr = i_t, Bi
    m1_t, Bm1 = a_t, Ba
    m2_t, Bm2 = s_t, Bs
    Bres = [Buf("res0"), Buf("res1")]

    ps = [nc.psum_tensor("ps%d" % i, [128, 512], F32).__enter__() for i in range(8)]
    Bps = [Buf("ps%d" % i) for i in range(8)]

    def psbf(i):
        return ps[i][:, :].bitcast(BF16)

    op = S_.op

    with nc.allow_non_contiguous_dma(reason="one-time small parameter layouts"):
        for ci, (src, col0, wd) in enumerate(chunk_src):
            S_.dma("pool", "cv", wbf[ci].rearrange("p (k j) -> p k j", k=8)[:, :, 0:wd],
                   src.rearrange("(k p) n -> p k n", p=128)[:, :, col0:col0 + wd], writes=[Bwbf[ci]])
        for b_ in Bwbf:
            b_.w = ("dma:cv", S_.dsem["dma:cv"][1])
        S_.dma("pool", "cs", wout_sb[:, :, :], w_out.rearrange("(k p) n -> p k n", p=128), writes=[Bconst])
        op("pool", lambda e: e.memset(wuvP[:, :, :], 0.0), writes=[Bconst])
        for par in range(2):
            S_.dma("pool", "cs", wuvP[:, :, :].rearrange("c (hp two) d -> c hp two d", two=2)[:, :, par, par * 64:par * 64 + 64],
                   w_uv.rearrange("(hp two) c d -> c hp two d", two=2)[:, :, par, :], reads=[Bconst], writes=[Bconst])
        S_.dma("pool", "cs", wuk_nat[:, :, :], w_uk.rearrange("h c d -> c h d"), writes=[Bconst])
        op("pool", lambda e: e.memset(BDa[:, :, :], 0.0), writes=[Bconst])
        op("pool", lambda e: e.memset(BDx[:, :, :], 0.0), writes=[Bconst])
        for (bd, wsrc) in ((BDa, w_rg_a), (BDx, w_rg_x)):
            for par in range(2):
                S_.dma("pool", "cs", bd[par * 64:par * 64 + 64, :, par * 64:par * 64 + 64],
                       wsrc.rearrange("(p two) i j -> two i p j", two=2)[par], reads=[Bconst], writes=[Bconst])
        for (dst, src, n) in ((gcol, norm_gain, 8), (bmcol, b_merge, 16), (cbcol, conv_b, 8), (bacol, b_rg_a, 8),
                              (bxcol, b_rg_x, 8), (lamcol, lru_lambda, 8)):
            S_.dma("sp", "cs2", dst[:, :], src.rearrange("(c p) -> p c", p=128), writes=[Bconst])
        S_.dma("sp", "cs2", cwcol[:, :, :], conv_w.rearrange("k (c p) -> p k c", p=128), writes=[Bconst])
        S_.dma("sp", "cs2", fg_bc[:, :], final_norm_gain.partition_broadcast(128), writes=[Bconst])
        S_.dma("sp", "cs2", gkv_bc[:, :], kv_norm_gain.partition_broadcast(128), writes=[Bconst])
        S_.dma("sp", "cs2", lng_bc[:, :], idx_ln_gain.partition_broadcast(128), writes=[Bconst])
        S_.dma("sp", "cs2", lnb_bc[:, :], idx_ln_bias.partition_broadcast(128), writes=[Bconst])

    for par in range(2):
        op("pool", lambda e, par=par: e.memset(kz[par][:, :], 0.0), writes=[BkT])
    op("pool", lambda e: e.iota(iota_row[:, :], [[1, 128]], base=0, channel_multiplier=0,
                                allow_small_or_imprecise_dtypes=True), writes=[Bconst])
    op("pool", lambda e: e.iota(pidx[:, :], [[0, 1]], base=0, channel_multiplier=1,
                                allow_small_or_imprecise_dtypes=True), reads=[Bconst], writes=[Bconst])
    op("dve", lambda e: e.tensor_scalar(out=ident[:, :], in0=iota_row[:, :], scalar1=pidx[:, 0:1], scalar2=None,
                                        op0=ALU.is_equal), reads=[Bconst], writes=[Bconst, Bident])
    op("dve", lambda e: e.tensor_scalar(out=triT[:, :], in0=iota_row[:, :], scalar1=pidx[:, 0:1], scalar2=None,
                                        op0=ALU.is_ge), reads=[Bconst], writes=[Bconst])
    op("dve", lambda e: e.tensor_scalar(out=causal_neg[:, :], in0=iota_row[:, :], scalar1=pidx[:, 0:1], scalar2=-1e30,
                                        op0=ALU.is_gt, op1=ALU.mult), reads=[Bconst], writes=[Bconst])
    op("dve", lambda e: e.memset(ones_bf[:, :], 1.0), reads=[Bconst], writes=[Bconst])
    for k in range(NBIS):
        op("dve", lambda e, k=k: e.memset(pow2[:, k:k + 1], 2.0 ** -(k + 1)), reads=[Bconst], writes=[Bconst])
    op("dve", lambda e: e.tensor_reduce(out=nhc[:, 0:1], in_=gkv_bc[:, :], axis=AX.X, op=ALU.max, apply_absolute_value=True),
       reads=[Bconst], writes=[Bconst])
    op("dve", lambda e: e.tensor_scalar(out=nhc[:, 0:1], in0=nhc[:, 0:1], scalar1=-0.5 * (128.0 ** 0.5), scalar2=None, op0=ALU.mult),
       reads=[Bconst], writes=[Bconst])
    op("dve", lambda e: e.tensor_copy(out=nhc_bf[:, 0:1], in_=nhc[:, 0:1]), reads=[Bconst], writes=[Bconst])
    op("dve", lambda e: e.memset(nb32[:, :], -32768.0), reads=[Bconst], writes=[Bconst])
    op("act", lambda e: e.activation(out=kap[:, :], in_=lamcol[:, :], func=AF.Exp, scale=-1.0), reads=[Bconst], writes=[Bconst])
    op("act", lambda e: e.activation(out=kap[:, :], in_=kap[:, :], func=AF.Ln, bias=1.0, scale=1.0), reads=[Bconst], writes=[Bconst])
    op("dve", lambda e: e.tensor_scalar(out=kap2[:, :], in0=kap[:, :], scalar1=-16.0, scalar2=None, op0=ALU.mult), reads=[Bconst], writes=[Bconst])
    op("dve", lambda e: e.tensor_scalar(out=kap[:, :], in0=kap[:, :], scalar1=-8.0, scalar2=None, op0=ALU.mult), reads=[Bconst], writes=[Bconst])
    for pr in range(8):
        op("pe", lambda e, pr=pr: e.transpose(out=psbf(0)[:, pr * 128:(pr + 1) * 128],
                                              in_=wuk_nat[:, 2 * pr:2 * pr + 2, :].rearrange("c h d -> c (h d)"),
                                              identity=ident[:, :]), reads=[Bconst], writes=[Bps[0]])
    op("dve", lambda e: e.memset(wukTz[:, :, :], 0.0), reads=[Bconst], writes=[Bconst])
    for par in range(2):
        op("dve", lambda e, par=par: e.tensor_copy(
            out=wukTz[par * 64:par * 64 + 64, :, :].rearrange("p (pr two) c -> p pr two c", two=2)[:, :, par, :],
            in_=psbf(0)[par * 64:par * 64 + 64, :].rearrange("p (pr c) -> p pr c", c=128)), reads=[Bps[0], Bconst], writes=[Bconst])
    def sel(dst, ks):
        for n, k in enumerate(ks):
            tgt = dst if n == 0 else etmp
            op("dve", lambda e, k=k, tgt=tgt: e.tensor_scalar(out=tgt[:, :], in0=pidx[0:32, :], scalar1=float(k), scalar2=None,
                                                             op0=ALU.is_equal), reads=[Bconst], writes=[Bconst])
            if n > 0:
                op("dve", lambda e: e.tensor_tensor(out=dst[:, :], in0=dst[:, :], in1=etmp[:, :], op=ALU.add), reads=[Bconst], writes=[Bconst])
    sel(e0, [0]); sel(e12, [1, 2]); sel(e34, [3, 4]); sel(e13, [1, 3]); sel(e24, [2, 4])
    slopes = [2.0 ** (-8.0 * (h + 1) / 16.0) for h in range(16)]
    for h in range(16):
        hi, lo = _bf16_split(slopes[h])
        op("dve", lambda e, h=h, hi=hi: e.memset(Bcol_hi[:, h:h + 1], hi), reads=[Bconst], writes=[Bconst])
        op("dve", lambda e, h=h, lo=lo: e.memset(Bcol_lo[:, h:h + 1], lo), reads=[Bconst], writes=[Bconst])
    op("dve", lambda e: e.tensor_scalar(out=Bcol[:, :], in0=Bcol_hi[:, :], scalar1=e13[:, 0:1], scalar2=None, op0=ALU.mult), reads=[Bconst], writes=[Bconst])
    op("dve", lambda e: e.scalar_tensor_tensor(out=Bcol[:, :], in0=Bcol_lo[:, :], scalar=e24[:, 0:1], in1=Bcol[:, :],
                                               op0=ALU.mult, op1=ALU.add), reads=[Bconst], writes=[Bconst])
    for g in range(4):
        for hl in range(4):
            h = 4 * g + hl
            op("dve", lambda e, g=g, hl=hl, h=h: e.tensor_scalar(out=Bwork[g][:, hl * 128:(hl + 1) * 128], in0=ones_bf[0:32, :],
                                                                  scalar1=Bcol[:, h:h + 1], scalar2=None, op0=ALU.mult),
               reads=[Bconst], writes=[BBwork[g]])
    for dl in range(16):
        op("dve", lambda e, dl=dl: e.tensor_scalar(out=cold[:, dl:dl + 1], in0=e12[:, :], scalar1=128.0 * dl, scalar2=None,
                                                   op0=ALU.mult), reads=[Bconst], writes=[Bconst])
        op("dve", lambda e, dl=dl: e.tensor_tensor(out=cold[:, dl:dl + 1], in0=cold[:, dl:dl + 1], in1=e0[:, :], op=ALU.add), reads=[Bconst], writes=[Bconst])
        op("dve", lambda e, dl=dl: e.tensor_scalar(out=A_all[:, dl, :], in0=iota_row[0:32, :], scalar1=e34[:, 0:1], scalar2=cold[:, dl:dl + 1],
                                                   op0=ALU.mult, op1=ALU.add), reads=[Bconst], writes=[Bconst])

    for r4 in range(4):
        op("dve", lambda e, r4=r4: e.tensor_copy(out=identrep4[:, r4 * 128:(r4 + 1) * 128], in_=ident[:, :]), reads=[Bconst], writes=[Bconst])
        op("dve", lambda e, r4=r4: e.tensor_scalar(out=ntri_rep[:, r4 * 128:(r4 + 1) * 128], in0=triT[:, :], scalar1=-1.0, scalar2=32768.0,
                                                   op0=ALU.add, op1=ALU.mult), reads=[Bconst], writes=[Bconst])
    for h in range(16):
        op("dve", lambda e, h=h: e.tensor_scalar(out=SI[:, h // 4, (h % 4) * 128:(h % 4 + 1) * 128], in0=ident[:, :], scalar1=slopes[h], scalar2=None,
                                                 op0=ALU.mult), reads=[Bconst], writes=[Bconst])
    op("pool", lambda e: e.iota(iota1_full[:, :], [[1, 2048]], base=1, channel_multiplier=0,
                                allow_small_or_imprecise_dtypes=True), reads=[Bconst], writes=[Bconst])

    wslot = [0]

    def load_chunk(ci):
        j = wslot[0] % 4
        wslot[0] += 1
        wd = chunk_src[ci][2]
        if wd == 128:
            S_.dma("sp", "wl%d" % j, wch[j][:, :, :].rearrange("p k j -> p (k j)"), wbf[ci], reads=[Bwbf[ci]], writes=[Bwch[j]])
        else:
            S_.dma("sp", "wl%d" % j, wch[j][:, :, 0:wd], wbf[ci].rearrange("p (k j) -> p k j", k=8)[:, :, 0:wd],
                   reads=[Bwbf[ci]], writes=[Bwch[j]])
        return j

    def proj_fm(j, wd, bank, rhs_t, rhs_b):
        def f(e):
            ins = None
            for k in range(8):
                ins = e.matmul(ps[bank][0:wd, :], lhsT=wch[j][:, k, 0:wd], rhs=rhs_t[:, k, :], start=(k == 0), stop=(k == 7))
            return ins
        op("pe", f, reads=[Bwch[j], rhs_b], writes=[Bps[bank]])

    def rstd_from_ss(stt, Bstt, col_ss, col_out, n):
        op("act", lambda e: e.activation(out=stt[:, col_out:col_out + 1], in_=stt[:, col_ss:col_ss + 1], func=AF.Sqrt,
                                         bias=EPS, scale=1.0 / n), reads=[Bstt], writes=[Bstt])
        op("dve", lambda e: e.reciprocal(out=stt[:, col_out:col_out + 1], in_=stt[:, col_out:col_out + 1]), reads=[Bstt], writes=[Bstt])

    bankrr = [0]

    def next_bank():
        b = (0, 1, 4, 5)[bankrr[0] % 4]
        bankrr[0] += 1
        return b

    dbg = {}

    def chk(n):
        if stop is not None and stop == n:
            raise _Stop()

    try:
        chk(0)
        for sq in range(nseq):
            for tb in range(NBLK):
                t0 = tb * 512
                for i in range(4):
                    xb = i % 2
                    S_.dma("sp", "xl%d" % xb, xt[xb][:, :], x[sq, t0 + i * 128:t0 + (i + 1) * 128, :], writes=[Bxt[xb]])
                    op("act", lambda e, xb=xb: e.activation(out=junk[:, 0:1024], in_=xt[xb][:, :], func=AF.Square, accum_out=st[:, 0:1]),
                       reads=[Bxt[xb]], writes=[Bjunk_act, Bst])
                    rstd_from_ss(st, Bst, 0, 1, 1024.0)
                    op("dve", lambda e, xb=xb: e.tensor_scalar(out=xs[:, :], in0=xt[xb][:, :], scalar1=st[:, 1:2], scalar2=None, op0=ALU.mult),
                       reads=[Bxt[xb], Bst], writes=[Bxs])
                    def ftr(e):
                        ins = None
                        for k in range(8):
                            ins = e.transpose(out=psbf(2)[:, k * 128:(k + 1) * 128], in_=xs[:, k * 128:(k + 1) * 128], identity=ident[:, :])
                        return ins
                    op("pe", ftr, reads=[Bxs, Bident], writes=[Bps[2]])
                    for k in range(8):
                        op("dve", lambda e, k=k, i=i: e.tensor_scalar(out=xnT[:, k, i * 128:(i + 1) * 128], in0=psbf(2)[:, k * 128:(k + 1) * 128],
                                                                       scalar1=gcol[:, k:k + 1], scalar2=None, op0=ALU.mult),
                           reads=[Bps[2], Bconst], writes=[BxnT])

                chk(1)
                def b_steps():
                    for c in range(8):
                        j = load_chunk(CH["q"][c]); bk = next_bank()
                        proj_fm(j, 128, bk, xnT, BxnT)
                        op("act", lambda e, c=c, bk=bk: e.copy(out=qT[:, c, :], in_=ps[bk][:, :]), reads=[Bps[bk]], writes=[BqT])
                        yield
                    for c in range(4):
                        j = load_chunk(CH["qidx"][c]); bk = next_bank()
                        proj_fm(j, 128, bk, xnT, BxnT)
                        op("act", lambda e, c=c, bk=bk: e.mul(out=qidxT[:, c, :], in_=ps[bk][:, :], mul=0.125),
                           reads=[Bps[bk]], writes=[BqidxT])
                        yield
                    j = load_chunk(CH["ckv"][0])
                    for i in range(4):
                        gt = tb * 4 + i
                        def fc(e, i=i, j=j):
                            ins = None
                            for k in range(8):
                                ins = e.matmul(ps[2][:, 0:128], lhsT=xnT[:, k, i * 128:(i + 1) * 128], rhs=wch[j][:, k, :], start=(k == 0), stop=(k == 7))
                            return ins
                        op("pe", fc, reads=[Bwch[j], BxnT], writes=[Bps[2]])
                        op("act", lambda e: e.activation(out=junk[:, 0:128], in_=ps[2][:, 0:128], func=AF.Square, accum_out=st2[:, 0:1]),
                           reads=[Bps[2]], writes=[Bjunk_act, Bst2])
                        rstd_from_ss(st2, Bst2, 0, 1, 128.0)
                        op("dve", lambda e, gt=gt: e.scalar_tensor_tensor(out=c_tm[:, gt, :], in0=ps[2][:, 0:128], scalar=st2[:, 1:2], in1=gkv_bc[:, :],
                                                                           op0=ALU.mult, op1=ALU.mult), reads=[Bps[2], Bst2, Bconst], writes=[Bctm])
                        op("pe", lambda e, gt=gt: e.transpose(out=psbf(3)[:, 0:128], in_=c_tm[:, gt, :], identity=ident[:, :]),
                           reads=[Bctm, Bident], writes=[Bps[3]])
                        op("dve", lambda e, gt=gt: e.tensor_copy(out=cT[:, gt * 128:(gt + 1) * 128], in_=psbf(3)[:, 0:128]), reads=[Bps[3]], writes=[BcT])
                    yield
                    j = load_chunk(CH["kw"][0])
                    for i in range(4):
                        gt = tb * 4 + i
                        def fk(e, i=i, j=j):
                            ins = None
                            for k in range(8):
                                ins = e.matmul(ps[2][:, 0:72], lhsT=xnT[:, k, i * 128:(i + 1) * 128], rhs=wch[j][:, k, 0:72], start=(k == 0), stop=(k == 7))
                            return ins
                        op("pe", fk, reads=[Bwch[j], BxnT], writes=[Bps[2]])
                        op("dve", lambda e, gt=gt: e.tensor_scalar(out=w_tm[:, gt, :], in0=ps[2][:, 64:72], scalar1=8.0 ** -0.5, scalar2=None, op0=ALU.mult),
                           reads=[Bps[2]], writes=[Bwtm])
                        op("dve", lambda e: e.tensor_reduce(out=st2[:, 2:3], in_=ps[2][:, 0:64], axis=AX.X, op=ALU.add), reads=[Bps[2]], writes=[Bst2])
                        op("dve", lambda e: e.tensor_scalar(out=st2[:, 2:3], in0=st2[:, 2:3], scalar1=-1.0 / 64.0, scalar2=None, op0=ALU.mult), reads=[Bst2], writes=[Bst2])
                        op("dve", lambda e: e.tensor_scalar(out=kc[:, :], in0=ps[2][:, 0:64], scalar1=st2[:, 2:3], scalar2=None, op0=ALU.add),
                           reads=[Bps[2], Bst2], writes=[Bkc])
                        op("act", lambda e: e.activation(out=junk[:, 0:64], in_=kc[:, :], func=AF.Square, accum_out=st2[:, 3:4]),
                           reads=[Bkc], writes=[Bjunk_act, Bst2])
                        rstd_from_ss(st2, Bst2, 3, 4, 64.0)
                        op("dve", lambda e: e.scalar_tensor_tensor(out=kc[:, :], in0=kc[:, :], scalar=st2[:, 4:5], in1=lng_bc[:, :], op0=ALU.mult, op1=ALU.mult),
                           reads=[Bkc, Bst2, Bconst], writes=[Bkc])
                        op("dve", lambda e: e.tensor_tensor(out=kn2[:, 0:64], in0=kc[:, :], in1=lnb_bc[:, :], op=ALU.add), reads=[Bkc, Bconst], writes=[Bkn2])
                        op("dve", lambda e: e.tensor_copy(out=kn2[:, 64:128], in_=kn2[:, 0:64]), reads=[Bkn2], writes=[Bkn2])
                        op("pe", lambda e: e.transpose(out=psbf(3)[:, 0:128], in_=kn2[:, :], identity=ident[:, :]), reads=[Bkn2, Bident], writes=[Bps[3]])
                        for par in range(2):
                            op("dve", lambda e, gt=gt, par=par: e.tensor_copy(out=kz[par][par * 64:par * 64 + 64, gt * 128:(gt + 1) * 128],
                                                                               in_=psbf(3)[par * 64:par * 64 + 64, 0:128]), reads=[Bps[3]], writes=[BkT])
                    yield
                def b_gattn():
                    for c in range(8):
                        j = load_chunk(CH["gattn"][c]); bk = next_bank()
                        proj_fm(j, 128, bk, xnT, BxnT)
                        op("act", lambda e, c=c, bk=bk: e.activation(out=sgT[:, c, :], in_=ps[bk][:, :], func=AF.Silu), reads=[Bps[bk]], writes=[BsgT[c]])

                chk(2)
                def topk_gen(i, gt):
                    ns = gt + 1
                    ncols = ns * 128
                    bis, Bbis, nsm, Bnsm = bis_s[i % 2], Bbis_s[i % 2], nsm_s[i % 2], Bnsm_s[i % 2]
                    nsb = (ncols + 511) // 512
                    for sbk in range(nsb):
                        c0 = sbk * 512
                        cw = min(512, ncols - c0)
                        for jh in range(8):
                            par = jh % 2
                            bk = 6 + (jh % 2)
                            op("pe", lambda e, jh=jh, par=par, bk=bk, c0=c0, cw=cw: e.matmul(
                                ps[bk][:, 0:cw], lhsT=qidxT[:, jh // 2, i * 128:(i + 1) * 128],
                                rhs=kz[par][:, c0:c0 + cw], start=True, stop=True),
                               reads=[BqidxT, BkT], writes=[Bps[bk]])
                            rb = jh % 2
                            op("act", lambda e, bk=bk, rb=rb, cw=cw: e.activation(out=rj[rb][:, 0:cw], in_=ps[bk][:, 0:cw], func=AF.Relu),
                               reads=[Bps[bk]], writes=[Brj[rb]])
                            if jh == 0:
                                op("dve", lambda e, rb=rb, c0=c0, cw=cw: e.tensor_scalar(
                                    out=I_t[:, c0:c0 + cw], in0=rj[rb][:, 0:cw], scalar1=w_tm[:, gt, 0:1], scalar2=None, op0=ALU.mult),
                                   reads=[Brj[rb], Bwtm], writes=[BI, Bres[0], Bres[1]])
                            else:
                                op("dve", lambda e, rb=rb, c0=c0, cw=cw, jh=jh: e.scalar_tensor_tensor(
                                    out=I_t[:, c0:c0 + cw], in0=rj[rb][:, 0:cw], scalar=w_tm[:, gt, jh:jh + 1], in1=I_t[:, c0:c0 + cw],
                                    op0=ALU.mult, op1=ALU.add), reads=[Brj[rb], Bwtm, BI], writes=[BI])
                            if jh % 2 == 1:
                                yield "idx"
                    yield "seg"
                    op("dve", lambda e: e.tensor_reduce(out=bis[:, 0:1], in_=I_t[:, 0:ncols], axis=AX.X, op=ALU.max,
                                                        apply_absolute_value=True), reads=[BI], writes=[Bbis])
                    op("dve", lambda e: e.tensor_scalar(out=bis[:, 0:1], in0=bis[:, 0:1], scalar1=1.01, scalar2=1e-6, op0=ALU.mult, op1=ALU.add),
                       reads=[Bbis], writes=[Bbis])
                    op("dve", lambda e: e.tensor_scalar(out=steps[:, :], in0=pow2[:, :], scalar1=bis[:, 0:1], scalar2=None, op0=ALU.mult),
                       reads=[Bbis, Bconst], writes=[Bsteps])
                    op("dve", lambda e: e.tensor_scalar(out=steps2[:, :], in0=steps[:, :], scalar1=2.0, scalar2=None, op0=ALU.mult),
                       reads=[Bsteps], writes=[Bsteps])
                    op("dve", lambda e: e.tensor_tensor(out=I_t[:, gt * 128:(gt + 1) * 128], in0=I_t[:, gt * 128:(gt + 1) * 128],
                                                        in1=causal_neg[:, :], op=ALU.add), reads=[BI, Bconst, Bbis], writes=[BI])
                    op("dve", lambda e: e.memset(bis[:, 1:2], 0.0), reads=[Bbis], writes=[Bbis])
                    for kb in range(NBIS):
                        op("dve", lambda e: e.tensor_scalar(out=mask[:, 0:ncols], in0=I_t[:, 0:ncols], scalar1=bis[:, 1:2], scalar2=None,
                                                            op0=ALU.is_ge, op1=ALU.add, accum_out=bis[:, 2:3]),
                           reads=[BI, Bbis], writes=[Bmask, Bbis])
                        op("dve", lambda e, kb=kb: e.tensor_scalar(out=bis[:, 3:4], in0=bis[:, 2:3], scalar1=TOPK - 0.5, scalar2=steps2[:, kb:kb + 1],
                                                                   op0=ALU.is_ge, op1=ALU.mult), reads=[Bbis, Bsteps], writes=[Bbis])
                        op("dve", lambda e, kb=kb: e.scalar_tensor_tensor(out=bis[:, 1:2], in0=bis[:, 1:2], scalar=steps[:, kb:kb + 1], in1=bis[:, 3:4],
                                                                          op0=ALU.subtract, op1=ALU.add), reads=[Bbis, Bsteps], writes=[Bbis])
                        if kb in (3, 7):
                            yield "seg"
                    op("dve", lambda e: e.tensor_scalar(out=mask[:, 0:ncols], in0=I_t[:, 0:ncols], scalar1=bis[:, 1:2], scalar2=None,
                                                        op0=ALU.is_ge), reads=[BI, Bbis], writes=[Bmask])
                    op("dve", lambda e: e.scalar_tensor_tensor(out=I_t[:, 0:ncols], in0=I_t[:, 0:ncols], scalar=bis[:, 1:2],
                                                               in1=iota1_full[:, 0:ncols], op0=ALU.is_ge, op1=ALU.mult),
                       reads=[BI, Bbis, Bconst], writes=[BI])
                    op("dve", lambda e: e.tensor_reduce(out=bis[:, 4:5], in_=I_t[:, 0:ncols], axis=AX.X, op=ALU.max), reads=[BI], writes=[Bbis])
                    op("dve", lambda e: e.tensor_scalar(out=nsm[:, :], in0=bis[:, 4:5], scalar1=-1.0, scalar2=1.0, op0=ALU.mult, op1=ALU.add),
                       reads=[Bbis], writes=[Bnsm])

                def nmask_build(i, gt):
                    for jj in range(gt + 1):
                        mbk = 6 + (jj % 2)
                        op("pe", lambda e, jj=jj, mbk=mbk: e.matmul(ps[mbk][:, :], lhsT=mask[:, jj * 128:(jj + 1) * 128], rhs=identrep4[:, :],
                                                                     start=True, stop=True), reads=[Bmask, Bconst], writes=[Bps[mbk]])
                        op("act", lambda e, jj=jj, mbk=mbk: e.activation(out=nmask[:, jj, :], in_=ps[mbk][:, :], func=AF.Identity,
                                                                          bias=nb32[:, 0:1], scale=32768.0), reads=[Bps[mbk], Bconst], writes=[Bnmask[jj]])

                def rnn_steps(c, k):
                    xc, Bxc, xcb, Bxcb = xc_s[k], Bxc_s[k], xcb_s[k], Bxcb_s[k]
                    rr, Brr, ii, Bii, aa, Baa, ss, Bss = r_s[k], Br_s[k], i_s[k], Bi_s[k], a_s[k], Ba_s[k], s_s[k], Bs_s[k]
                    sg, Bsg, uu, Buu = sgr_s[k], Bsgr_s[k], ub[k], Bub[k]
                    j = load_chunk(CH["xrnn"][c]); bk = next_bank()
                    proj_fm(j, 128, bk, xnT, BxnT)
                    if tb == 0:
                        op("pool", lambda e: e.memset(uu[:, 0:3], 0.0), reads=[], writes=[Buu])
                        op("dve", lambda e: e.memset(hcar[:, c:c + 1], 0.0), reads=[], writes=[Bhcar[c]])
                    else:
                        op("pool", lambda e: e.tensor_copy(out=uu[:, 0:3], in_=ucar[:, c, :]), reads=[Bucar[c]], writes=[Buu])
                    op("act", lambda e: e.copy(out=uu[:, 3:515], in_=ps[bk][:, :]), reads=[Bps[bk]], writes=[Buu])
                    yield
                    op("dve", lambda e: e.tensor_scalar(out=xc[:, :], in0=uu[:, 3:515], scalar1=cwcol[:, 3, c:c + 1], scalar2=cbcol[:, c:c + 1],
                                                        op0=ALU.mult, op1=ALU.add), reads=[Buu, Bconst], writes=[Bxc])
                    for kk in range(3):
                        op("dve", lambda e, kk=kk: e.scalar_tensor_tensor(out=xc[:, :], in0=uu[:, kk:kk + 512], scalar=cwcol[:, kk, c:c + 1],
                                                                          in1=xc[:, :], op0=ALU.mult, op1=ALU.add),
                           reads=[Buu, Bconst, Bxc], writes=[Bxc])
                    op("pool", lambda e: e.tensor_copy(out=ucar[:, c, :], in_=uu[:, 512:515]), reads=[Buu], writes=[Bucar[c]])
                    op("pool", lambda e: e.tensor_copy(out=xcb[:, :], in_=xc[:, :]), reads=[Bxc], writes=[Bxcb])
                    yield
                    bk1 = next_bank()
                    op("pe", lambda e: e.matmul(ps[bk1][:, :], lhsT=BDa[:, c, :], rhs=xcb[:, :], start=True, stop=True),
                       reads=[Bxcb, Bconst], writes=[Bps[bk1]])
                    op("act", lambda e: e.activation(out=rr[:, :], in_=ps[bk1][:, :], func=AF.Sigmoid, bias=bacol[:, c:c + 1], scale=1.0),
                       reads=[Bps[bk1], Bconst], writes=[Brr])
                    bk2 = next_bank()
                    op("pe", lambda e: e.matmul(ps[bk2][:, :], lhsT=BDx[:, c, :], rhs=xcb[:, :], start=True, stop=True),
                       reads=[Bxcb, Bconst], writes=[Bps[bk2]])
                    op("act", lambda e: e.activation(out=ii[:, :], in_=ps[bk2][:, :], func=AF.Sigmoid, bias=bxcol[:, c:c + 1], scale=1.0),
                       reads=[Bps[bk2], Bconst], writes=[Bii])
                    yield
                    op("act", lambda e: e.activation(out=aa[:, :], in_=rr[:, :], func=AF.Exp, scale=kap[:, c:c + 1]), reads=[Brr, Bconst], writes=[Baa])
                    op("act", lambda e: e.activation(out=ss[:, :], in_=rr[:, :], func=AF.Exp, scale=kap2[:, c:c + 1]), reads=[Brr, Bconst], writes=[Bss])
                    yield
                    op("act", lambda e: e.activation(out=ss[:, :], in_=ss[:, :], func=AF.Sqrt, bias=1.0, scale=-1.0), reads=[Bss], writes=[Bss])
                    op("dve", lambda e: e.tensor_tensor(out=ii[:, :], in0=ii[:, :], in1=xc[:, :], op=ALU.mult), reads=[Bii, Bxc], writes=[Bii])
                    yield
                    op("dve", lambda e: e.tensor_tensor(out=ii[:, :], in0=ii[:, :], in1=ss[:, :], op=ALU.mult), reads=[Bii, Bss], writes=[Bii])
                    op("dve", lambda e: e.tensor_tensor_scan(out=rr[:, :], data0=aa[:, :], data1=ii[:, :], initial=hcar[:, c:c + 1],
                                                             op0=ALU.mult, op1=ALU.add), reads=[Baa, Bii, Bhcar[c]], writes=[Brr])
                    op("dve", lambda e: e.tensor_copy(out=hcar[:, c:c + 1], in_=rr[:, 511:512]), reads=[Brr], writes=[Bhcar[c]])
                    j2 = load_chunk(CH["grnn"][c]); bk3 = next_bank()
                    proj_fm(j2, 128, bk3, xnT, BxnT)
                    yield
                    op("act", lambda e: e.activation(out=sg[:, :], in_=ps[bk3][:, :], func=AF.Silu), reads=[Bps[bk3]], writes=[Bsg])
                    op("dve", lambda e: e.tensor_tensor(out=hgT[:, c, :], in0=rr[:, :], in1=sg[:, :], op=ALU.mult), reads=[Brr, Bsg], writes=[BhgT])

                def c_rounds():
                    for c in range(0, 8, 2):
                        g0, g1 = rnn_steps(c, 0), rnn_steps(c + 1, 1)
                        alive = [g0, g1]
                        while alive:
                            for g in list(alive):
                                try:
                                    next(g)
                                except StopIteration:
                                    alive.remove(g)
                            yield

                tk0 = topk_gen(0, tb * 4) if tb >= 1 else None
                tk0s = Stepper(tk0)
                bg, cg = b_steps(), c_rounds()
                b_done, c_done, rnd = False, False, 0
                _END = object()
                while not (b_done and c_done):
                    if not b_done:
                        b_done = next(bg, _END) is _END
                    if not c_done:
                        c_done = next(cg, _END) is _END
                    rnd += 1
                    if b_done and tk0 is not None and rnd % 6 == 0:
                        tk0s.seg()
                b_gattn()
                if tk0 is not None:
                    tk0s.drain()
                    nmask_build(0, tb * 4)
                chk(3)
                def attn_prologue(i, gt):
                    nsm, Bnsm = nsm_s[i % 2], Bnsm_s[i % 2]
                    if gt < 2:
                        op("dve", lambda e: e.tensor_scalar(out=nsm[:, :], in0=pidx[:, :], scalar1=-1.0, scalar2=-128.0 * gt, op0=ALU.mult, op1=ALU.add),
                           reads=[Bconst], writes=[Bnsm])
                    for hg in range(4):
                        def fql(e, hg=hg):
                            ins = None
                            for hl in range(4):
                                h = 4 * hg + hl
                                ins = e.matmul(ps[6][:, hl * 128:(hl + 1) * 128], lhsT=wukTz[:, h, :],
                                               rhs=qT[:, h // 2, i * 128:(i + 1) * 128], start=True, stop=True)
                            return ins
                        op("pe", fql, reads=[BqT, Bconst], writes=[Bps[6]])
                        op("act", lambda e, hg=hg: e.mul(out=qlb_s[hg][:, :], in_=ps[6][:, :], mul=0.125), reads=[Bps[6]], writes=[Bqlb_s[hg]])
                        op("act", lambda e: e.activation(out=absq[:, :], in_=ps[6][:, :], func=AF.Square, scale=0.125), reads=[Bps[6]], writes=[Babsq])
                        def fnm(e, hg=hg):
                            e.matmul(ps[4][0:1, :], lhsT=nhc_bf[:, 0:1], rhs=absq[:, :], start=True, stop=False)
                            return e.matmul(ps[4][0:1, :], lhsT=nsm[:, 0:1], rhs=SI[:, hg, :], start=False, stop=True)
                        op("pe", fnm, reads=[Babsq, Bnsm, Bconst], writes=[Bps[4]])
                        op("act", lambda e, hg=hg: e.activation(out=Bwork[hg][0:1, :], in_=ps[4][0:1, :], func=AF.Identity, bias=nhc[0:1, 0:1], scale=1.0),
                           reads=[Bps[4], Bconst], writes=[BBwork[hg]])

                def attn_hg(i, gt, hg, stp=None):
                    ns = gt + 1
                    qlb, Bqlb = qlb_s[hg], Bqlb_s[hg]
                    def flg_exp(js):
                        lb = js % 2
                        if gt >= 2:
                            mrhs, mbuf = nmask[:, js, :], Bnmask[js]
                        elif js == gt:
                            mrhs, mbuf = ntri_rep[:, :], Bconst
                        else:
                            mrhs, mbuf = None, None
                        def flg(e):
                            e.matmul(ps[lb][:, :], lhsT=cT[:, js * 128:(js + 1) * 128], rhs=qlb[:, :], start=True, stop=False)
                            if mrhs is None:
                                return e.matmul(ps[lb][:, :], lhsT=A_all[:, js, :], rhs=Bwork[hg][:, :], start=False, stop=True)
                            e.matmul(ps[lb][:, :], lhsT=A_all[:, js, :], rhs=Bwork[hg][:, :], start=False, stop=False)
                            return e.matmul(ps[lb][:, :], lhsT=ident[:, :], rhs=mrhs, start=False, stop=True)
                        op("pe", flg, reads=[BcT, Bqlb, BBwork[hg], Bconst] + ([mbuf] if mbuf is not None else []), writes=[Bps[lb]])
                        op("act", lambda e: e.activation(out=p_t[lb][:, :], in_=ps[lb][:, :], func=AF.Exp), reads=[Bps[lb]], writes=[Bp[lb]])
                    bo, bs_ = (2, 3) if hg % 2 == 0 else (4, 5)
                    def fpv_(js):
                        lb = js % 2
                        def fpv(e):
                            e.matmul(ps[bo][:, :], lhsT=c_tm[:, js, :], rhs=p_t[lb][:, :], start=(js == 0), stop=(js == ns - 1))
                            return e.matmul(ps[bs_][:, :], lhsT=ones_bf[:, :], rhs=p_t[lb][:, :], start=(js == 0), stop=(js == ns - 1))
                        op("pe", fpv, reads=[Bctm, Bp[lb], Bconst], writes=[Bps[bo], Bps[bs_]])
                    flg_exp(0)
                    for js in range(ns):
                        if js + 1 < ns:
                            flg_exp(js + 1)
                        fpv_(js)
                        if stp is not None:
                            stp.tick()
                    rs_t, Brs, oTn, BoTn = rs_s[hg % 2], Brs_s[hg % 2], oTn_s[hg % 2], BoTn_s[hg % 2]
                    op("dve", lambda e: e.reciprocal(out=rs_t[:, :], in_=ps[bs_][:, :]), reads=[Bps[bs_]], writes=[Brs])
                    op("dve", lambda e: e.tensor_tensor(out=oTn[:, :], in0=ps[bo][:, :], in1=rs_t[:, :], op=ALU.mult), reads=[Bps[bo], Brs], writes=[BoTn])

                def attn_epi(i, hg):
                    oTn, BoTn = oTn_s[hg % 2], BoTn_s[hg % 2]
                    def fy(e):
                        ins = None
                        for pp in range(2):
                            for q2 in range(2):
                                hl = 2 * pp + q2
                                h = 4 * hg + hl
                                ins = e.matmul(ps[6][:, pp * 128:(pp + 1) * 128], lhsT=wuvP[:, h, :], rhs=oTn[:, hl * 128:(hl + 1) * 128],
                                               start=(q2 == 0), stop=(q2 == 1))
                        return ins
                    op("pe", fy, reads=[BoTn, Bconst], writes=[Bps[6]])
                    for pp in range(2):
                        cc = 2 * hg + pp
                        op("dve", lambda e, pp=pp, cc=cc: e.tensor_tensor(out=sgT[:, cc, i * 128:(i + 1) * 128], in0=ps[6][:, pp * 128:(pp + 1) * 128],
                                                                           in1=sgT[:, cc, i * 128:(i + 1) * 128], op=ALU.mult),
                           reads=[Bps[6], BsgT[cc]], writes=[BsgT[cc]])

                gens = {}
                for i in range(1, 4):
                    if tb * 4 + i >= 2:
                        gens[i] = topk_gen(i, tb * 4 + i)
                attn_prologue(0, tb * 4)
                for i in range(4):
                    gt = tb * 4 + i
                    g = gens.get(i + 1)
                    stp = Stepper(g)
                    for hg in range(4):
                        attn_hg(i, gt, hg, stp)
                        if hg > 0:
                            attn_epi(i, hg - 1)
                        if g is not None and hg >= 1:
                            stp.seg()
                    stp.drain()
                    if i + 1 < 4:
                        attn_prologue(i + 1, gt + 1)
                    if g is not None:
                        nmask_build(i + 1, gt + 1)
                    attn_epi(i, 3)

                chk(4)
                mixedT, BmixedT = qT, BqT
                for f in range(8):
                    ja = load_chunk(CH["wap"][f])
                    def fya(e, ja=ja):
                        ins = None
                        for k in range(8):
                            ins = e.matmul(ps[0][:, :], lhsT=wch[ja][:, k, :], rhs=sgT[:, k, :], start=(k == 0), stop=(k == 7))
                        return ins
                    op("pe", fya, reads=[Bwch[ja]] + BsgT, writes=[Bps[0]])
                    jr = load_chunk(CH["wrp"][f])
                    proj_fm(jr, 128, 1, hgT, BhgT)
                    jg = load_chunk(CH["mga"][f])
                    proj_fm(jg, 128, 2, xnT, BxnT)
                    op("act", lambda e, f=f: e.activation(out=ga_t[:, :], in_=ps[2][:, :], func=AF.Sigmoid, bias=bmcol[:, f:f + 1], scale=1.0),
                       reads=[Bps[2], Bconst], writes=[Bga])
                    jg2 = load_chunk(CH["mgr"][f])
                    proj_fm(jg2, 128, 3, xnT, BxnT)
                    op("act", lambda e, f=f: e.activation(out=gr_t[:, :], in_=ps[3][:, :], func=AF.Sigmoid, bias=bmcol[:, 8 + f:9 + f], scale=1.0),
                       reads=[Bps[3], Bconst], writes=[Bgr])
                    op("dve", lambda e: e.tensor_tensor(out=m1_t[:, :], in0=ps[0][:, :], in1=ga_t[:, :], op=ALU.mult), reads=[Bps[0], Bga], writes=[Bm1])
                    op("dve", lambda e: e.tensor_tensor(out=m2_t[:, :], in0=ps[1][:, :], in1=gr_t[:, :], op=ALU.mult), reads=[Bps[1], Bgr], writes=[Bm2])
                    op("pool", lambda e, f=f: e.tensor_tensor(out=mixedT[:, f, :], in0=m1_t[:, :], in1=m2_t[:, :], op=ALU.add), reads=[Bm1, Bm2], writes=[BmixedT])
                for i in range(4):
                    xb = i % 2
                    rk = i % 2
                    res_t = I_t[:, rk * 1024:(rk + 1) * 1024]
                    S_.dma("sp", "xl%d" % xb, xt[xb][:, :], x[sq, t0 + i * 128:t0 + (i + 1) * 128, :], writes=[Bxt[xb]])
                    for db in range(2):
                        def fo(e, i=i, db=db):
                            ins = None
                            for k in range(8):
                                ins = e.matmul(ps[4 + db][:, :], lhsT=mixedT[:, k, i * 128:(i + 1) * 128], rhs=wout_sb[:, k, db * 512:(db + 1) * 512],
                                               start=(k == 0), stop=(k == 7))
                            return ins
                        op("pe", fo, reads=[BmixedT, Bconst], writes=[Bps[4 + db]])
                        op("dve", lambda e, db=db, xb=xb, res_t=res_t: e.tensor_tensor(out=res_t[:, db * 512:(db + 1) * 512], in0=ps[4 + db][:, :],
                                                                                       in1=xt[xb][:, db * 512:(db + 1) * 512], op=ALU.add),
                           reads=[Bps[4 + db], Bxt[xb]], writes=[Bres[rk], BI])
                    op("act", lambda e, res_t=res_t: e.activation(out=junk[:, 0:1024], in_=res_t, func=AF.Square, accum_out=st[:, 2:3]),
                       reads=[Bres[rk]], writes=[Bjunk_act, Bst])
                    rstd_from_ss(st, Bst, 2, 3, 1024.0)
                    op("dve", lambda e, res_t=res_t: e.scalar_tensor_tensor(out=res_t, in0=res_t, scalar=st[:, 3:4], in1=fg_bc[:, :],
                                                                            op0=ALU.mult, op1=ALU.mult), reads=[Bres[rk], Bst, Bconst], writes=[Bres[rk]])
                    S_.dma("pool", "os%d" % rk, out[sq, t0 + i * 128:t0 + (i + 1) * 128, :], res_t, reads=[Bres[rk]])
    except _Stop:
        pass
    for key in sorted(S_.dsem):
        nc.gpsimd.wait_ge(S_.dsem[key][0], S_.dsem[key][1])
    for k_ in S_.sem:
        if S_.cnt[k_] > 0 and k_ != "pool":
            nc.gpsimd.wait_ge(S_.sem[k_], S_.cnt[k_])
    return nc


_PARAMS = ["norm_gain", "w_in", "b_merge", "kv_norm_gain", "w_uk", "w_uv", "idx_ln_gain", "idx_ln_bias", "w_attn_proj",
           "conv_w", "conv_b", "w_rg_a", "b_rg_a", "w_rg_x", "b_rg_x", "lru_lambda", "w_rnn_proj", "w_out"]


def kernel(**inputs):
    x = np.ascontiguousarray(np.asarray(inputs["x"], dtype=np.float32))
    B, S, _ = x.shape
    nseq = B // NCORES
    base = {}
    for k in _PARAMS:
        a = np.asarray(inputs[k], dtype=np.float32)
        base[k] = np.ascontiguousarray(a.reshape(a.shape[1:]))
    base["final_norm_gain"] = np.ascontiguousarray(np.asarray(inputs["final_norm_gain"], dtype=np.float32))
    nc = build(nseq, S)
    in_maps = []
    for c in range(NCORES):
        m = dict(base)
        m["x"] = np.ascontiguousarray(x[c * nseq:(c + 1) * nseq])
        in_maps.append(m)
    res = run_bass_kernel_spmd(nc, in_maps, core_ids=list(range(NCORES)))
    return np.concatenate([np.asarray(r["out"], dtype=np.float32) for r in res.results], axis=0)
```

```python
import numpy as np
import concourse.bass as bass
import concourse.mybir as mybir
from concourse.bass_utils import run_bass_kernel_spmd

F32 = mybir.dt.float32
BF16 = mybir.dt.bfloat16
ALU = mybir.AluOpType
AF = mybir.ActivationFunctionType
AX = mybir.AxisListType

D = 1024
NCORES = 8
EPS = 1e-6
TOPK = 256
NBIS = 12


class Buf:
    __slots__ = ("name", "w", "r")

    def __init__(self, name):
        self.name = name
        self.w = None
        self.r = []


class Sched:
    def __init__(self, nc):
        self.nc = nc
        self.eng = {"pe": nc.tensor, "act": nc.scalar, "dve": nc.vector, "pool": nc.gpsimd, "sp": nc.sync}
        self.sem, self.cnt, self.waited, self.dsem = {}, {}, {}, {}
        for k in self.eng:
            self.sem[k] = nc.semaphore("s_" + k).__enter__()
            self.cnt[k] = 0
            self.waited[k] = {}

    def _wait(self, e, dep):
        de, val = dep
        if de == e and e == "pe":
            return
        if self.waited[e].get(de, 0) >= val:
            return
        self.waited[e][de] = val
        s = self.dsem[de][0] if de.startswith("dma:") else self.sem[de]
        self.eng[e].wait_ge(s, val)

    def _deps(self, e, reads, writes):
        deps = set()
        for b in reads:
            if b.w is not None:
                deps.add(b.w)
        for b in writes:
            if b.w is not None:
                deps.add(b.w)
            deps.update(b.r)
        for d in sorted(deps):
            self._wait(e, d)

    def _mark(self, me, reads, writes):
        for b in reads:
            if len(b.r) > 24:
                last = {}
                for (q, v) in b.r:
                    last[q] = max(last.get(q, 0), v)
                b.r = list(last.items())
            b.r.append(me)
        for b in writes:
            b.w = me
            b.r = []

    def op(self, e, fn, reads=(), writes=()):
        self._deps(e, reads, writes)
        ins = fn(self.eng[e])
        self.cnt[e] += 1
        ins.then_inc(self.sem[e], 1)
        me = (e, self.cnt[e])
        self._mark(me, reads, writes)
        return me

    def dma(self, e, q, out, in_, reads=(), writes=()):
        key = "dma:" + q
        if key not in self.dsem:
            self.dsem[key] = [self.nc.semaphore("d_" + q).__enter__(), 0]
        self._deps(e, reads, writes)
        ins = self.eng[e].dma_start(out=out, in_=in_)
        self.dsem[key][1] += 16
        ins.then_inc(self.dsem[key][0], 16)
        me = (key, self.dsem[key][1])
        self._mark(me, reads, writes)
        return me


class Stepper:
    def __init__(self, g):
        self.g, self.idx = g, True
    def tick(self):
        if self.g is not None and self.idx:
            if next(self.g, None) != "idx":
                self.idx = False
    def seg(self):
        if self.g is None:
            return
        while self.idx:
            self.tick()
        next(self.g, None)
    def drain(self):
        if self.g is not None:
            for _ in self.g:
                pass


def _bf16_split(v):
    import ml_dtypes
    hi = float(np.float32(v).astype(ml_dtypes.bfloat16))
    lo = float(np.float32(v - hi).astype(ml_dtypes.bfloat16))
    return hi, lo


class _Stop(Exception):
    pass


def build(nseq, S, debug=False, stop=None):
    NBLK = S // 512
    NTS = S // 128
    nc = bass.Bass("TRN2", target_bir_lowering=False)
    S_ = Sched(nc)

    def din(name, shape):
        return nc.dram_tensor(name, list(shape), F32, kind="ExternalInput").ap()

    x = din("x", [nseq, S, D])
    norm_gain = din("norm_gain", [D])
    w_in = din("w_in", [D, 6856])
    b_merge = din("b_merge", [2048])
    kv_norm_gain = din("kv_norm_gain", [128])
    w_uk = din("w_uk", [16, 128, 64])
    w_uv = din("w_uv", [16, 128, 64])
    idx_ln_gain = din("idx_ln_gain", [64])
    idx_ln_bias = din("idx_ln_bias", [64])
    w_attn_proj = din("w_attn_proj", [D, D])
    conv_w = din("conv_w", [4, D])
    conv_b = din("conv_b", [D])
    w_rg_a = din("w_rg_a", [16, 64, 64])
    b_rg_a = din("b_rg_a", [D])
    w_rg_x = din("w_rg_x", [16, 64, 64])
    b_rg_x = din("b_rg_x", [D])
    lru_lambda = din("lru_lambda", [D])
    w_rnn_proj = din("w_rnn_proj", [D, D])
    w_out = din("w_out", [D, D])
    final_norm_gain = din("final_norm_gain", [D])
    out = nc.dram_tensor("out", [nseq, S, D], F32, kind="ExternalOutput").ap()
    wbf = nc.dram_tensor("wbf", [70, 128, 1024], BF16, kind="Internal").ap()

    def sb(name, shape, dt=F32):
        return nc.sbuf_tensor(name, list(shape), dt).__enter__()

    CH = {}
    chunk_src = []

    def add_chunks(name, src, col0, n, width=128):
        CH[name] = []
        for c in range(n):
            CH[name].append(len(chunk_src))
            chunk_src.append((src, col0 + c * 128, width))

    add_chunks("q", w_in, 0, 8)
    add_chunks("ckv", w_in, 1024, 1)
    add_chunks("qidx", w_in, 1152, 4)
    add_chunks("kw", w_in, 1664, 1, 72)
    add_chunks("gattn", w_in, 1736, 8)
    add_chunks("xrnn", w_in, 2760, 8)
    add_chunks("grnn", w_in, 3784, 8)
    add_chunks("mga", w_in, 4808, 8)
    add_chunks("mgr", w_in, 5832, 8)
    add_chunks("wap", w_attn_proj, 0, 8)
    add_chunks("wrp", w_rnn_proj, 0, 8)
    assert len(chunk_src) == 70
    Bwbf = [Buf("wbf%d" % i) for i in range(70)]

    ident = sb("ident", [128, 128], BF16); Bident = Buf("ident")
    ones_bf = sb("ones_bf", [128, 128], BF16)
    triT = sb("triT", [128, 128], BF16)
    causal_neg = sb("causal_neg", [128, 128])
    iota_row = sb("iota_row", [128, 128])
    pidx = sb("pidx", [128, 1])
    Bconst = Buf("const")

    wout_sb = sb("wout_sb", [128, 8, 1024], BF16)
    wuvP = sb("wuvP", [128, 16, 128], BF16)
    wuk_nat = sb("wuk_nat", [128, 16, 64], BF16)
    wukTz = sb("wukTz", [128, 16, 128], BF16)
    BDa = sb("BDa", [128, 8, 128], BF16)
    BDx = sb("BDx", [128, 8, 128], BF16)
    gcol = sb("gcol", [128, 8])
    bmcol = sb("bmcol", [128, 16])
    cwcol = sb("cwcol", [128, 4, 8])
    cbcol = sb("cbcol", [128, 8])
    bacol = sb("bacol", [128, 8])
    bxcol = sb("bxcol", [128, 8])
    lamcol = sb("lamcol", [128, 8])
    kap = sb("kap", [128, 8])
    kap2 = sb("kap2", [128, 8])
    fg_bc = sb("fg_bc", [128, 1024])
    gkv_bc = sb("gkv_bc", [128, 128])
    lng_bc = sb("lng_bc", [128, 64])
    lnb_bc = sb("lnb_bc", [128, 64])
    A_all = sb("A_all", [32, 16, 128], BF16)
    Bcol_hi = sb("Bcol_hi", [32, 16])
    Bcol_lo = sb("Bcol_lo", [32, 16])
    Bcol = sb("Bcol", [32, 16])
    e0 = sb("e0", [32, 1]); e12 = sb("e12", [32, 1]); e34 = sb("e34", [32, 1])
    e13 = sb("e13", [32, 1]); e24 = sb("e24", [32, 1]); etmp = sb("etmp", [32, 1])
    cold = sb("cold", [32, 16])
    Bwork = [sb("Bwork%d" % g, [32, 512], BF16) for g in range(4)]
    BBwork = [Buf("Bwork%d" % g) for g in range(4)]
    pow2 = sb("pow2", [128, NBIS])
    nhc = sb("nhc", [128, 1]); nhc_bf = sb("nhc_bf", [128, 1], BF16)
    nb32 = sb("nb32", [128, 1])

    cT = sb("cT", [128, S], BF16); BcT = Buf("cT")
    c_tm = sb("c_tm", [128, NTS, 128], BF16); Bctm = Buf("c_tm")
    kz = [sb("kz%d" % i, [128, S], BF16) for i in range(2)]; BkT = Buf("kidxT")
    w_tm = sb("w_tm", [128, NTS, 8]); Bwtm = Buf("w_tm")
    cabs = sb("cabs", [128, 1]); cabs_b = sb("cabs_b", [128, 1]); ncabs = sb("ncabs", [128, 1], BF16)
    Bcabs = Buf("cabs")
    xnT = sb("xnT", [128, 8, 512], BF16); BxnT = Buf("xnT")
    qT = sb("qT", [128, 8, 512], BF16); BqT = Buf("qT")
    sgT = sb("sgT", [128, 8, 512], BF16); BsgT = [Buf("sgT%d" % c) for c in range(8)]
    hgT = sb("hgT", [128, 8, 512], BF16); BhgT = Buf("hgT")
    qidxT = sb("qidxT", [128, 4, 512], BF16); BqidxT = Buf("qidxT")
    wch = [sb("wch%d" % i, [128, 8, 128], BF16) for i in range(4)]
    Bwch = [Buf("wch%d" % i) for i in range(4)]
    xt = [sb("xt%d" % i, [128, 1024]) for i in range(2)]
    Bxt = [Buf("xt%d" % i) for i in range(2)]
    xs = sb("xs", [128, 1024], BF16); Bxs = Buf("xs")
    junk = sb("junk", [128, 2048], BF16); Bjunk_act = Buf("junk_act"); Bjunk_dve = Buf("junk_dve")
    st = sb("st", [128, 16]); Bst = Buf("st")
    st2 = sb("st2", [128, 16]); Bst2 = Buf("st2")
    kc = sb("kc", [128, 64]); Bkc = Buf("kc")
    kn2 = sb("kn2", [128, 128], BF16); Bkn2 = Buf("kn2")
    ub = [sb("ub%d" % k, [128, 515], BF16) for k in range(2)]; Bub = [Buf("ub%d" % k) for k in range(2)]
    ucar = sb("ucar", [128, 8, 3], BF16); Bucar = [Buf("ucar%d" % c) for c in range(8)]
    xc_s = [sb("xc%d" % k, [128, 512]) for k in range(2)]; Bxc_s = [Buf("xc%d" % k) for k in range(2)]
    xcb_s = [sb("xcb%d" % k, [128, 512], BF16) for k in range(2)]; Bxcb_s = [Buf("xcb%d" % k) for k in range(2)]
    r_s = [sb("r%d" % k, [128, 512]) for k in range(2)]; Br_s = [Buf("r%d" % k) for k in range(2)]
    i_s = [sb("i%d" % k, [128, 512]) for k in range(2)]; Bi_s = [Buf("i%d" % k) for k in range(2)]
    a_s = [sb("a%d" % k, [128, 512]) for k in range(2)]; Ba_s = [Buf("a%d" % k) for k in range(2)]
    s_s = [sb("s%d" % k, [128, 512]) for k in range(2)]; Bs_s = [Buf("s%d" % k) for k in range(2)]
    sgr_s = [sb("sgr%d" % k, [128, 512], BF16) for k in range(2)]; Bsgr_s = [Buf("sgr%d" % k) for k in range(2)]
    r_t, Br, i_t, Bi, a_t, Ba, s_t, Bs = r_s[0], Br_s[0], i_s[0], Bi_s[0], a_s[0], Ba_s[0], s_s[0], Bs_s[0]
    ptmp, Bptmp = s_s[1], Bs_s[1]
    hcar = sb("hcar", [128, 8]); Bhcar = [Buf("hcar%d" % c) for c in range(8)]
    I_t = sb("I_t", [128, 2048]); BI = Buf("I")
    rj = [sb("rj%d" % i, [128, 512]) for i in range(2)]; Brj = [Buf("rj%d" % i) for i in range(2)]
    mask = sb("mask", [128, 2048], BF16); Bmask = Buf("mask")
    nmask = sb("nmask", [128, 16, 512], BF16); Bnmask = [Buf("nmask%d" % j) for j in range(16)]
    identrep4 = sb("identrep4", [128, 512], BF16)
    SI = sb("SI", [128, 4, 512], BF16)
    iota1_full = sb("iota1_full", [128, 2048], BF16)
    ntri_rep = sb("ntri_rep", [128, 512], BF16)
    nsm_s = [sb("nsm%d" % k, [128, 1], BF16) for k in range(2)]; Bnsm_s = [Buf("nsm%d" % k) for k in range(2)]
    bis_s = [sb("bis%d" % k, [128, 8]) for k in range(2)]; Bbis_s = [Buf("bis%d" % k) for k in range(2)]
    steps = sb("steps", [128, NBIS]); steps2 = sb("steps2", [128, NBIS]); Bsteps = Buf("steps")
    qlb_s = [sb("qlb%d" % k, [128, 512], BF16) for k in range(4)]; Bqlb_s = [Buf("qlb%d" % k) for k in range(4)]
    absq_s = [sb("absq%d" % k, [128, 512], BF16) for k in range(2)]; Babsq_s = [Buf("absq%d" % k) for k in range(2)]
    p_t = [sb("p%d" % i, [128, 512], BF16) for i in range(2)]; Bp = [Buf("p%d" % i) for i in range(2)]
    rs_s = [sb("rs%d" % k, [128, 512]) for k in range(2)]; Brs_s = [Buf("rs%d" % k) for k in range(2)]
    oTn_s = [sb("oTn%d" % k, [128, 512], BF16) for k in range(2)]; BoTn_s = [Buf("oTn%d" % k) for k in range(2)]
    ga_t, Bga = r_t, Br
    gr_t, Bgr = i_t, Bi
    m1_t, Bm1 = a_t, Ba
    m2_t, Bm2 = s_t, Bs
    Bres = [Buf("res0"), Buf("res1")]

    ps = [nc.psum_tensor("ps%d" % i, [128, 512], F32).__enter__() for i in range(8)]
    Bps = [Buf("ps%d" % i) for i in range(8)]

    def psbf(i):
        return ps[i][:, :].bitcast(BF16)

    op = S_.op

    with nc.allow_non_contiguous_dma(reason="one-time small parameter layouts"):
        for ci, (src, col0, wd) in enumerate(chunk_src):
            S_.dma("pool", "cv", wbf[ci].rearrange("p (k j) -> p k j", k=8)[:, :, 0:wd],
                   src.rearrange("(k p) n -> p k n", p=128)[:, :, col0:col0 + wd], writes=[Bwbf[ci]])
        for b_ in Bwbf:
            b_.w = ("dma:cv", S_.dsem["dma:cv"][1])
        S_.dma("pool", "cs", wout_sb[:, :, :], w_out.rearrange("(k p) n -> p k n", p=128), writes=[Bconst])
        op("pool", lambda e: e.memset(wuvP[:, :, :], 0.0), writes=[Bconst])
        for par in range(2):
            S_.dma("pool", "cs", wuvP[:, :, :].rearrange("c (hp two) d -> c hp two d", two=2)[:, :, par, par * 64:par * 64 + 64],
                   w_uv.rearrange("(hp two) c d -> c hp two d", two=2)[:, :, par, :], reads=[Bconst], writes=[Bconst])
        S_.dma("pool", "cs", wuk_nat[:, :, :], w_uk.rearrange("h c d -> c h d"), writes=[Bconst])
        op("pool", lambda e: e.memset(BDa[:, :, :], 0.0), writes=[Bconst])
        op("pool", lambda e: e.memset(BDx[:, :, :], 0.0), writes=[Bconst])
        for (bd, wsrc) in ((BDa, w_rg_a), (BDx, w_rg_x)):
            for par in range(2):
                S_.dma("pool", "cs", bd[par * 64:par * 64 + 64, :, par * 64:par * 64 + 64],
                       wsrc.rearrange("(p two) i j -> two i p j", two=2)[par], reads=[Bconst], writes=[Bconst])
        for (dst, src, n) in ((gcol, norm_gain, 8), (bmcol, b_merge, 16), (cbcol, conv_b, 8), (bacol, b_rg_a, 8),
                              (bxcol, b_rg_x, 8), (lamcol, lru_lambda, 8)):
            S_.dma("sp", "cs2", dst[:, :], src.rearrange("(c p) -> p c", p=128), writes=[Bconst])
        S_.dma("sp", "cs2", cwcol[:, :, :], conv_w.rearrange("k (c p) -> p k c", p=128), writes=[Bconst])
        S_.dma("sp", "cs2", fg_bc[:, :], final_norm_gain.partition_broadcast(128), writes=[Bconst])
        S_.dma("sp", "cs2", gkv_bc[:, :], kv_norm_gain.partition_broadcast(128), writes=[Bconst])
        S_.dma("sp", "cs2", lng_bc[:, :], idx_ln_gain.partition_broadcast(128), writes=[Bconst])
        S_.dma("sp", "cs2", lnb_bc[:, :], idx_ln_bias.partition_broadcast(128), writes=[Bconst])

    for par in range(2):
        op("pool", lambda e, par=par: e.memset(kz[par][:, :], 0.0), writes=[BkT])
    op("pool", lambda e: e.iota(iota_row[:, :], [[1, 128]], base=0, channel_multiplier=0,
                                allow_small_or_imprecise_dtypes=True), writes=[Bconst])
    op("pool", lambda e: e.iota(pidx[:, :], [[0, 1]], base=0, channel_multiplier=1,
                                allow_small_or_imprecise_dtypes=True), reads=[Bconst], writes=[Bconst])
    op("dve", lambda e: e.tensor_scalar(out=ident[:, :], in0=iota_row[:, :], scalar1=pidx[:, 0:1], scalar2=None,
                                        op0=ALU.is_equal), reads=[Bconst], writes=[Bconst, Bident])
    op("dve", lambda e: e.tensor_scalar(out=triT[:, :], in0=iota_row[:, :], scalar1=pidx[:, 0:1], scalar2=None,
                                        op0=ALU.is_ge), reads=[Bconst], writes=[Bconst])
    op("dve", lambda e: e.tensor_scalar(out=causal_neg[:, :], in0=iota_row[:, :], scalar1=pidx[:, 0:1], scalar2=-1e30,
                                        op0=ALU.is_gt, op1=ALU.mult), reads=[Bconst], writes=[Bconst])
    op("dve", lambda e: e.memset(ones_bf[:, :], 1.0), reads=[Bconst], writes=[Bconst])
    for k in range(NBIS):
        op("dve", lambda e, k=k: e.memset(pow2[:, k:k + 1], 2.0 ** -(k + 1)), reads=[Bconst], writes=[Bconst])
    op("dve", lambda e: e.tensor_reduce(out=nhc[:, 0:1], in_=gkv_bc[:, :], axis=AX.X, op=ALU.max, apply_absolute_value=True),
       reads=[Bconst], writes=[Bconst])
    op("dve", lambda e: e.tensor_scalar(out=nhc[:, 0:1], in0=nhc[:, 0:1], scalar1=-0.5 * (128.0 ** 0.5), scalar2=None, op0=ALU.mult),
       reads=[Bconst], writes=[Bconst])
    op("dve", lambda e: e.tensor_copy(out=nhc_bf[:, 0:1], in_=nhc[:, 0:1]), reads=[Bconst], writes=[Bconst])
    op("dve", lambda e: e.memset(nb32[:, :], -32768.0), reads=[Bconst], writes=[Bconst])
    op("act", lambda e: e.activation(out=kap[:, :], in_=lamcol[:, :], func=AF.Exp, scale=-1.0), reads=[Bconst], writes=[Bconst])
    op("act", lambda e: e.activation(out=kap[:, :], in_=kap[:, :], func=AF.Ln, bias=1.0, scale=1.0), reads=[Bconst], writes=[Bconst])
    op("dve", lambda e: e.tensor_scalar(out=kap2[:, :], in0=kap[:, :], scalar1=-16.0, scalar2=None, op0=ALU.mult), reads=[Bconst], writes=[Bconst])
    op("dve", lambda e: e.tensor_scalar(out=kap[:, :], in0=kap[:, :], scalar1=-8.0, scalar2=None, op0=ALU.mult), reads=[Bconst], writes=[Bconst])
    for pr in range(8):
        op("pe", lambda e, pr=pr: e.transpose(out=psbf(0)[:, pr * 128:(pr + 1) * 128],
                                              in_=wuk_nat[:, 2 * pr:2 * pr + 2, :].rearrange("c h d -> c (h d)"),
                                              identity=ident[:, :]), reads=[Bconst], writes=[Bps[0]])
    op("dve", lambda e: e.memset(wukTz[:, :, :], 0.0), reads=[Bconst], writes=[Bconst])
    for par in range(2):
        op("dve", lambda e, par=par: e.tensor_copy(
            out=wukTz[par * 64:par * 64 + 64, :, :].rearrange("p (pr two) c -> p pr two c", two=2)[:, :, par, :],
            in_=psbf(0)[par * 64:par * 64 + 64, :].rearrange("p (pr c) -> p pr c", c=128)), reads=[Bps[0], Bconst], writes=[Bconst])
    def sel(dst, ks):
        for n, k in enumerate(ks):
            tgt = dst if n == 0 else etmp
            op("dve", lambda e, k=k, tgt=tgt: e.tensor_scalar(out=tgt[:, :], in0=pidx[0:32, :], scalar1=float(k), scalar2=None,
                                                             op0=ALU.is_equal), reads=[Bconst], writes=[Bconst])
            if n > 0:
                op("dve", lambda e: e.tensor_tensor(out=dst[:, :], in0=dst[:, :], in1=etmp[:, :], op=ALU.add), reads=[Bconst], writes=[Bconst])
    sel(e0, [0]); sel(e12, [1, 2]); sel(e34, [3, 4]); sel(e13, [1, 3]); sel(e24, [2, 4])
    slopes = [2.0 ** (-8.0 * (h + 1) / 16.0) for h in range(16)]
    for h in range(16):
        hi, lo = _bf16_split(slopes[h])
        op("dve", lambda e, h=h, hi=hi: e.memset(Bcol_hi[:, h:h + 1], hi), reads=[Bconst], writes=[Bconst])
        op("dve", lambda e, h=h, lo=lo: e.memset(Bcol_lo[:, h:h + 1], lo), reads=[Bconst], writes=[Bconst])
    op("dve", lambda e: e.tensor_scalar(out=Bcol[:, :], in0=Bcol_hi[:, :], scalar1=e13[:, 0:1], scalar2=None, op0=ALU.mult), reads=[Bconst], writes=[Bconst])
    op("dve", lambda e: e.scalar_tensor_tensor(out=Bcol[:, :], in0=Bcol_lo[:, :], scalar=e24[:, 0:1], in1=Bcol[:, :],
                                               op0=ALU.mult, op1=ALU.add), reads=[Bconst], writes=[Bconst])
    for g in range(4):
        for hl in range(4):
            h = 4 * g + hl
            op("dve", lambda e, g=g, hl=hl, h=h: e.tensor_scalar(out=Bwork[g][:, hl * 128:(hl + 1) * 128], in0=ones_bf[0:32, :],
                                                                  scalar1=Bcol[:, h:h + 1], scalar2=None, op0=ALU.mult),
               reads=[Bconst], writes=[BBwork[g]])
    for dl in range(16):
        op("dve", lambda e, dl=dl: e.tensor_scalar(out=cold[:, dl:dl + 1], in0=e12[:, :], scalar1=128.0 * dl, scalar2=None,
                                                   op0=ALU.mult), reads=[Bconst], writes=[Bconst])
        op("dve", lambda e, dl=dl: e.tensor_tensor(out=cold[:, dl:dl + 1], in0=cold[:, dl:dl + 1], in1=e0[:, :], op=ALU.add), reads=[Bconst], writes=[Bconst])
        op("dve", lambda e, dl=dl: e.tensor_scalar(out=A_all[:, dl, :], in0=iota_row[0:32, :], scalar1=e34[:, 0:1], scalar2=cold[:, dl:dl + 1],
                                                   op0=ALU.mult, op1=ALU.add), reads=[Bconst], writes=[Bconst])

    for r4 in range(4):
        op("dve", lambda e, r4=r4: e.tensor_copy(out=identrep4[:, r4 * 128:(r4 + 1) * 128], in_=ident[:, :]), reads=[Bconst], writes=[Bconst])
        op("dve", lambda e, r4=r4: e.tensor_scalar(out=ntri_rep[:, r4 * 128:(r4 + 1) * 128], in0=triT[:, :], scalar1=-1.0, scalar2=32768.0,
                                                   op0=ALU.add, op1=ALU.mult), reads=[Bconst], writes=[Bconst])
    for h in range(16):
        op("dve", lambda e, h=h: e.tensor_scalar(out=SI[:, h // 4, (h % 4) * 128:(h % 4 + 1) * 128], in0=ident[:, :], scalar1=slopes[h], scalar2=None,
                                                 op0=ALU.mult), reads=[Bconst], writes=[Bconst])
    op("pool", lambda e: e.iota(iota1_full[:, :], [[1, 2048]], base=1, channel_multiplier=0,
                                allow_small_or_imprecise_dtypes=True), reads=[Bconst], writes=[Bconst])

    wslot = [0]

    def load_chunk(ci):
        j = wslot[0] % 4
        wslot[0] += 1
        wd = chunk_src[ci][2]
        if wd == 128:
            S_.dma("sp", "wl%d" % j, wch[j][:, :, :].rearrange("p k j -> p (k j)"), wbf[ci], reads=[Bwbf[ci]], writes=[Bwch[j]])
        else:
            S_.dma("sp", "wl%d" % j, wch[j][:, :, 0:wd], wbf[ci].rearrange("p (k j) -> p k j", k=8)[:, :, 0:wd],
                   reads=[Bwbf[ci]], writes=[Bwch[j]])
        return j

    def proj_fm(j, wd, bank, rhs_t, rhs_b):
        def f(e):
            ins = None
            for k in range(8):
                ins = e.matmul(ps[bank][0:wd, :], lhsT=wch[j][:, k, 0:wd], rhs=rhs_t[:, k, :], start=(k == 0), stop=(k == 7))
            return ins
        op("pe", f, reads=[Bwch[j], rhs_b], writes=[Bps[bank]])

    def rstd_from_ss(stt, Bstt, col_ss, col_out, n):
        op("act", lambda e: e.activation(out=stt[:, col_out:col_out + 1], in_=stt[:, col_ss:col_ss + 1], func=AF.Sqrt,
                                         bias=EPS, scale=1.0 / n), reads=[Bstt], writes=[Bstt])
        op("dve", lambda e: e.reciprocal(out=stt[:, col_out:col_out + 1], in_=stt[:, col_out:col_out + 1]), reads=[Bstt], writes=[Bstt])

    bankrr = [0]

    def next_bank():
        b = (0, 1, 4, 5)[bankrr[0] % 4]
        bankrr[0] += 1
        return b

    dbg = {}

    def chk(n):
        if stop is not None and stop == n:
            raise _Stop()

    try:
        chk(0)
        for sq in range(nseq):
            for tb in range(NBLK):
                t0 = tb * 512
                for i in range(4):
                    xb = i % 2
                    S_.dma("sp", "xl%d" % xb, xt[xb][:, :], x[sq, t0 + i * 128:t0 + (i + 1) * 128, :], writes=[Bxt[xb]])
                    op("act", lambda e, xb=xb: e.activation(out=junk[:, 0:1024], in_=xt[xb][:, :], func=AF.Square, accum_out=st[:, 0:1]),
                       reads=[Bxt[xb]], writes=[Bjunk_act, Bst])
                    rstd_from_ss(st, Bst, 0, 1, 1024.0)
                    op("dve", lambda e, xb=xb: e.tensor_scalar(out=xs[:, :], in0=xt[xb][:, :], scalar1=st[:, 1:2], scalar2=None, op0=ALU.mult),
                       reads=[Bxt[xb], Bst], writes=[Bxs])
                    def ftr(e):
                        ins = None
                        for k in range(8):
                            ins = e.transpose(out=psbf(2)[:, k * 128:(k + 1) * 128], in_=xs[:, k * 128:(k + 1) * 128], identity=ident[:, :])
                        return ins
                    op("pe", ftr, reads=[Bxs, Bident], writes=[Bps[2]])
                    for k in range(8):
                        op("dve", lambda e, k=k, i=i: e.tensor_scalar(out=xnT[:, k, i * 128:(i + 1) * 128], in0=psbf(2)[:, k * 128:(k + 1) * 128],
                                                                       scalar1=gcol[:, k:k + 1], scalar2=None, op0=ALU.mult),
                           reads=[Bps[2], Bconst], writes=[BxnT])

                chk(1)
                def b_steps():
                    for c in range(8):
                        j = load_chunk(CH["q"][c]); bk = next_bank()
                        proj_fm(j, 128, bk, xnT, BxnT)
                        op("act", lambda e, c=c, bk=bk: e.copy(out=qT[:, c, :], in_=ps[bk][:, :]), reads=[Bps[bk]], writes=[BqT])
                        yield
                    for c in range(4):
                        j = load_chunk(CH["qidx"][c]); bk = next_bank()
                        proj_fm(j, 128, bk, xnT, BxnT)
                        op("act", lambda e, c=c, bk=bk: e.mul(out=qidxT[:, c, :], in_=ps[bk][:, :], mul=0.125),
                           reads=[Bps[bk]], writes=[BqidxT])
                        yield
                    j = load_chunk(CH["ckv"][0])
                    for i in range(4):
                        gt = tb * 4 + i
                        def fc(e, i=i, j=j):
                            ins = None
                            for k in range(8):
                                ins = e.matmul(ps[2][:, 0:128], lhsT=xnT[:, k, i * 128:(i + 1) * 128], rhs=wch[j][:, k, :], start=(k == 0), stop=(k == 7))
                            return ins
                        op("pe", fc, reads=[Bwch[j], BxnT], writes=[Bps[2]])
                        op("act", lambda e: e.activation(out=junk[:, 0:128], in_=ps[2][:, 0:128], func=AF.Square, accum_out=st2[:, 0:1]),
                           reads=[Bps[2]], writes=[Bjunk_act, Bst2])
                        rstd_from_ss(st2, Bst2, 0, 1, 128.0)
                        op("dve", lambda e, gt=gt: e.scalar_tensor_tensor(out=c_tm[:, gt, :], in0=ps[2][:, 0:128], scalar=st2[:, 1:2], in1=gkv_bc[:, :],
                                                                           op0=ALU.mult, op1=ALU.mult), reads=[Bps[2], Bst2, Bconst], writes=[Bctm])
                        op("pe", lambda e, gt=gt: e.transpose(out=psbf(3)[:, 0:128], in_=c_tm[:, gt, :], identity=ident[:, :]),
                           reads=[Bctm, Bident], writes=[Bps[3]])
                        op("dve", lambda e, gt=gt: e.tensor_copy(out=cT[:, gt * 128:(gt + 1) * 128], in_=psbf(3)[:, 0:128]), reads=[Bps[3]], writes=[BcT])
                    yield
                    j = load_chunk(CH["kw"][0])
                    for i in range(4):
                        gt = tb * 4 + i
                        def fk(e, i=i, j=j):
                            ins = None
                            for k in range(8):
                                ins = e.matmul(ps[2][:, 0:72], lhsT=xnT[:, k, i * 128:(i + 1) * 128], rhs=wch[j][:, k, 0:72], start=(k == 0), stop=(k == 7))
                            return ins
                        op("pe", fk, reads=[Bwch[j], BxnT], writes=[Bps[2]])
                        op("dve", lambda e, gt=gt: e.tensor_scalar(out=w_tm[:, gt, :], in0=ps[2][:, 64:72], scalar1=8.0 ** -0.5, scalar2=None, op0=ALU.mult),
                           reads=[Bps[2]], writes=[Bwtm])
                        op("dve", lambda e: e.tensor_reduce(out=st2[:, 2:3], in_=ps[2][:, 0:64], axis=AX.X, op=ALU.add), reads=[Bps[2]], writes=[Bst2])
                        op("dve", lambda e: e.tensor_scalar(out=st2[:, 2:3], in0=st2[:, 2:3], scalar1=-1.0 / 64.0, scalar2=None, op0=ALU.mult), reads=[Bst2], writes=[Bst2])
                        op("dve", lambda e: e.tensor_scalar(out=kc[:, :], in0=ps[2][:, 0:64], scalar1=st2[:, 2:3], scalar2=None, op0=ALU.add),
                           reads=[Bps[2], Bst2], writes=[Bkc])
                        op("act", lambda e: e.activation(out=junk[:, 0:64], in_=kc[:, :], func=AF.Square, accum_out=st2[:, 3:4]),
                           reads=[Bkc], writes=[Bjunk_act, Bst2])
                        rstd_from_ss(st2, Bst2, 3, 4, 64.0)
                        op("dve", lambda e: e.scalar_tensor_tensor(out=kc[:, :], in0=kc[:, :], scalar=st2[:, 4:5], in1=lng_bc[:, :], op0=ALU.mult, op1=ALU.mult),
                           reads=[Bkc, Bst2, Bconst], writes=[Bkc])
                        op("dve", lambda e: e.tensor_tensor(out=kn2[:, 0:64], in0=kc[:, :], in1=lnb_bc[:, :], op=ALU.add), reads=[Bkc, Bconst], writes=[Bkn2])
                        op("dve", lambda e: e.tensor_copy(out=kn2[:, 64:128], in_=kn2[:, 0:64]), reads=[Bkn2], writes=[Bkn2])
                        op("pe", lambda e: e.transpose(out=psbf(3)[:, 0:128], in_=kn2[:, :], identity=ident[:, :]), reads=[Bkn2, Bident], writes=[Bps[3]])
                        for par in range(2):
                            op("dve", lambda e, gt=gt, par=par: e.tensor_copy(out=kz[par][par * 64:par * 64 + 64, gt * 128:(gt + 1) * 128],
                                                                               in_=psbf(3)[par * 64:par * 64 + 64, 0:128]), reads=[Bps[3]], writes=[BkT])
                    yield
                def b_gattn():
                    for c in range(8):
                        j = load_chunk(CH["gattn"][c]); bk = next_bank()
                        proj_fm(j, 128, bk, xnT, BxnT)
                        op("act", lambda e, c=c, bk=bk: e.activation(out=sgT[:, c, :], in_=ps[bk][:, :], func=AF.Silu), reads=[Bps[bk]], writes=[BsgT[c]])

                chk(2)
                def topk_gen(i, gt):
                    ns = gt + 1
                    ncols = ns * 128
                    bis, Bbis, nsm, Bnsm = bis_s[i % 2], Bbis_s[i % 2], nsm_s[i % 2], Bnsm_s[i % 2]
                    nsb = (ncols + 511) // 512
                    for sbk in range(nsb):
                        c0 = sbk * 512
                        cw = min(512, ncols - c0)
                        for jh in range(8):
                            par = jh % 2
                            bk = 6 + (jh % 2)
                            op("pe", lambda e, jh=jh, par=par, bk=bk, c0=c0, cw=cw: e.matmul(
                                ps[bk][:, 0:cw], lhsT=qidxT[:, jh // 2, i * 128:(i + 1) * 128],
                                rhs=kz[par][:, c0:c0 + cw], start=True, stop=True),
                               reads=[BqidxT, BkT], writes=[Bps[bk]])
                            rb = jh % 2
                            op("act", lambda e, bk=bk, rb=rb, cw=cw: e.activation(out=rj[rb][:, 0:cw], in_=ps[bk][:, 0:cw], func=AF.Relu),
                               reads=[Bps[bk]], writes=[Brj[rb]])
                            if jh == 0:
                                op("dve", lambda e, rb=rb, c0=c0, cw=cw: e.tensor_scalar(
                                    out=I_t[:, c0:c0 + cw], in0=rj[rb][:, 0:cw], scalar1=w_tm[:, gt, 0:1], scalar2=None, op0=ALU.mult),
                                   reads=[Brj[rb], Bwtm], writes=[BI, Bres[0], Bres[1]])
                            else:
                                op("dve", lambda e, rb=rb, c0=c0, cw=cw, jh=jh: e.scalar_tensor_tensor(
                                    out=I_t[:, c0:c0 + cw], in0=rj[rb][:, 0:cw], scalar=w_tm[:, gt, jh:jh + 1], in1=I_t[:, c0:c0 + cw],
                                    op0=ALU.mult, op1=ALU.add), reads=[Brj[rb], Bwtm, BI], writes=[BI])
                            if jh % 2 == 1:
                                yield "idx"
                    yield "seg"
                    op("dve", lambda e: e.tensor_reduce(out=bis[:, 0:1], in_=I_t[:, 0:ncols], axis=AX.X, op=ALU.max,
                                                        apply_absolute_value=True), reads=[BI], writes=[Bbis])
                    op("dve", lambda e: e.tensor_scalar(out=bis[:, 0:1], in0=bis[:, 0:1], scalar1=1.01, scalar2=1e-6, op0=ALU.mult, op1=ALU.add),
                       reads=[Bbis], writes=[Bbis])
                    op("dve", lambda e: e.tensor_scalar(out=steps[:, :], in0=pow2[:, :], scalar1=bis[:, 0:1], scalar2=None, op0=ALU.mult),
                       reads=[Bbis, Bconst], writes=[Bsteps])
                    op("dve", lambda e: e.tensor_scalar(out=steps2[:, :], in0=steps[:, :], scalar1=2.0, scalar2=None, op0=ALU.mult),
                       reads=[Bsteps], writes=[Bsteps])
                    op("dve", lambda e: e.tensor_tensor(out=I_t[:, gt * 128:(gt + 1) * 128], in0=I_t[:, gt * 128:(gt + 1) * 128],
                                                        in1=causal_neg[:, :], op=ALU.add), reads=[BI, Bconst, Bbis], writes=[BI])
                    op("dve", lambda e: e.memset(bis[:, 1:2], 0.0), reads=[Bbis], writes=[Bbis])
                    for kb in range(NBIS):
                        op("dve", lambda e: e.tensor_scalar(out=mask[:, 0:ncols], in0=I_t[:, 0:ncols], scalar1=bis[:, 1:2], scalar2=None,
                                                            op0=ALU.is_ge, op1=ALU.add, accum_out=bis[:, 2:3]),
                           reads=[BI, Bbis], writes=[Bmask, Bbis])
                        op("dve", lambda e, kb=kb: e.tensor_scalar(out=bis[:, 3:4], in0=bis[:, 2:3], scalar1=TOPK - 0.5, scalar2=steps2[:, kb:kb + 1],
                                                                   op0=ALU.is_ge, op1=ALU.mult), reads=[Bbis, Bsteps], writes=[Bbis])
                        op("dve", lambda e, kb=kb: e.scalar_tensor_tensor(out=bis[:, 1:2], in0=bis[:, 1:2], scalar=steps[:, kb:kb + 1], in1=bis[:, 3:4],
                                                                          op0=ALU.subtract, op1=ALU.add), reads=[Bbis, Bsteps], writes=[Bbis])
                        if kb in (3, 7):
                            yield "seg"
                    op("dve", lambda e: e.tensor_scalar(out=mask[:, 0:ncols], in0=I_t[:, 0:ncols], scalar1=bis[:, 1:2], scalar2=None,
                                                        op0=ALU.is_ge), reads=[BI, Bbis], writes=[Bmask])
                    op("dve", lambda e: e.scalar_tensor_tensor(out=I_t[:, 0:ncols], in0=I_t[:, 0:ncols], scalar=bis[:, 1:2],
                                                               in1=iota1_full[:, 0:ncols], op0=ALU.is_ge, op1=ALU.mult),
                       reads=[BI, Bbis, Bconst], writes=[BI])
                    op("dve", lambda e: e.tensor_reduce(out=bis[:, 4:5], in_=I_t[:, 0:ncols], axis=AX.X, op=ALU.max), reads=[BI], writes=[Bbis])
                    op("dve", lambda e: e.tensor_scalar(out=nsm[:, :], in0=bis[:, 4:5], scalar1=-1.0, scalar2=1.0, op0=ALU.mult, op1=ALU.add),
                       reads=[Bbis], writes=[Bnsm])

                def nmask_build(i, gt):
                    for jj in range(gt + 1):
                        mbk = 6 + (jj % 2)
                        op("pe", lambda e, jj=jj, mbk=mbk: e.matmul(ps[mbk][:, :], lhsT=mask[:, jj * 128:(jj + 1) * 128], rhs=identrep4[:, :],
                                                                     start=True, stop=True), reads=[Bmask, Bconst], writes=[Bps[mbk]])
                        op("act", lambda e, jj=jj, mbk=mbk: e.activation(out=nmask[:, jj, :], in_=ps[mbk][:, :], func=AF.Identity,
                                                                          bias=nb32[:, 0:1], scale=32768.0), reads=[Bps[mbk], Bconst], writes=[Bnmask[jj]])

                def rnn_steps(c, k):
                    xc, Bxc, xcb, Bxcb = xc_s[k], Bxc_s[k], xcb_s[k], Bxcb_s[k]
                    rr, Brr, ii, Bii, aa, Baa, ss, Bss = r_s[k], Br_s[k], i_s[k], Bi_s[k], a_s[k], Ba_s[k], s_s[k], Bs_s[k]
                    sg, Bsg, uu, Buu = sgr_s[k], Bsgr_s[k], ub[k], Bub[k]
                    j = load_chunk(CH["xrnn"][c]); bk = next_bank()
                    proj_fm(j, 128, bk, xnT, BxnT)
                    if tb == 0:
                        op("pool", lambda e: e.memset(uu[:, 0:3], 0.0), reads=[], writes=[Buu])
                        op("dve", lambda e: e.memset(hcar[:, c:c + 1], 0.0), reads=[], writes=[Bhcar[c]])
                    else:
                        op("pool", lambda e: e.tensor_copy(out=uu[:, 0:3], in_=ucar[:, c, :]), reads=[Bucar[c]], writes=[Buu])
                    op("act", lambda e: e.copy(out=uu[:, 3:515], in_=ps[bk][:, :]), reads=[Bps[bk]], writes=[Buu])
                    yield
                    op("dve", lambda e: e.tensor_scalar(out=xc[:, :], in0=uu[:, 3:515], scalar1=cwcol[:, 3, c:c + 1], scalar2=cbcol[:, c:c + 1],
                                                        op0=ALU.mult, op1=ALU.add), reads=[Buu, Bconst], writes=[Bxc])
                    for kk in range(3):
                        op("dve", lambda e, kk=kk: e.scalar_tensor_tensor(out=xc[:, :], in0=uu[:, kk:kk + 512], scalar=cwcol[:, kk, c:c + 1],
                                                                          in1=xc[:, :], op0=ALU.mult, op1=ALU.add),
                           reads=[Buu, Bconst, Bxc], writes=[Bxc])
                    op("pool", lambda e: e.tensor_copy(out=ucar[:, c, :], in_=uu[:, 512:515]), reads=[Buu], writes=[Bucar[c]])
                    op("pool", lambda e: e.tensor_copy(out=xcb[:, :], in_=xc[:, :]), reads=[Bxc], writes=[Bxcb])
                    yield
                    bk1 = next_bank()
                    op("pe", lambda e: e.matmul(ps[bk1][:, :], lhsT=BDa[:, c, :], rhs=xcb[:, :], start=True, stop=True),
                       reads=[Bxcb, Bconst], writes=[Bps[bk1]])
                    op("act", lambda e: e.activation(out=rr[:, :], in_=ps[bk1][:, :], func=AF.Sigmoid, bias=bacol[:, c:c + 1], scale=1.0),
                       reads=[Bps[bk1], Bconst], writes=[Brr])
                    bk2 = next_bank()
                    op("pe", lambda e: e.matmul(ps[bk2][:, :], lhsT=BDx[:, c, :], rhs=xcb[:, :], start=True, stop=True),
                       reads=[Bxcb, Bconst], writes=[Bps[bk2]])
                    op("act", lambda e: e.activation(out=ii[:, :], in_=ps[bk2][:, :], func=AF.Sigmoid, bias=bxcol[:, c:c + 1], scale=1.0),
                       reads=[Bps[bk2], Bconst], writes=[Bii])
                    yield
                    op("act", lambda e: e.activation(out=aa[:, :], in_=rr[:, :], func=AF.Exp, scale=kap[:, c:c + 1]), reads=[Brr, Bconst], writes=[Baa])
                    op("act", lambda e: e.activation(out=ss[:, :], in_=rr[:, :], func=AF.Exp, scale=kap2[:, c:c + 1]), reads=[Brr, Bconst], writes=[Bss])
                    yield
                    op("act", lambda e: e.activation(out=ss[:, :], in_=ss[:, :], func=AF.Sqrt, bias=1.0, scale=-1.0), reads=[Bss], writes=[Bss])
                    op("dve", lambda e: e.tensor_tensor(out=ii[:, :], in0=ii[:, :], in1=xc[:, :], op=ALU.mult), reads=[Bii, Bxc], writes=[Bii])
                    yield
                    op("dve", lambda e: e.tensor_tensor(out=ii[:, :], in0=ii[:, :], in1=ss[:, :], op=ALU.mult), reads=[Bii, Bss], writes=[Bii])
                    op("dve", lambda e: e.tensor_tensor_scan(out=rr[:, :], data0=aa[:, :], data1=ii[:, :], initial=hcar[:, c:c + 1],
                                                             op0=ALU.mult, op1=ALU.add), reads=[Baa, Bii, Bhcar[c]], writes=[Brr])
                    op("dve", lambda e: e.tensor_copy(out=hcar[:, c:c + 1], in_=rr[:, 511:512]), reads=[Brr], writes=[Bhcar[c]])
                    j2 = load_chunk(CH["grnn"][c]); bk3 = next_bank()
                    proj_fm(j2, 128, bk3, xnT, BxnT)
                    yield
                    op("act", lambda e: e.activation(out=sg[:, :], in_=ps[bk3][:, :], func=AF.Silu), reads=[Bps[bk3]], writes=[Bsg])
                    op("dve", lambda e: e.tensor_tensor(out=hgT[:, c, :], in0=rr[:, :], in1=sg[:, :], op=ALU.mult), reads=[Brr, Bsg], writes=[BhgT])

                def c_rounds():
                    for c in range(0, 8, 2):
                        g0, g1 = rnn_steps(c, 0), rnn_steps(c + 1, 1)
                        alive = [g0, g1]
                        while alive:
                            for g in list(alive):
                                try:
                                    next(g)
                                except StopIteration:
                                    alive.remove(g)
                            yield

                tk0 = topk_gen(0, tb * 4) if tb >= 1 else None
                tk0s = Stepper(tk0)
                bg, cg = b_steps(), c_rounds()
                b_done, c_done, rnd = False, False, 0
                _END = object()
                while not (b_done and c_done):
                    if not b_done:
                        b_done = next(bg, _END) is _END
                    if not c_done:
                        c_done = next(cg, _END) is _END
                    rnd += 1
                    if b_done and tk0 is not None and rnd % 6 == 0:
                        tk0s.seg()
                b_gattn()
                if tk0 is not None:
                    tk0s.drain()
                    nmask_build(0, tb * 4)
                chk(3)
                def attn_prologue(i, gt):
                    nsm, Bnsm = nsm_s[i % 2], Bnsm_s[i % 2]
                    if gt < 2:
                        op("dve", lambda e: e.tensor_scalar(out=nsm[:, :], in0=pidx[:, :], scalar1=-1.0, scalar2=-128.0 * gt, op0=ALU.mult, op1=ALU.add),
                           reads=[Bconst], writes=[Bnsm])
                    def fql_(hg):
                        bq = 6 + (hg % 2)
                        def fql(e):
                            ins = None
                            for hl in range(4):
                                h = 4 * hg + hl
                                ins = e.matmul(ps[bq][:, hl * 128:(hl + 1) * 128], lhsT=wukTz[:, h, :],
                                               rhs=qT[:, h // 2, i * 128:(i + 1) * 128], start=True, stop=True)
                            return ins
                        op("pe", fql, reads=[BqT, Bconst], writes=[Bps[bq]])
                        op("act", lambda e: e.mul(out=qlb_s[hg][:, :], in_=ps[bq][:, :], mul=0.125), reads=[Bps[bq]], writes=[Bqlb_s[hg]])
                        op("act", lambda e: e.activation(out=absq_s[hg % 2][:, :], in_=ps[bq][:, :], func=AF.Square, scale=0.125),
                           reads=[Bps[bq]], writes=[Babsq_s[hg % 2]])
                    def fnm_(hg):
                        bn = 4 + (hg % 2)
                        def fnm(e):
                            e.matmul(ps[bn][0:1, :], lhsT=nhc_bf[:, 0:1], rhs=absq_s[hg % 2][:, :], start=True, stop=False)
                            return e.matmul(ps[bn][0:1, :], lhsT=nsm[:, 0:1], rhs=SI[:, hg, :], start=False, stop=True)
                        op("pe", fnm, reads=[Babsq_s[hg % 2], Bnsm, Bconst], writes=[Bps[bn]])
                        op("act", lambda e: e.activation(out=Bwork[hg][0:1, :], in_=ps[bn][0:1, :], func=AF.Identity, bias=nhc[0:1, 0:1], scale=1.0),
                           reads=[Bps[bn], Bconst], writes=[BBwork[hg]])
                    fql_(0); fql_(1); fnm_(0); fql_(2); fnm_(1); fql_(3); fnm_(2); fnm_(3)

                def attn_hg(i, gt, hg, stp=None):
                    ns = gt + 1
                    qlb, Bqlb = qlb_s[hg], Bqlb_s[hg]
                    def flg_exp(js):
                        lb = js % 2
                        if gt >= 2:
                            mrhs, mbuf = nmask[:, js, :], Bnmask[js]
                        elif js == gt:
                            mrhs, mbuf = ntri_rep[:, :], Bconst
                        else:
                            mrhs, mbuf = None, None
                        def flg(e):
                            e.matmul(ps[lb][:, :], lhsT=cT[:, js * 128:(js + 1) * 128], rhs=qlb[:, :], start=True, stop=False)
                            if mrhs is None:
                                return e.matmul(ps[lb][:, :], lhsT=A_all[:, js, :], rhs=Bwork[hg][:, :], start=False, stop=True)
                            e.matmul(ps[lb][:, :], lhsT=A_all[:, js, :], rhs=Bwork[hg][:, :], start=False, stop=False)
                            return e.matmul(ps[lb][:, :], lhsT=ident[:, :], rhs=mrhs, start=False, stop=True)
                        op("pe", flg, reads=[BcT, Bqlb, BBwork[hg], Bconst] + ([mbuf] if mbuf is not None else []), writes=[Bps[lb]])
                        op("act", lambda e: e.activation(out=p_t[lb][:, :], in_=ps[lb][:, :], func=AF.Exp), reads=[Bps[lb]], writes=[Bp[lb]])
                    bo, bs_ = (2, 3) if hg % 2 == 0 else (4, 5)
                    def fpv_(js):
                        lb = js % 2
                        def fpv(e):
                            e.matmul(ps[bo][:, :], lhsT=c_tm[:, js, :], rhs=p_t[lb][:, :], start=(js == 0), stop=(js == ns - 1))
                            return e.matmul(ps[bs_][:, :], lhsT=ones_bf[:, :], rhs=p_t[lb][:, :], start=(js == 0), stop=(js == ns - 1))
                        op("pe", fpv, reads=[Bctm, Bp[lb], Bconst], writes=[Bps[bo], Bps[bs_]])
                    flg_exp(0)
                    for js in range(ns):
                        if js + 1 < ns:
                            flg_exp(js + 1)
                        fpv_(js)
                        if stp is not None:
                            stp.tick()
                    rs_t, Brs, oTn, BoTn = rs_s[hg % 2], Brs_s[hg % 2], oTn_s[hg % 2], BoTn_s[hg % 2]
                    op("dve", lambda e: e.reciprocal(out=rs_t[:, :], in_=ps[bs_][:, :]), reads=[Bps[bs_]], writes=[Brs])
                    op("dve", lambda e: e.tensor_tensor(out=oTn[:, :], in0=ps[bo][:, :], in1=rs_t[:, :], op=ALU.mult), reads=[Bps[bo], Brs], writes=[BoTn])

                def attn_epi(i, hg):
                    oTn, BoTn = oTn_s[hg % 2], BoTn_s[hg % 2]
                    def fy(e):
                        ins = None
                        for pp in range(2):
                            for q2 in range(2):
                                hl = 2 * pp + q2
                                h = 4 * hg + hl
                                ins = e.matmul(ps[6][:, pp * 128:(pp + 1) * 128], lhsT=wuvP[:, h, :], rhs=oTn[:, hl * 128:(hl + 1) * 128],
                                               start=(q2 == 0), stop=(q2 == 1))
                        return ins
                    op("pe", fy, reads=[BoTn, Bconst], writes=[Bps[6]])
                    for pp in range(2):
                        cc = 2 * hg + pp
                        op("dve", lambda e, pp=pp, cc=cc: e.tensor_tensor(out=sgT[:, cc, i * 128:(i + 1) * 128], in0=ps[6][:, pp * 128:(pp + 1) * 128],
                                                                           in1=sgT[:, cc, i * 128:(i + 1) * 128], op=ALU.mult),
                           reads=[Bps[6], BsgT[cc]], writes=[BsgT[cc]])

                gens = {}
                for i in range(1, 4):
                    if tb * 4 + i >= 2:
                        gens[i] = topk_gen(i, tb * 4 + i)
                attn_prologue(0, tb * 4)
                for i in range(4):
                    gt = tb * 4 + i
                    g = gens.get(i + 1)
                    stp = Stepper(g)
                    for hg in range(4):
                        attn_hg(i, gt, hg, stp)
                        if hg > 0:
                            attn_epi(i, hg - 1)
                        if g is not None and hg >= 1:
                            stp.seg()
                    stp.drain()
                    if i + 1 < 4:
                        attn_prologue(i + 1, gt + 1)
                    if g is not None:
                        nmask_build(i + 1, gt + 1)
                    attn_epi(i, 3)

                chk(4)
                mixedT, BmixedT = qT, BqT
                for f in range(8):
                    ja = load_chunk(CH["wap"][f])
                    def fya(e, ja=ja):
                        ins = None
                        for k in range(8):
                            ins = e.matmul(ps[0][:, :], lhsT=wch[ja][:, k, :], rhs=sgT[:, k, :], start=(k == 0), stop=(k == 7))
                        return ins
                    op("pe", fya, reads=[Bwch[ja]] + BsgT, writes=[Bps[0]])
                    jr = load_chunk(CH["wrp"][f])
                    proj_fm(jr, 128, 1, hgT, BhgT)
                    jg = load_chunk(CH["mga"][f])
                    proj_fm(jg, 128, 2, xnT, BxnT)
                    op("act", lambda e, f=f: e.activation(out=ga_t[:, :], in_=ps[2][:, :], func=AF.Sigmoid, bias=bmcol[:, f:f + 1], scale=1.0),
                       reads=[Bps[2], Bconst], writes=[Bga])
                    jg2 = load_chunk(CH["mgr"][f])
                    proj_fm(jg2, 128, 3, xnT, BxnT)
                    op("act", lambda e, f=f: e.activation(out=gr_t[:, :], in_=ps[3][:, :], func=AF.Sigmoid, bias=bmcol[:, 8 + f:9 + f], scale=1.0),
                       reads=[Bps[3], Bconst], writes=[Bgr])
                    op("dve", lambda e: e.tensor_tensor(out=m1_t[:, :], in0=ps[0][:, :], in1=ga_t[:, :], op=ALU.mult), reads=[Bps[0], Bga], writes=[Bm1])
                    op("dve", lambda e: e.tensor_tensor(out=m2_t[:, :], in0=ps[1][:, :], in1=gr_t[:, :], op=ALU.mult), reads=[Bps[1], Bgr], writes=[Bm2])
                    op("pool", lambda e, f=f: e.tensor_tensor(out=mixedT[:, f, :], in0=m1_t[:, :], in1=m2_t[:, :], op=ALU.add), reads=[Bm1, Bm2], writes=[BmixedT])
                for i in range(4):
                    xb = i % 2
                    rk = i % 2
                    res_t = I_t[:, rk * 1024:(rk + 1) * 1024]
                    S_.dma("sp", "xl%d" % xb, xt[xb][:, :], x[sq, t0 + i * 128:t0 + (i + 1) * 128, :], writes=[Bxt[xb]])
                    for db in range(2):
                        def fo(e, i=i, db=db):
                            ins = None
                            for k in range(8):
                                ins = e.matmul(ps[4 + db][:, :], lhsT=mixedT[:, k, i * 128:(i + 1) * 128], rhs=wout_sb[:, k, db * 512:(db + 1) * 512],
                                               start=(k == 0), stop=(k == 7))
                            return ins
                        op("pe", fo, reads=[BmixedT, Bconst], writes=[Bps[4 + db]])
                        op("dve", lambda e, db=db, xb=xb, res_t=res_t: e.tensor_tensor(out=res_t[:, db * 512:(db + 1) * 512], in0=ps[4 + db][:, :],
                                                                                       in1=xt[xb][:, db * 512:(db + 1) * 512], op=ALU.add),
                           reads=[Bps[4 + db], Bxt[xb]], writes=[Bres[rk], BI])
                    op("act", lambda e, res_t=res_t: e.activation(out=junk[:, 0:1024], in_=res_t, func=AF.Square, accum_out=st[:, 2:3]),
                       reads=[Bres[rk]], writes=[Bjunk_act, Bst])
                    rstd_from_ss(st, Bst, 2, 3, 1024.0)
                    op("dve", lambda e, res_t=res_t: e.scalar_tensor_tensor(out=res_t, in0=res_t, scalar=st[:, 3:4], in1=fg_bc[:, :],
                                                                            op0=ALU.mult, op1=ALU.mult), reads=[Bres[rk], Bst, Bconst], writes=[Bres[rk]])
                    S_.dma("pool", "os%d" % rk, out[sq, t0 + i * 128:t0 + (i + 1) * 128, :], res_t, reads=[Bres[rk]])
    except _Stop:
        pass
    for key in sorted(S_.dsem):
        nc.gpsimd.wait_ge(S_.dsem[key][0], S_.dsem[key][1])
    for k_ in S_.sem:
        if S_.cnt[k_] > 0 and k_ != "pool":
            nc.gpsimd.wait_ge(S_.sem[k_], S_.cnt[k_])
    return nc


_PARAMS = ["norm_gain", "w_in", "b_merge", "kv_norm_gain", "w_uk", "w_uv", "idx_ln_gain", "idx_ln_bias", "w_attn_proj",
           "conv_w", "conv_b", "w_rg_a", "b_rg_a", "w_rg_x", "b_rg_x", "lru_lambda", "w_rnn_proj", "w_out"]


def kernel(**inputs):
    x = np.ascontiguousarray(np.asarray(inputs["x"], dtype=np.float32))
    B, S, _ = x.shape
    nseq = B // NCORES
    base = {}
    for k in _PARAMS:
        a = np.asarray(inputs[k], dtype=np.float32)
        base[k] = np.ascontiguousarray(a.reshape(a.shape[1:]))
    base["final_norm_gain"] = np.ascontiguousarray(np.asarray(inputs["final_norm_gain"], dtype=np.float32))
    nc = build(nseq, S)
    in_maps = []
    for c in range(NCORES):
        m = dict(base)
        m["x"] = np.ascontiguousarray(x[c * nseq:(c + 1) * nseq])
        in_maps.append(m)
    res = run_bass_kernel_spmd(nc, in_maps, core_ids=list(range(NCORES)))
    return np.concatenate([np.asarray(r["out"], dtype=np.float32) for r in res.results], axis=0)
```
